# Optimizing a Trainium2 kernel written in Bass

```python
import math
import jax, jax.numpy as jnp
from jax import lax
import numpy as np

D_MODEL = 1024
BATCH = 8
SEQ = 2048
DEPTH = 1

D_SSM = 512
SSM_GROUP_WIDTH = 16
SSM_GROUPS = D_SSM // SSM_GROUP_WIDTH
SSM_STATE = 64
DT_MIN = 0.001
DT_MAX = 0.1
D_CONV = 512
CONV_WIDTH = 31
D_IN = D_SSM + 2 * D_CONV + 2 * D_MODEL
N_GROUPS_MOE = 4
EXPERTS_PER_GROUP = 8
N_EXPERTS = N_GROUPS_MOE * EXPERTS_PER_GROUP
TOPK_IN_GROUP = 2
D_EXPERT = 512
ROW_BLOCK = 128
D_PLE = 256
EPS = 1e-6

kernel_name = "hybrid_s5_conformer_hiermoe_block"


def rmsnorm(x, g):
    x32 = x.astype(jnp.float32)
    y = x32 * lax.rsqrt(jnp.mean(x32 * x32, axis=-1, keepdims=True) + EPS)
    return (y * g.astype(jnp.float32)).astype(x.dtype)


def layernorm(x, g, b):
    x32 = x.astype(jnp.float32)
    mu = jnp.mean(x32, axis=-1, keepdims=True)
    var = jnp.mean(jnp.square(x32 - mu), axis=-1, keepdims=True)
    y = (x32 - mu) * lax.rsqrt(var + EPS)
    return (y * g.astype(jnp.float32) + b.astype(jnp.float32)).astype(x.dtype)


def _complex_affine_combine(earlier, later):
    a_re, a_im, b_re, b_im = earlier
    c_re, c_im, d_re, d_im = later
    n_a_re = c_re * a_re - c_im * a_im
    n_a_im = c_re * a_im + c_im * a_re
    n_b_re = c_re * b_re - c_im * b_im + d_re
    n_b_im = c_re * b_im + c_im * b_re + d_im
    return (n_a_re, n_a_im, n_b_re, n_b_im)


def s5_ssm(u, a_re, a_im, log_dt, b_re, b_im, c_re, c_im, d):
    bsz, seq, _ = u.shape
    u32 = u.astype(jnp.float32).reshape(bsz, seq, SSM_GROUPS, SSM_GROUP_WIDTH)
    ar = a_re.astype(jnp.float32)
    ai = a_im.astype(jnp.float32)
    dt = jnp.exp(log_dt.astype(jnp.float32))[:, None]
    mag = jnp.exp(ar * dt)
    lam_re = mag * jnp.cos(ai * dt)
    lam_im = mag * jnp.sin(ai * dt)
    den = ar * ar + ai * ai
    nr = lam_re - 1.0
    ni = lam_im
    z_re = (nr * ar + ni * ai) / den
    z_im = (ni * ar - nr * ai) / den
    br = b_re.astype(jnp.float32)
    bi = b_im.astype(jnp.float32)
    bbar_re = z_re[..., None] * br - z_im[..., None] * bi
    bbar_im = z_re[..., None] * bi + z_im[..., None] * br
    bu_re = jnp.einsum('blgc,gnc->blgn', u32, bbar_re)
    bu_im = jnp.einsum('blgc,gnc->blgn', u32, bbar_im)
    lam_re_b = jnp.broadcast_to(lam_re, bu_re.shape)
    lam_im_b = jnp.broadcast_to(lam_im, bu_im.shape)
    _, _, s_re, s_im = lax.associative_scan(
        _complex_affine_combine, (lam_re_b, lam_im_b, bu_re, bu_im), axis=1)
    y = (jnp.einsum('blgn,gcn->blgc', s_re, c_re.astype(jnp.float32))
         - jnp.einsum('blgn,gcn->blgc', s_im, c_im.astype(jnp.float32)))
    y = y.reshape(bsz, seq, D_SSM) + d.astype(jnp.float32) * u32.reshape(bsz, seq, D_SSM)
    return y.astype(u.dtype)


def conformer_conv(v, dw, dw_b, ln_g, ln_b, w_pw_out):
    val, gate = jnp.split(v, 2, axis=-1)
    z = val * jax.nn.sigmoid(gate)
    z = lax.conv_general_dilated(
        z, dw[:, None, :].astype(z.dtype), window_strides=(1,),
        padding=[(CONV_WIDTH - 1, 0)],
        dimension_numbers=('NWC', 'WIO', 'NWC'),
        feature_group_count=D_CONV) + dw_b
    z = layernorm(z, ln_g, ln_b)
    z = jax.nn.silu(z)
    return z @ w_pw_out


def hierarchical_moe(h, w_rg, b_rg, w_re, b_re, w_gate, w_up, w_down):
    bsz, seq, dm = h.shape
    n_tok = bsz * seq
    ht = h.reshape(n_tok, dm)
    g_logits = (ht @ w_rg + b_rg).astype(jnp.float32)
    g_prob = jax.nn.softmax(g_logits, axis=-1)
    g_sel = jnp.argmax(g_logits, axis=-1).astype(jnp.int32)
    p_g = jnp.take_along_axis(g_prob, g_sel[:, None], axis=1)[:, 0]
    e_logits = (ht @ w_re + b_re).astype(jnp.float32).reshape(n_tok, N_GROUPS_MOE, EXPERTS_PER_GROUP)
    e_sel_logits = jnp.take_along_axis(e_logits, g_sel[:, None, None], axis=1)[:, 0]
    top_v, top_j = lax.top_k(e_sel_logits, TOPK_IN_GROUP)
    wts = jax.nn.softmax(top_v, axis=-1) * p_g[:, None]
    eid = (g_sel[:, None] * EXPERTS_PER_GROUP + top_j).reshape(-1).astype(jnp.int32)
    tok = jnp.repeat(jnp.arange(n_tok, dtype=jnp.int32), TOPK_IN_GROUP)
    wflat = wts.reshape(-1)
    n_assign = n_tok * TOPK_IN_GROUP

    order = jnp.argsort(eid)
    e_sorted = eid[order]
    counts = jnp.bincount(eid, length=N_EXPERTS).astype(jnp.int32)
    starts = jnp.cumsum(counts) - counts
    pcounts = (counts + ROW_BLOCK - 1) // ROW_BLOCK * ROW_BLOCK
    pends = jnp.cumsum(pcounts)
    pstarts = pends - pcounts
    dest = pstarts[e_sorted] + jnp.arange(n_assign, dtype=jnp.int32) - starts[e_sorted]
    n_blocks = (n_assign + N_EXPERTS * (ROW_BLOCK - 1) + ROW_BLOCK - 1) // ROW_BLOCK
    n_rows = n_blocks * ROW_BLOCK
    row_tok = jnp.full((n_rows,), n_tok, jnp.int32).at[dest].set(tok[order])
    row_w = jnp.zeros((n_rows,), jnp.float32).at[dest].set(wflat[order])
    blk_exp = jnp.minimum(
        jnp.searchsorted(pends, jnp.arange(n_blocks, dtype=jnp.int32) * ROW_BLOCK, side='right'),
        N_EXPERTS - 1).astype(jnp.int32)
    x_pad = jnp.concatenate([ht, jnp.zeros((1, dm), ht.dtype)], axis=0)
    xr = x_pad[row_tok].reshape(n_blocks, ROW_BLOCK, dm)

    def expert_block(args):
        xb, e = args
        return (jax.nn.silu(xb @ w_gate[e]) * (xb @ w_up[e])) @ w_down[e]

    yr = lax.map(expert_block, (xr, blk_exp)).reshape(n_rows, dm)
    out = jnp.zeros((n_tok, dm), h.dtype).at[row_tok].add(
        yr * row_w[:, None].astype(h.dtype), mode='drop')
    return out.reshape(bsz, seq, dm)


def setup_inputs(seed: int = 0) -> dict:
    key = jax.random.key(seed)
    ks = jax.random.split(key, 40)
    f32 = jnp.float32
    L, D = DEPTH, D_MODEL
    G, N, C = SSM_GROUPS, SSM_STATE, SSM_GROUP_WIDTH

    def nrm(k, shape, scale):
        return jax.random.normal(k, shape, f32) * scale

    def gain(k, shape):
        return 1.0 + 0.02 * jax.random.normal(k, shape, f32)

    a_re = -0.5 + 0.01 * jax.random.normal(ks[3], (L, G, N), f32)
    a_im = (math.pi * jnp.arange(N, dtype=f32))[None, None, :] + 0.01 * jax.random.normal(ks[4], (L, G, N), f32)
    log_dt = jax.random.uniform(ks[5], (L, G), f32, math.log(DT_MIN), math.log(DT_MAX))
    return {
        "x": jax.random.normal(ks[0], (BATCH, SEQ, D), f32),
        "p": jax.random.normal(ks[1], (DEPTH, BATCH, SEQ, D_PLE), f32),
        "g_mix": gain(ks[2], (L, D)),
        "w_in": nrm(ks[6], (L, D, D_IN), D ** -0.5),
        "b_gate": nrm(ks[7], (L, 2 * D), 0.02),
        "ssm_a_re": a_re,
        "ssm_a_im": a_im,
        "ssm_log_dt": log_dt,
        "ssm_b_re": nrm(ks[8], (L, G, N, C), (2 * C) ** -0.5),
        "ssm_b_im": nrm(ks[9], (L, G, N, C), (2 * C) ** -0.5),
        "ssm_c_re": nrm(ks[10], (L, G, C, N), N ** -0.5),
        "ssm_c_im": nrm(ks[11], (L, G, C, N), N ** -0.5),
        "ssm_d": nrm(ks[12], (L, D_SSM), 1.0),
        "w_glu": nrm(ks[13], (L, D_SSM, 2 * D), D_SSM ** -0.5),
        "conv_dw": nrm(ks[14], (L, CONV_WIDTH, D_CONV), CONV_WIDTH ** -0.5),
        "conv_dw_b": nrm(ks[15], (L, D_CONV), 0.02),
        "conv_ln_g": gain(ks[16], (L, D_CONV)),
        "conv_ln_b": nrm(ks[17], (L, D_CONV), 0.02),
        "w_conv_out": nrm(ks[18], (L, D_CONV, D), D_CONV ** -0.5),
        "w_out": nrm(ks[19], (L, D, D), D ** -0.5),
        "g_moe": gain(ks[20], (L, D)),
        "w_router_group": nrm(ks[21], (L, D, N_GROUPS_MOE), D ** -0.5),
        "b_router_group": nrm(ks[22], (L, N_GROUPS_MOE), 0.01),
        "w_router_expert": nrm(ks[23], (L, D, N_EXPERTS), D ** -0.5),
        "b_router_expert": nrm(ks[24], (L, N_EXPERTS), 0.01),
        "w_exp_gate": nrm(ks[25], (L, N_EXPERTS, D, D_EXPERT), D ** -0.5),
        "w_exp_up": nrm(ks[26], (L, N_EXPERTS, D, D_EXPERT), D ** -0.5),
        "w_exp_down": nrm(ks[27], (L, N_EXPERTS, D_EXPERT, D), D_EXPERT ** -0.5),
        "g_ple": gain(ks[28], (L, D)),
        "w_ple_gate": nrm(ks[29], (L, D, D), D ** -0.5),
        "w_ple": nrm(ks[30], (L, D_PLE, D), D_PLE ** -0.5),
        "g_final": gain(ks[31], (D,)),
    }


def reference(x, p, g_mix, w_in, b_gate, ssm_a_re, ssm_a_im, ssm_log_dt, ssm_b_re, ssm_b_im,
              ssm_c_re, ssm_c_im, ssm_d, w_glu, conv_dw, conv_dw_b, conv_ln_g, conv_ln_b,
              w_conv_out, w_out, g_moe, w_router_group, b_router_group, w_router_expert,
              b_router_expert, w_exp_gate, w_exp_up, w_exp_down, g_ple, w_ple_gate, w_ple,
              g_final):
    for i in range(DEPTH):
        h = rmsnorm(x, g_mix[i])
        proj = h @ w_in[i]
        u_ssm = proj[..., :D_SSM]
        v_conv = proj[..., D_SSM:D_SSM + 2 * D_CONV]
        gates = proj[..., D_SSM + 2 * D_CONV:] + b_gate[i]
        gate_ssm, gate_conv = jnp.split(gates, 2, axis=-1)

        y = s5_ssm(u_ssm, ssm_a_re[i], ssm_a_im[i], ssm_log_dt[i], ssm_b_re[i], ssm_b_im[i],
                   ssm_c_re[i], ssm_c_im[i], ssm_d[i])
        z_val, z_gate = jnp.split(jax.nn.gelu(y) @ w_glu[i], 2, axis=-1)
        y_ssm = z_val * jax.nn.sigmoid(z_gate)

        y_conv = conformer_conv(v_conv, conv_dw[i], conv_dw_b[i], conv_ln_g[i], conv_ln_b[i],
                                w_conv_out[i])

        merged = jax.nn.sigmoid(gate_ssm) * y_ssm + jax.nn.sigmoid(gate_conv) * y_conv
        x = x + merged @ w_out[i]

        x = x + hierarchical_moe(rmsnorm(x, g_moe[i]), w_router_group[i], b_router_group[i],
                                 w_router_expert[i], b_router_expert[i], w_exp_gate[i],
                                 w_exp_up[i], w_exp_down[i])

        ple_gate = jax.nn.sigmoid(rmsnorm(x, g_ple[i]) @ w_ple_gate[i])
        x = x + ple_gate * (p[i] @ w_ple[i])
    return rmsnorm(x, g_final)
```

```python
import math
from contextlib import ExitStack

import numpy as np
import concourse.bass as bass
import concourse.mybir as mybir
from concourse.bass_utils import run_bass_kernel_spmd

F32 = mybir.dt.float32
BF16 = mybir.dt.bfloat16
AF = mybir.ActivationFunctionType
ALU = mybir.AluOpType
AX = mybir.AxisListType

COMPUTE = ("pe", "act", "dve", "pool")
ALLENG = ("pe", "act", "dve", "pool", "sp")
PI = math.pi


class Op:
    __slots__ = ("eng", "fn", "reads", "writes", "dma", "waits", "signal", "sigval", "deps",
                 "needed", "idx", "barrier", "bg")

    def __init__(self, eng, fn, reads, writes, dma):
        self.eng = eng
        self.fn = fn
        self.reads = tuple(reads)
        self.writes = tuple(writes)
        self.dma = dma
        self.waits = []
        self.signal = None
        self.sigval = None
        self.deps = ()
        self.needed = False
        self.barrier = False
        self.bg = False


class Sched:
    def __init__(self, nc, ring=8):
        self.nc = nc
        self.ops = []
        self.ring = ring

    def add(self, eng, fn, reads=(), writes=(), dma=False):
        op = Op(eng, fn, reads, writes, dma)
        op.idx = len(self.ops)
        self.ops.append(op)
        return op

    def pe(self, fn, reads=(), writes=()):
        return self.add("pe", fn, reads, writes)

    def act(self, fn, reads=(), writes=()):
        return self.add("act", fn, reads, writes)

    def dve(self, fn, reads=(), writes=()):
        return self.add("dve", fn, reads, writes)

    def pool(self, fn, reads=(), writes=()):
        return self.add("pool", fn, reads, writes)

    def dma(self, eng, fn, reads=(), writes=()):
        return self.add(eng, fn, reads, writes, dma=True)

    def dma_bg(self, eng, fn, writes=(), reads=()):
        op = self.add(eng, fn, reads, writes, dma=True)
        op.bg = True
        return op

    def barrier(self):
        for e in ALLENG:
            op = self.add(e, None)
            op.barrier = True

    def schedule(self, sems_compute, sems_ring):
        ops = self.ops
        last_w = {}
        last_w_bg = {}
        readers = {}
        last_on = {}
        pending_dma = []
        i = 0
        n = len(ops)
        while i < n:
            op = ops[i]
            if op.barrier:
                grp = []
                while i < n and ops[i].barrier:
                    grp.append(ops[i])
                    i += 1
                deps = list(last_on.values()) + list(pending_dma)
                for b in grp:
                    b.deps = tuple(sorted(set(deps)))
                for d in deps:
                    ops[d].needed = True
                pending_dma = []
                last_w = {}
                readers = {}
                continue
            deps = set()
            if op.bg:
                bdeps = set()
                for k in op.reads:
                    w = last_w.get(k)
                    if w is None:
                        w = last_w_bg.get(k)
                    if w is not None:
                        bdeps.add(w)
                for k in op.writes:
                    last_w_bg[k] = op.idx
                op.deps = tuple(sorted(bdeps))
                for d_ in op.deps:
                    ops[d_].needed = True
                i += 1
                continue
            for k in op.reads:
                w = last_w.get(k)
                if w is None:
                    w = last_w_bg.get(k)
                if w is not None:
                    deps.add(w)
            for k in op.writes:
                w = last_w.get(k)
                if w is not None:
                    deps.add(w)
                for r in readers.get(k, ()):
                    deps.add(r)
            deps.discard(op.idx)
            fdeps = []
            for d in deps:
                p = ops[d]
                if (not p.dma) and p.eng == op.eng and p.eng == "pe" and not op.dma:
                    continue
                fdeps.append(d)
            op.deps = tuple(sorted(fdeps))
            for d in op.deps:
                ops[d].needed = True
            for k in op.reads:
                readers.setdefault(k, []).append(op.idx)
            for k in op.writes:
                last_w[k] = op.idx
                readers[k] = []
            if op.dma:
                pending_dma.append(op.idx)
            else:
                last_on[op.eng] = op.idx
            i += 1
        cnt = {e: 0 for e in COMPUTE}
        ring_i = {e: 0 for e in ALLENG}
        ring_cnt = {}
        waited = {e: {} for e in ALLENG}
        for op in ops:
            waits = {}
            if op.dma:
                rn = op.eng + ("_bg" if op.bg else "")
                k = ring_i.get(rn, 0)
                ring_i[rn] = k + 1
                sem = sems_ring[rn][k % self.ring]
                prev = ring_cnt.get(sem, 0)
                if prev > 0:
                    waits[sem] = prev
                ring_cnt[sem] = prev + 16
                op.signal = (sem, 16)
                op.sigval = prev + 16
            for d in op.deps:
                p = ops[d]
                sem = p.signal[0]
                v = p.sigval
                if waits.get(sem, 0) < v:
                    waits[sem] = v
            wl = []
            for sem, v in waits.items():
                if waited[op.eng].get(sem, 0) >= v:
                    continue
                waited[op.eng][sem] = v
                wl.append((sem, v))
            op.waits = wl
            if (not op.dma) and op.needed and not op.barrier:
                cnt[op.eng] += 1
                op.signal = (sems_compute[op.eng], 1)
                op.sigval = cnt[op.eng]
        self.ring_cnt = ring_cnt

    def emit_engine(self, eng_name, eng):
        for op in self.ops:
            if op.eng != eng_name:
                continue
            for sem, v in op.waits:
                eng.wait_ge(sem, v)
            if op.fn is None:
                continue
            inst = op.fn(eng)
            if op.signal is not None:
                inst.then_inc(op.signal[0], op.signal[1])

    def final_waits(self, eng_name, eng):
        for sem, v in self.ring_cnt.items():
            if sem in self._ring_of[eng_name]:
                eng.wait_ge(sem, v)

    def run(self):
        nc = self.nc
        with ExitStack() as st:
            sems_compute = {e: st.enter_context(nc.semaphore(f"c_{e}")) for e in COMPUTE}
            dma_engs = sorted({op.eng + ("_bg" if op.bg else "") for op in self.ops if op.dma})
            sems_ring = {e: [st.enter_context(nc.semaphore(f"r_{e}_{i}")) for i in range(self.ring)]
                         for e in dma_engs}
            self._ring_of = {e: set(sems_ring.get(e, ())) | set(sems_ring.get(e + "_bg", ())) for e in ALLENG}
            self.schedule(sems_compute, sems_ring)
            block = st.enter_context(nc.Block())
            sched = self

            @block.sync
            def _(e):
                sched.emit_engine("sp", e)
                sched.final_waits("sp", e)

            @block.tensor
            def _(e):
                sched.emit_engine("pe", e)

            @block.scalar
            def _(e):
                sched.emit_engine("act", e)
                sched.final_waits("act", e)

            @block.vector
            def _(e):
                sched.emit_engine("dve", e)

            @block.gpsimd
            def _(e):
                sched.emit_engine("pool", e)
                sched.final_waits("pool", e)


T = 2048
D = 1024
NT = T // 128
NP_ = T // 512
EPS = 1e-6
SB_BASE = 16640
KB = 1024
NEXP = 32


def build_nc(stop_after=None, debug=False, n_exp=NEXP):
    nc = bass.Bass("TRN2", target_bir_lowering=False)
    S = Sched(nc, ring=16)
    dbg = {}

    def din(name, shape, dt=F32):
        return nc.dram_tensor(name, list(shape), dt, kind="ExternalInput").ap()

    x_d = din("x", [T, D])
    p_d = din("p", [T, 256])
    w_in_d = din("w_in", [D, 3584])
    w_glu_d = din("w_glu", [512, 2048])
    w_co_d = din("w_conv_out", [512, D])
    w_out_d = din("w_out", [D, D])
    w_pg_d = din("w_ple_gate", [D, D])
    w_ple_d = din("w_ple", [256, D])
    wg_d = din("w_exp_gate", [n_exp, D, 512])
    wu_d = din("w_exp_up", [n_exp, D, 512])
    wd_d = din("w_exp_down", [n_exp, 512, D])
    gcols_d = din("gcols", [128, 24])
    bgate_d = din("bgate", [128, 16])
    convw_d = din("convw", [128, 4, 31])
    convp_d = din("convp", [128, 12])
    ssmd_d = din("ssmd", [128, 4])
    ssmcol_d = din("ssmcol", [128, 48])
    bcol_re_d = din("bcol_re", [128, 16, 32])
    bcol_im_d = din("bcol_im", [128, 16, 32])
    ccol_re_d = din("ccol_re", [128, 16, 32])
    ccol_im_d = din("ccol_im", [128, 16, 32])
    wr_d = din("wr", [128, 8, 36])
    br_d = din("br", [36])
    gfin_d = din("gfin", [D])
    gmoe_d = din("gmoe", [D])
    out_d = nc.dram_tensor("out", [T, D], F32, kind="ExternalOutput").ap()

    def dbg_out(name, shape, dt=F32):
        t = nc.dram_tensor(name, list(shape), dt, kind="ExternalOutput").ap()
        dbg[name] = t
        return t

    def sbt(name, shape, dt, off):
        return nc.alloc_sbuf_tensor_at(name, list(shape), dt, offset=SB_BASE + off)

    R0 = 0
    R1 = 64 * KB
    R2 = 96 * KB
    R3 = 113 * KB
    R4 = 129 * KB
    R5 = 145 * KB
    R6 = 177 * KB
    CST = 193 * KB

    XRES = sbt("xres", [128, NT, D], F32, R0)
    hT = sbt("hT", [128, 8, T], BF16, R1)

    co = [CST]

    def calloc(name, shape, dt):
        nbytes = int(np.prod(shape[1:])) * (2 if dt == BF16 else 4)
        nbytes = (nbytes + 31) // 32 * 32
        t = sbt(name, shape, dt, co[0])
        co[0] += nbytes
        return t

    ident = calloc("ident", [128, 128], F32)
    identb = calloc("identb", [128, 128], BF16)
    onesf = calloc("onesf", [128, 128], F32)
    gcols = calloc("gcols_s", [128, 24], F32)
    bgate = calloc("bgate_s", [128, 16], F32)
    convw = calloc("convw_s", [128, 4, 31], F32)
    convp = calloc("convp_s", [128, 12], F32)
    ssmd = calloc("ssmd_s", [128, 4], F32)
    ss = calloc("ss", [128, 16], F32)
    rs = calloc("rs", [128, 16], F32)
    brb = calloc("brb", [128, 36], F32)
    assert co[0] <= 206 * KB, co[0]

    pst = [nc.alloc_psum_tensor(f"ps{i}", [128, 1024], F32) for i in range(4)]

    def bank(k):
        return pst[k // 2][:, (k % 2) * 512:(k % 2) * 512 + 512]

    bank_ctr = [0]

    def nbank():
        k = bank_ctr[0] % 8
        bank_ctr[0] += 1
        return k

    def DMA(q, out, in_, reads=(), writes=()):
        S.dma(q, lambda e: e.dma_start(out=out, in_=in_), reads, writes)

    def MM(out, lhsT, rhs, start, stop, reads, writes, tp=None):
        if tp is None:
            S.pe(lambda e: e.matmul(out, lhsT=lhsT, rhs=rhs, start=start, stop=stop), reads, writes)
        else:
            S.pe(lambda e: e.matmul(out, lhsT=lhsT, rhs=rhs, start=start, stop=stop, tile_position=tp),
                 reads, writes)

    def TR(out, in_, idn, reads, writes):
        S.pe(lambda e: e.transpose(out, in_, idn), reads, writes)

    def ACT(out, in_, func, reads, writes, bias=None, scale=None, accum_out=None):
        kw = {}
        if bias is not None:
            kw["bias"] = bias
        if scale is not None:
            kw["scale"] = scale
        if accum_out is not None:
            kw["accum_out"] = accum_out
        S.act(lambda e: e.activation(out=out, in_=in_, func=func, **kw), reads, writes)

    def TT(eng, out, in0, in1, op, reads, writes):
        S.add(eng, lambda e: e.tensor_tensor(out=out, in0=in0, in1=in1, op=op), reads, writes)

    def TS(eng, out, in0, s1, s2, op0, op1, reads, writes):
        if op1 is None:
            S.add(eng, lambda e: e.tensor_scalar(out=out, in0=in0, scalar1=s1, scalar2=None, op0=op0),
                  reads, writes)
        else:
            S.add(eng, lambda e: e.tensor_scalar(out=out, in0=in0, scalar1=s1, scalar2=s2, op0=op0, op1=op1),
                  reads, writes)

    def STT(out, in0, scalar, in1, op0, op1, reads, writes):
        S.dve(lambda e: e.scalar_tensor_tensor(out=out, in0=in0, scalar=scalar, in1=in1, op0=op0, op1=op1),
              reads, writes)

    def MEMSET(eng, ap, val, writes):
        S.add(eng, lambda e: e.memset(ap, val), (), writes)

    wgb_d = nc.dram_tensor("wgb_scr", [n_exp, D, 512], BF16).ap()
    wub_d = nc.dram_tensor("wub_scr", [n_exp, D, 512], BF16).ap()
    wdb_d = nc.dram_tensor("wdb_scr", [n_exp, 512, D], BF16).ap()
    bg_list = []
    for e_ in range(n_exp):
        for c2 in range(2):
            bg_list.append((wgb_d[e_, c2 * 512:(c2 + 1) * 512, :], wg_d[e_, c2 * 512:(c2 + 1) * 512, :], ("BGg", e_, c2)))
        for c2 in range(2):
            bg_list.append((wub_d[e_, c2 * 512:(c2 + 1) * 512, :], wu_d[e_, c2 * 512:(c2 + 1) * 512, :], ("BGu", e_, c2)))
        for c2 in range(2):
            bg_list.append((wdb_d[e_, c2 * 256:(c2 + 1) * 256, :], wd_d[e_, c2 * 256:(c2 + 1) * 256, :], ("BGd", e_, c2)))
    bg_pos = [0]
    globals_ = {}

    def emit_bg(n):
        if "emit_zero_fill" in globals_:
            globals_["emit_zero_fill"](1)
        for _ in range(n):
            if bg_pos[0] >= len(bg_list):
                return
            o_, i_, ky = bg_list[bg_pos[0]]
            bg_pos[0] += 1
            S.dma_bg("pool", lambda e, o_=o_, i_=i_: e.dma_start(out=o_, in_=i_), [ky])

    MEMSET("dve", onesf[:, :], 1.0, ["onesf0"])
    MEMSET("pool", ident[:, :], 0.0, ["ident0"])
    S.pool(lambda e: e.affine_select(out=ident[:, :], in_=onesf[:, :], pattern=[[-1, 128]],
                                     compare_op=ALU.is_equal, fill=0.0, base=0, channel_multiplier=1),
           ["onesf0", "ident0"], ["ident"])
    S.dve(lambda e: e.tensor_copy(out=identb[:, :], in_=ident[:, :]), ["ident"], ["identb"])
    TS("dve", onesf[:, :], onesf[:, :], 1.0 / 512.0, None, ALU.mult, None, ["onesf0", "ident"], ["onesf"])
    DMA("sp", gcols[:, :], gcols_d[:, :], (), ["gcols"])
    DMA("sp", bgate[:, :], bgate_d[:, :], (), ["bgate"])
    DMA("sp", convw[:, :, :], convw_d[:, :, :], (), ["convw"])
    DMA("sp", convp[:, :], convp_d[:, :], (), ["convp"])
    DMA("sp", ssmd[:, :], ssmd_d[:, :], (), ["ssmd"])
    DMA("sp", brb[:, :], br_d.partition_broadcast(128), (), ["brb"])
    emit_bg(8)

    def norm_T(gi, tmp_off):
        xn = [sbt(f"nt_xn{gi}_{k}", [128, D], F32, tmp_off + k * 4 * KB) for k in range(3)]
        junk = sbt(f"nt_junk{gi}", [128, D], BF16, tmp_off + 12 * KB)
        g_bc = gcols[:, gi * 8:gi * 8 + 8].unsqueeze(2).to_broadcast([128, 8, 128])
        for i in range(NT):
            ACT(junk[:, :], XRES[:, i, :], AF.Square, [("xres", i)], [("ss", i), "junk"], accum_out=ss[:, i:i + 1])
        SS_ALL = [("ss", i) for i in range(NT)]
        TS("dve", rs[:, :], ss[:, :], 1.0 / D, EPS, ALU.mult, ALU.add, SS_ALL, ["rs"])
        ACT(rs[:, :], rs[:, :], AF.Sqrt, ["rs"], ["rs"])
        S.dve(lambda e: e.reciprocal(out=rs[:, :], in_=rs[:, :]), ["rs"], ["rs"])
        for i in range(NT):
            k = i % 3
            kp = i % 2
            ACT(xn[k][:, :], XRES[:, i, :], AF.Copy, [("xres", i), "rs"], [("xn", k)], scale=rs[:, i:i + 1])
            pk = [("ps", 2 * kp), ("ps", 2 * kp + 1)]
            for c in range(8):
                TR(pst[kp][:, c * 128:(c + 1) * 128], xn[k][:, c * 128:(c + 1) * 128], ident[:, :],
                   [("xn", k), "ident"], pk)
            TT("dve", hT[:, :, i * 128:(i + 1) * 128], pst[kp][:, :].rearrange("p (c t) -> p c t", c=8), g_bc,
               ALU.mult, pk + ["gcols"], [("hT", i)])

    CAP = 256
    NSLOT = NEXP * CAP
    if debug:
        Xs = nc.dram_tensor("Xs_scr", [NSLOT + 2, D], BF16, kind="ExternalOutput").ap()
        Ys = nc.dram_tensor("Ys_scr", [NSLOT + 1, D], F32, kind="ExternalOutput").ap()
    else:
        Xs = nc.dram_tensor("Xs_scr", [NSLOT + 2, D], BF16).ap()
        Ys = nc.dram_tensor("Ys_scr", [NSLOT + 1, D], F32).ap()
    zsrc = nc.dram_tensor("zsrc_scr", [256, D], BF16).ap()
    zt = sbt("zt", [128, 2, D], BF16, R3)
    MEMSET("pool", zt[:, :, :], 0.0, ["zt"])
    DMA("pool", zsrc[:, :].rearrange("(p a) d -> p a d", p=128), zt[:, :, :], ["zt"], ["zsrc"])
    S.barrier()
    XS0_KEYS = [("BGx0", n_) for n_ in range(NSLOT // 256)]
    zf_pos = [0]

    def emit_zero_fill(n):
        for _ in range(n):
            n_ = zf_pos[0]
            if n_ >= NSLOT // 256:
                return
            zf_pos[0] += 1
            S.dma_bg("pool", lambda e, n_=n_: e.dma_start(out=Xs[n_ * 256:(n_ + 1) * 256, :], in_=zsrc[:, :]),
                     [("BGx0", n_)], ["BGz"])

    globals_["emit_zero_fill"] = emit_zero_fill
    so = [R4]

    def salloc(name, shape, dt=F32):
        nbytes = int(np.prod(shape[1:])) * (4 if dt == F32 else 2)
        nbytes = (nbytes + 31) // 32 * 32
        t = sbt(name, shape, dt, so[0])
        so[0] += nbytes
        return t

    scol = salloc("scol", [128, 48])
    DMA("sp", scol[:, :], ssmcol_d[:, :], (), ["scol"])
    are = scol[:, 0:16]
    aim = scol[:, 16:32]
    ldt = scol[:, 32:48]
    sv = {}
    for nm in ["dt", "mag", "th", "t0", "t1", "t2", "acc", "cs", "sn", "lr", "li", "den", "nr", "zr", "zi"]:
        sv[nm] = salloc("sv_" + nm, [128, 16])
    zlr = salloc("zlr", [128, 16, 11])
    zli = salloc("zli", [128, 16, 11])
    nzli = salloc("nzli", [128, 16, 11])
    K_ = "ssmp"

    def sACT(out, in_, func, **kw):
        ACT(out, in_, func, [K_, "scol"], [K_], **kw)

    def sTT(out, a, b, op):
        TT("dve", out, a, b, op, [K_, "scol"], [K_])

    def sTS(out, a, s1, s2, op0, op1):
        TS("dve", out, a, s1, s2, op0, op1, [K_, "scol"], [K_])

    sACT(sv["dt"][:, :], ldt, AF.Exp)
    sTT(sv["t0"][:, :], are, sv["dt"][:, :], ALU.mult)
    sACT(sv["mag"][:, :], sv["t0"][:, :], AF.Exp)
    sTT(sv["th"][:, :], aim, sv["dt"][:, :], ALU.mult)

    def range_reduce(out, shift):
        sTS(sv["t1"][:, :], sv["th"][:, :], float(shift), None, ALU.add, None)
        sTS(out, sv["t1"][:, :], 1.0, None, ALU.mult, None)
        for kk in range(1, 8):
            sTS(sv["t2"][:, :], sv["t1"][:, :], (2 * kk - 1) * PI, -2 * PI, ALU.is_gt, ALU.mult)
            sTT(out, out, sv["t2"][:, :], ALU.add)
        sTS(sv["t2"][:, :], sv["t1"][:, :], -PI, 2 * PI, ALU.is_lt, ALU.mult)
        sTT(out, out, sv["t2"][:, :], ALU.add)
        sTS(out, out, 3.1415925, -3.1415925, ALU.min, ALU.max)

    range_reduce(sv["acc"][:, :], 0.0)
    sACT(sv["sn"][:, :], sv["acc"][:, :], AF.Sin)
    range_reduce(sv["acc"][:, :], PI / 2)
    sACT(sv["cs"][:, :], sv["acc"][:, :], AF.Sin)
    sTT(sv["lr"][:, :], sv["mag"][:, :], sv["cs"][:, :], ALU.mult)
    sTT(sv["li"][:, :], sv["mag"][:, :], sv["sn"][:, :], ALU.mult)
    sTT(sv["t0"][:, :], are, are, ALU.mult)
    sTT(sv["t1"][:, :], aim, aim, ALU.mult)
    sTT(sv["den"][:, :], sv["t0"][:, :], sv["t1"][:, :], ALU.add)
    S.dve(lambda e: e.reciprocal(out=sv["den"][:, :], in_=sv["den"][:, :]), [K_], [K_])
    sTS(sv["nr"][:, :], sv["lr"][:, :], -1.0, None, ALU.add, None)
    sTT(sv["t0"][:, :], sv["nr"][:, :], are, ALU.mult)
    sTT(sv["t1"][:, :], sv["li"][:, :], aim, ALU.mult)
    sTT(sv["t0"][:, :], sv["t0"][:, :], sv["t1"][:, :], ALU.add)
    sTT(sv["zr"][:, :], sv["t0"][:, :], sv["den"][:, :], ALU.mult)
    sTT(sv["t0"][:, :], sv["li"][:, :], are, ALU.mult)
    sTT(sv["t1"][:, :], sv["nr"][:, :], aim, ALU.mult)
    sTT(sv["t0"][:, :], sv["t0"][:, :], sv["t1"][:, :], ALU.subtract)
    sTT(sv["zi"][:, :], sv["t0"][:, :], sv["den"][:, :], ALU.mult)
    sTS(zlr[:, :, 0], sv["cs"][:, :], 1.0, None, ALU.mult, None)
    sTS(zli[:, :, 0], sv["sn"][:, :], 1.0, None, ALU.mult, None)
    for l in range(10):
        sTT(sv["t0"][:, :], zlr[:, :, l], zlr[:, :, l], ALU.mult)
        sTT(sv["t1"][:, :], zli[:, :, l], zli[:, :, l], ALU.mult)
        sTT(zlr[:, :, l + 1], sv["t0"][:, :], sv["t1"][:, :], ALU.subtract)
        sTT(sv["t0"][:, :], zlr[:, :, l], zli[:, :, l], ALU.mult)
        sTS(zli[:, :, l + 1], sv["t0"][:, :], 2.0, None, ALU.mult, None)

    sTS(nzli[:, :, :], zli[:, :, :], -1.0, None, ALU.mult, None)
    LPr = salloc("LPr", [128, 16, 9])
    LPi = salloc("LPi", [128, 16, 9])
    sTS(LPr[:, :, 0], sv["lr"][:, :], 0.0, 1.0, ALU.mult, ALU.add)
    sTS(LPi[:, :, 0], sv["lr"][:, :], 0.0, None, ALU.mult, None)
    for m_ in range(8):
        sTT(sv["t0"][:, :], LPr[:, :, m_], sv["lr"][:, :], ALU.mult)
        sTT(sv["t1"][:, :], LPi[:, :, m_], sv["li"][:, :], ALU.mult)
        sTT(LPr[:, :, m_ + 1], sv["t0"][:, :], sv["t1"][:, :], ALU.subtract)
        sTT(sv["t0"][:, :], LPr[:, :, m_], sv["li"][:, :], ALU.mult)
        sTT(sv["t1"][:, :], LPi[:, :, m_], sv["lr"][:, :], ALU.mult)
        sTT(LPi[:, :, m_ + 1], sv["t0"][:, :], sv["t1"][:, :], ALU.add)
    R8 = salloc("R8", [128, 16])
    sTT(sv["t0"][:, :], sv["mag"][:, :], sv["mag"][:, :], ALU.mult)
    sTT(sv["t1"][:, :], sv["t0"][:, :], sv["t0"][:, :], ALU.mult)
    sTT(R8[:, :], sv["t1"][:, :], sv["t1"][:, :], ALU.mult)
    bcr = salloc("bcr", [128, 16, 32])
    bci = salloc("bci", [128, 16, 32])
    ccr = salloc("ccr", [128, 16, 32])
    cci = salloc("cci", [128, 16, 32])
    diagd = salloc("diagd", [128, 4, 128], BF16)
    assert so[0] <= R5, so[0]
    DMA("sp", bcr[:, :, :], bcol_re_d[:, :, :], (), ["bcr"])
    DMA("sp", bci[:, :, :], bcol_im_d[:, :, :], (), ["bci"])
    DMA("sp", ccr[:, :, :], ccol_re_d[:, :, :], (), ["ccr"])
    DMA("sp", cci[:, :, :], ccol_im_d[:, :, :], (), ["cci"])
    for i in range(NT):
        DMA("sp", XRES[:, i, :], x_d[i * 128:(i + 1) * 128, :], (), [("xres", i)])
    norm_T(0, R5)
    if debug:
        d_hT = dbg_out("d_hT", [128, 8, T], BF16)
        DMA("sp", d_hT[:, :, :], hT[:, :, :], [("hT", i) for i in range(NT)], ())
    S.barrier()
    HT_ALL = ["hT_all"]

    uT = sbt("uT", [128, 4, T], BF16, R2)
    wbuf = [sbt(f"wbuf{k}", [128, 8, 512], BF16, R6 + k * 8 * KB) for k in range(2)]
    win_v = w_in_d.rearrange("(c p) n -> p c n", p=128)

    def load_win_block(blk, k):
        for c2 in range(2):
            DMA("pool", wbuf[k][:, c2 * 4:(c2 + 1) * 4, :], win_v[:, c2 * 4:(c2 + 1) * 4, blk * 512:(blk + 1) * 512],
                (), [("wbuf", k, c2)])

    load_win_block(0, 0)
    for m in range(4):
        emit_bg(4)
        for n in range(NP_):
            b = nbank()
            for c in range(8):
                MM(bank(b), wbuf[0][:, c, m * 128:(m + 1) * 128], hT[:, c, n * 512:(n + 1) * 512], c == 0, c == 7,
                   [("wbuf", 0, c // 4)], [("ps", b)])
            ACT(uT[:, m, n * 512:(n + 1) * 512], bank(b), AF.Copy, [("ps", b)], [("uT", m)])
    if debug:
        d_uT = dbg_out("d_uT", [128, 4, T], BF16)
        DMA("sp", d_uT[:, :, :], uT[:, :, :], [("uT", m) for m in range(4)], ())
    if stop_after == "B":
        S.run()
        return nc, dbg

    gT = sbt("gT", [128, 4, T], BF16, R3)
    czr = sbt("czr", [128, 16, 32], F32, R0 + 56 * KB)
    czi = sbt("czi", [128, 16, 32], F32, R0 + 58 * KB)
    czrb = sbt("czrb", [128, 16, 32], BF16, R0 + 60 * KB)
    nczib = sbt("nczib", [128, 16, 32], BF16, R0 + 61 * KB)
    xt_ = [sbt(f"xtmp{k}", [128, 16, 32], F32, R0 + k * 2 * KB) for k in range(4)]

    def b32(ap2):
        return ap2.unsqueeze(2).to_broadcast([128, 16, 32])

    CK = "cprep"
    TT("dve", xt_[0][:, :, :], ccr[:, :, :], b32(sv["zr"][:, :]), ALU.mult, ["ccr", K_], [CK])
    TT("dve", xt_[1][:, :, :], cci[:, :, :], b32(sv["zi"][:, :]), ALU.mult, ["cci", K_, CK], [CK])
    TT("dve", czr[:, :, :], xt_[0][:, :, :], xt_[1][:, :, :], ALU.subtract, [CK], [CK])
    TT("dve", xt_[0][:, :, :], ccr[:, :, :], b32(sv["zi"][:, :]), ALU.mult, [CK], [CK])
    TT("dve", xt_[1][:, :, :], cci[:, :, :], b32(sv["zr"][:, :]), ALU.mult, [CK], [CK])
    TT("dve", czi[:, :, :], xt_[0][:, :, :], xt_[1][:, :, :], ALU.add, [CK], [CK])
    S.dve(lambda e: e.tensor_copy(out=czrb[:, :, :], in_=czr[:, :, :]), [CK], [CK])
    TS("dve", nczib[:, :, :], czi[:, :, :], -1.0, None, ALU.mult, None, [CK], [CK])
    for ch in range(4):
        TS("dve", diagd[:, ch, :], identb[:, :], ssmd[:, ch:ch + 1], None, ALU.mult, None,
           ["identb", "ssmd"], ["diagd"])
    S.barrier()
    WW = [[sbt(f"ww{k}_{ri}", [128, 16, 128], BF16, R6 + k * 8 * KB + ri * 4 * KB) for ri in range(2)]
          for k in range(2)]
    for k in range(2):
        for ri in range(2):
            MEMSET("pool", WW[k][ri][:, :, :], 0.0, [("ww", k, ri)])
    WE = sbt("WE", [128, 4, 8, 2, 128], BF16, R5)
    KT = sbt("KT", [128, 4, 8, 128], BF16, R5 + 16 * KB)
    Spr = calloc("Spr", [128, 16, 256], BF16)
    Spi = sbt("Spi", [128, 16, 256], BF16, R5 + 24 * KB)
    assert co[0] <= 212736, co[0]

    def wide_build(k, src_r, src_i, lr_b, li_b, neg_im, rkeys):
        wk = [("ww", k, 0), ("ww", k, 1)]
        TT("dve", xt_[0][:, :, :], src_r, lr_b, ALU.mult, rkeys + [("xt", 0)], [("xt", 0)])
        TT("pool", xt_[1][:, :, :], src_i, li_b, ALU.mult, rkeys + [("xt", 1)], [("xt", 1)])
        TT("dve", xt_[2][:, :, :], src_r, li_b, ALU.mult, rkeys + [("xt", 2)], [("xt", 2)])
        TT("pool", xt_[3][:, :, :], src_i, lr_b, ALU.mult, rkeys + [("xt", 3)], [("xt", 3)])
        for j in range(4):
            TT("dve", WW[k][0][:, j::4, 32 * j:32 * j + 32], xt_[0][:, j::4, :], xt_[1][:, j::4, :], ALU.subtract,
               [("xt", 0), ("xt", 1)], [wk[0]])
            if neg_im:
                STT(WW[k][1][:, j::4, 32 * j:32 * j + 32], xt_[2][:, j::4, :], -1.0, xt_[3][:, j::4, :],
                    ALU.mult, ALU.subtract, [("xt", 2), ("xt", 3)], [wk[1]])
            else:
                TT("dve", WW[k][1][:, j::4, 32 * j:32 * j + 32], xt_[2][:, j::4, :], xt_[3][:, j::4, :], ALU.add,
                   [("xt", 2), ("xt", 3)], [wk[1]])

    for m_ in range(8):
        k = m_ % 2
        emit_bg(4)
        wide_build(k, bcr[:, :, :], bci[:, :, :], b32(LPr[:, :, m_]), b32(LPi[:, :, m_]), False,
                   ["bcr", "bci", K_])
        wk = [("ww", k, 0), ("ww", k, 1)]
        bK, bWr, bWi = nbank(), nbank(), nbank()
        for ch in range(4):
            for j in range(4):
                p = 4 * ch + j
                o = ch * 128 + 32 * j
                MM(bank(bK)[:, o:o + 32], WW[k][0][:, p, :], czrb[:, p, :], True, False, [wk[0], CK], [("ps", bK)])
                MM(bank(bK)[:, o:o + 32], WW[k][1][:, p, :], nczib[:, p, :], False, True, [wk[1], CK], [("ps", bK)])
        for ri, bW in ((0, bWr), (1, bWi)):
            for ch in range(4):
                for j in range(4):
                    p = 4 * ch + j
                    MM(bank(bW)[:, ch * 128:(ch + 1) * 128], WW[k][ri][:, p, :], identb[:, :], j == 0, j == 3,
                       [wk[ri], "identb"], [("ps", bW)])
        S.act(lambda e, m_=m_, bK=bK: e.activation(out=KT[:, :, m_, :],
                                                   in_=bank(bK).rearrange("p (c n) -> p c n", c=4), func=AF.Copy),
              [("ps", bK)], [("KT", m_)])
        for ri, bW in ((0, bWr), (1, bWi)):
            S.dve(lambda e, m_=m_, ri=ri, bW=bW: e.tensor_copy(out=WE[:, :, 7 - m_, ri, :],
                                                              in_=bank(bW).rearrange("p (c n) -> p c n", c=4)),
                  [("ps", bW)], [("WE", 7 - m_, ri)])
    TT("dve", KT[:, :, 0, :], KT[:, :, 0, :], diagd[:, :, :], ALU.add, [("KT", 0), "diagd"], [("KT", 0)])
    S.barrier()

    tc_ = sbt("l2c", [128, 8, 256], F32, R0)
    td_ = sbt("l2d", [128, 8, 256], F32, R0 + 8 * KB)
    Eb = sbt("l2E", [128, 8, 512], F32, R0 + 16 * KB)
    q1 = sbt("l2q1", [128, 8, 256], F32, R0 + 32 * KB)
    q2 = sbt("l2q2", [128, 8, 256], F32, R0 + 40 * KB)
    Rt = sbt("l2R", [128, 8, 256], F32, R0 + 48 * KB)
    MEMSET("pool", Spr[:, :, 0:1], 0.0, ["Spr0"])
    MEMSET("pool", Spi[:, :, 0:1], 0.0, ["Spi0"])
    for hb_ in range(2):
        ps_ = slice(hb_ * 8, hb_ * 8 + 8)
        LK = ("l2", hb_)
        emit_bg(12)
        for pp in range(8):
            p = hb_ * 8 + pp
            ch, j = p // 4, p % 4
            bE = 4 + (p % 4)
            for ri in range(2):
                for kk in range(8):
                    MM(bank(bE)[:, ri * 256:(ri + 1) * 256], WE[32 * j:32 * j + 32, ch, kk, ri, :],
                       uT[32 * j:32 * j + 32, ch, kk:T:8], kk == 0, kk == 7, [("WE", kk, ri)], [("ps", bE)],
                       tp=(32 * j, 0))
            ACT(Eb[:, pp, :], bank(bE), AF.Copy, [("ps", bE)], [("E", pp), "Er", "Ei"])
        EK = [("E", pp) for pp in range(8)]
        TK = "l2tab"
        MEMSET("dve", tc_[:, :, 0:1], 1.0, [TK])
        MEMSET("dve", td_[:, :, 0:1], 0.0, [TK])
        for l in range(8):
            n_ = 1 << l
            zr_b = zlr[:, ps_, 3 + l].unsqueeze(2).to_broadcast([128, 8, n_])
            zi_b = zli[:, ps_, 3 + l].unsqueeze(2).to_broadcast([128, 8, n_])
            TT("dve", q1[:, :, 0:n_], td_[:, :, 0:n_], zi_b, ALU.mult, [TK, "q1"], ["q1"])
            TT("dve", tc_[:, :, n_:2 * n_], tc_[:, :, 0:n_], zr_b, ALU.mult, [TK], [TK])
            TT("dve", tc_[:, :, n_:2 * n_], tc_[:, :, n_:2 * n_], q1[:, :, 0:n_], ALU.subtract, [TK, "q1"], [TK])
            TT("dve", q1[:, :, 0:n_], td_[:, :, 0:n_], zr_b, ALU.mult, [TK, "q1"], ["q1"])
            TT("dve", td_[:, :, n_:2 * n_], tc_[:, :, 0:n_], zi_b, ALU.mult, [TK], [TK])
            TT("dve", td_[:, :, n_:2 * n_], td_[:, :, n_:2 * n_], q1[:, :, 0:n_], ALU.add, [TK, "q1"], [TK])
        S.pool(lambda e, ps_=ps_: e.tensor_copy(out=Rt[:, :, :], in_=R8[:, ps_].unsqueeze(2).to_broadcast([128, 8, 256])),
               [K_, "Rt"], ["Rt"])
        MEMSET("pool", Rt[:, :, 0:1], 0.0, ["Rt"])
        Er = Eb[:, :, 0:256]
        Ei = Eb[:, :, 256:512]
        TT("dve", q1[:, :, :], tc_[:, :, :], Er, ALU.mult, [TK, "q1"] + EK, ["q1"])
        TT("pool", q2[:, :, :], td_[:, :, :], Ei, ALU.mult, [TK, "q2"] + EK, ["q2"])
        TT("dve", q1[:, :, :], q1[:, :, :], q2[:, :, :], ALU.add, ["q1", "q2"], ["q1"])
        TT("pool", q2[:, :, :], tc_[:, :, :], Ei, ALU.mult, [TK, "q2"] + EK, ["q2"])
        TT("dve", Er, td_[:, :, :], Er, ALU.mult, [TK] + EK, ["Er"])
        TT("dve", q2[:, :, :], q2[:, :, :], Er, ALU.subtract, ["q2", "Er"], ["q2"])
        q1f = q1[:, :, :].rearrange("p a s -> p (a s)")
        q2f = q2[:, :, :].rearrange("p a s -> p (a s)")
        Rtf = Rt[:, :, :].rearrange("p a s -> p (a s)")
        S.dve(lambda e, q1f=q1f, Rtf=Rtf: e.tensor_tensor_scan(out=q1f, data0=Rtf, data1=q1f, initial=0.0,
                                                               op0=ALU.mult, op1=ALU.add), ["q1", "Rt"], ["q1"])
        S.dve(lambda e, q2f=q2f, Rtf=Rtf: e.tensor_tensor_scan(out=q2f, data0=Rtf, data1=q2f, initial=0.0,
                                                               op0=ALU.mult, op1=ALU.add), ["q2", "Rt"], ["q2"])
        TT("pool", Er, tc_[:, :, :], q1[:, :, :], ALU.mult, [TK, "q1", "Er"], ["Er"])
        TT("dve", Ei, td_[:, :, :], q2[:, :, :], ALU.mult, [TK, "q2"] + EK, ["Ei"])
        TT("dve", Spr[:, ps_, 1:256], Eb[:, :, 0:255], Eb[:, :, 256:511], ALU.subtract, ["Er", "Ei"], [("Spr", hb_)])
        TT("pool", Er, tc_[:, :, :], q2[:, :, :], ALU.mult, [TK, "q2", "Er", ("Spr", hb_)], ["Er"])
        TT("dve", Ei, td_[:, :, :], q1[:, :, :], ALU.mult, [TK, "q1", "Ei", ("Spr", hb_)], ["Ei"])
        TT("dve", Spi[:, ps_, 1:256], Eb[:, :, 0:255], Eb[:, :, 256:511], ALU.add, ["Er", "Ei"], [("Spi", hb_)])
    S.barrier()

    for i_ in range(8):
        k = i_ % 2
        emit_bg(3)
        wide_build(k, czr[:, :, :], czi[:, :, :], b32(LPr[:, :, i_ + 1]), b32(LPi[:, :, i_ + 1]), True, [])
        wk = [("ww", k, 0), ("ww", k, 1)]
        for ch in range(4):
            b = nbank()
            for kk in range(i_ + 1):
                MM(bank(b)[:, 0:256], KT[:, ch, i_ - kk, :], uT[:, ch, kk:T:8], kk == 0, False, [], [("ps", b)])
            for j in range(4):
                p = 4 * ch + j
                MM(bank(b)[:, 0:256], WW[k][0][:, p, :], Spr[:, p, :], False, False, [wk[0]], [("ps", b)])
                MM(bank(b)[:, 0:256], WW[k][1][:, p, :], Spi[:, p, :], False, j == 3, [wk[1]], [("ps", b)])
            ACT(gT[:, ch, i_:T:8], bank(b)[:, 0:256], AF.Gelu, [("ps", b)], [("gT", ch)])
    if debug:
        d_gT = dbg_out("d_gT", [128, 4, T], BF16)
        DMA("sp", d_gT[:, :, :], gT[:, :, :], [("gT", m) for m in range(4)], ())
    S.barrier()
    if stop_after == "C":
        S.run()
        return nc, dbg

    zT = sbt("zT", [128, 4, T + 32], BF16, R2)
    HO = 32
    cT = sbt("cT", [128, 4, T], BF16, R4)
    zc = sbt("zc", [128, 4, T], F32, R5)
    dgm = [sbt(f"dgm{k}", [128, 31, 128], BF16, R0 + k * 8 * KB) for k in range(2)]
    sg_t = [sbt(f"sgt{k}", [128, 512], F32, R0 + 16 * KB + k * 2 * KB) for k in range(2)]
    lnm = sbt("lnm", [128, 512], F32, R0 + 20 * KB)
    lnr = sbt("lnr", [128, 512], F32, R0 + 22 * KB)
    lnt = [sbt(f"lnt{k}", [128, 512], F32, R0 + 24 * KB + k * 2 * KB) for k in range(2)]
    sqt = [sbt(f"sqt{k}", [128, 512], F32, R0 + 28 * KB + k * 2 * KB) for k in range(2)]
    load_win_block(1, 1)
    load_win_block(2, 0)
    MEMSET("pool", zT[:, :, 0:HO], 0.0, [("zT", m) for m in range(4)])
    for m in range(4):
        emit_bg(4)
        for n in range(NP_):
            bv, bg = nbank(), nbank()
            for c in range(8):
                MM(bank(bv), wbuf[1][:, c, m * 128:(m + 1) * 128], hT[:, c, n * 512:(n + 1) * 512], c == 0, c == 7,
                   [("wbuf", 1, c // 4)], [("ps", bv)])
            for c in range(8):
                MM(bank(bg), wbuf[0][:, c, m * 128:(m + 1) * 128], hT[:, c, n * 512:(n + 1) * 512], c == 0, c == 7,
                   [("wbuf", 0, c // 4)], [("ps", bg)])
            kk = (m * NP_ + n) % 2
            ACT(sg_t[kk][:, :], bank(bg), AF.Sigmoid, [("ps", bg)], [("sgt", kk)])
            TT("dve", zT[:, m, HO + n * 512:HO + (n + 1) * 512], bank(bv), sg_t[kk][:, :], ALU.mult,
               [("ps", bv), ("sgt", kk)], [("zT", m)])
    if debug:
        d_zT = dbg_out("d_zT", [128, 4, T + 32], BF16)
        DMA("sp", d_zT[:, :, :], zT[:, :, :], [("zT", m) for m in range(4)], ())
    for m in range(4):
        k = m % 2
        emit_bg(6)
        for tp_ in range(31):
            TS("dve", dgm[k][:, tp_, :], identb[:, :], convw[:, m, tp_:tp_ + 1], None, ALU.mult, None,
               ["identb", "convw"], [("dgm", k)])
        for n in range(NP_):
            b = nbank()
            for tp_ in range(31):
                s0 = HO + n * 512 + tp_ - 30
                MM(bank(b), dgm[k][:, tp_, :], zT[:, m, s0:s0 + 512], tp_ == 0, tp_ == 30,
                   [("dgm", k), ("zT", m)], [("ps", b)])
            ACT(zc[:, m, n * 512:(n + 1) * 512], bank(b), AF.Identity, [("ps", b), "convp"], [("zc", m, n)],
                bias=convp[:, m:m + 1])
    if debug:
        d_zc = dbg_out("d_zc", [128, 4, T], F32)
        DMA("sp", d_zc[:, :, :], zc[:, :, :], [("zc", m, n) for m in range(4) for n in range(NP_)], ())
    for n in range(NP_):
        sl = slice(n * 512, (n + 1) * 512)
        emit_bg(2)
        bm, bq = nbank(), nbank()
        for m in range(4):
            MM(bank(bm), onesf[:, :], zc[:, m, sl], m == 0, m == 3, ["onesf", ("zc", m, n)], [("ps", bm)])
        for m in range(4):
            kk = m % 2
            ACT(sqt[kk][:, :], zc[:, m, sl], AF.Square, [("zc", m, n)], [("sqt", kk)])
            MM(bank(bq), onesf[:, :], sqt[kk][:, :], m == 0, m == 3, ["onesf", ("sqt", kk)], [("ps", bq)])
        ACT(lnm[:, :], bank(bm), AF.Copy, [("ps", bm)], ["lnm"])
        TT("dve", lnr[:, :], lnm[:, :], lnm[:, :], ALU.mult, ["lnm"], ["lnr"])
        STT(lnr[:, :], bank(bq), EPS, lnr[:, :], ALU.add, ALU.subtract, [("ps", bq), "lnr"], ["lnr"])
        ACT(lnr[:, :], lnr[:, :], AF.Sqrt, ["lnr"], ["lnr"])
        S.dve(lambda e: e.reciprocal(out=lnr[:, :], in_=lnr[:, :]), ["lnr"], ["lnr"])
        for m in range(4):
            kk = m % 2
            TT("dve", lnt[kk][:, :], zc[:, m, sl], lnm[:, :], ALU.subtract, [("zc", m, n), "lnm"], [("lnt", kk)])
            TT("dve", lnt[kk][:, :], lnt[kk][:, :], lnr[:, :], ALU.mult, [("lnt", kk), "lnr"], [("lnt", kk)])
            ACT(cT[:, m, sl], lnt[kk][:, :], AF.Silu, [("lnt", kk), "convp"], [("cT", m)],
                scale=convp[:, 4 + m:5 + m], bias=convp[:, 8 + m:9 + m])
    if debug:
        d_cT = dbg_out("d_cT", [128, 4, T], BF16)
        DMA("sp", d_cT[:, :, :], cT[:, :, :], [("cT", m) for m in range(4)], ())
    S.barrier()
    if stop_after == "D":
        S.run()
        return nc, dbg

    mT = sbt("mT", [128, 8, T], BF16, R5)
    wE = []
    for k in range(2):
        o = R6 + k * 8 * KB
        wE.append(dict(
            gv=sbt(f"wE_gv{k}", [128, 4, 128], BF16, o),
            gg=sbt(f"wE_gg{k}", [128, 4, 128], BF16, o + 1 * KB),
            co=sbt(f"wE_co{k}", [128, 4, 128], BF16, o + 2 * KB),
            gs=sbt(f"wE_gs{k}", [128, 8, 128], BF16, o + 3 * KB),
            gc=sbt(f"wE_gc{k}", [128, 8, 128], BF16, o + 5 * KB)))
    wglu_v = w_glu_d.rearrange("(c p) n -> p c n", p=128)
    wco_v = w_co_d.rearrange("(c p) n -> p c n", p=128)
    et = [sbt(f"et{k}", [128, 512], F32, R0 + k * 2 * KB) for k in range(6)]

    def load_wE(fc):
        k = fc % 2
        w = wE[k]
        ky = ("wE", k)
        DMA("pool", w["gv"][:, :, :], wglu_v[:, :, fc * 128:(fc + 1) * 128], (), [(ky, "gv")])
        DMA("pool", w["gg"][:, :, :], wglu_v[:, :, 1024 + fc * 128:1024 + (fc + 1) * 128], (), [(ky, "gg")])
        DMA("pool", w["co"][:, :, :], wco_v[:, :, fc * 128:(fc + 1) * 128], (), [(ky, "co")])
        DMA("pool", w["gs"][:, :, :], win_v[:, :, 1536 + fc * 128:1536 + (fc + 1) * 128], (), [(ky, "gs")])
        DMA("pool", w["gc"][:, :, :], win_v[:, :, 2560 + fc * 128:2560 + (fc + 1) * 128], (), [(ky, "gc")])

    load_wE(0)
    wout = sbt("wout", [128, 8, D], BF16, R2)
    wout_v = w_out_d.rearrange("(c p) n -> p c n", p=128)
    for c2 in range(4):
        DMA("pool", wout[:, c2 * 2:(c2 + 1) * 2, :], wout_v[:, c2 * 2:(c2 + 1) * 2, :], (), [("wout", c2)])
    for i in range(3, NT):
        DMA("sp", XRES[:, i, :], x_d[i * 128:(i + 1) * 128, :], (), [("xres", i)])
    for fc in range(8):
        emit_bg(4)
        if fc + 1 < 8:
            load_wE(fc + 1)
        k = fc % 2
        w = wE[k]
        ky = ("wE", k)
        for n in range(NP_):
            sl = slice(n * 512, (n + 1) * 512)
            bzv, bzg, byc, bgs, bgc = nbank(), nbank(), nbank(), nbank(), nbank()
            for c in range(4):
                MM(bank(bzv), w["gv"][:, c, :], gT[:, c, sl], c == 0, c == 3, [(ky, "gv")], [("ps", bzv)])
            for c in range(4):
                MM(bank(bzg), w["gg"][:, c, :], gT[:, c, sl], c == 0, c == 3, [(ky, "gg")], [("ps", bzg)])
            for c in range(4):
                MM(bank(byc), w["co"][:, c, :], cT[:, c, sl], c == 0, c == 3, [(ky, "co")], [("ps", byc)])
            for c in range(8):
                MM(bank(bgs), w["gs"][:, c, :], hT[:, c, sl], c == 0, c == 7, [(ky, "gs")], [("ps", bgs)])
            for c in range(8):
                MM(bank(bgc), w["gc"][:, c, :], hT[:, c, sl], c == 0, c == 7, [(ky, "gc")], [("ps", bgc)])
            ACT(et[0][:, :], bank(bzg), AF.Sigmoid, [("ps", bzg)], [("et", 0)])
            ACT(et[1][:, :], bank(bgs), AF.Sigmoid, [("ps", bgs), "bgate"], [("et", 1)], bias=bgate[:, fc:fc + 1])
            ACT(et[2][:, :], bank(bgc), AF.Sigmoid, [("ps", bgc), "bgate"], [("et", 2)],
                bias=bgate[:, 8 + fc:9 + fc])
            TT("dve", et[3][:, :], bank(bzv), et[0][:, :], ALU.mult, [("ps", bzv), ("et", 0)], [("et", 3)])
            TT("pool", et[3][:, :], et[3][:, :], et[1][:, :], ALU.mult, [("et", 3), ("et", 1)], [("et", 3)])
            TT("dve", et[4][:, :], bank(byc), et[2][:, :], ALU.mult, [("ps", byc), ("et", 2)], [("et", 4)])
            TT("pool", mT[:, fc, sl], et[3][:, :], et[4][:, :], ALU.add, [("et", 3), ("et", 4)], [("mT", fc)])
    if debug:
        d_mT = dbg_out("d_mT", [128, 8, T], BF16)
        DMA("sp", d_mT[:, :, :], mT[:, :, :], [("mT", m) for m in range(8)], ())
    S.barrier()
    if stop_after == "E":
        S.run()
        return nc, dbg

    for i in range(3):
        DMA("sp", XRES[:, i, :], x_d[i * 128:(i + 1) * 128, :], (), [("xres", i)])
    emit_bg(1000)
    for i in range(NT):
        for hf in range(2):
            b = nbank()
            for c in range(8):
                MM(bank(b), mT[:, c, i * 128:(i + 1) * 128], wout[:, c, hf * 512:(hf + 1) * 512], c == 0, c == 7,
                   [("wout", c // 2)], [("ps", b)])
            TT("dve", XRES[:, i, hf * 512:(hf + 1) * 512], bank(b), XRES[:, i, hf * 512:(hf + 1) * 512], ALU.add,
               [("ps", b), ("xres", i)], [("xres", i)])
    if debug:
        d_x1 = dbg_out("d_x1", [128, NT, D], F32)
        for i in range(NT):
            DMA("sp", d_x1[:, i, :], XRES[:, i, :], [("xres", i)], ())
    S.barrier()
    if stop_after == "F":
        S.run()
        return nc, dbg

    I32 = mybir.dt.int32
    hb = sbt("hb", [128, NT, D], BF16, R1)
    wr = sbt("wr_s", [128, 8, 36], F32, R5 + 26 * KB)
    DMA("sp", wr[:, :, :], wr_d[:, :, :], (), ["wr"])
    gmb = sbt("gmb", [128, D], F32, R5 + 28 * KB)
    DMA("sp", gmb[:, :], gmoe_d.partition_broadcast(128), (), ["gmb"])
    w12 = calloc("w12", [128, 2, NT], F32)
    sidx = calloc("sidx", [128, NT, 2], I32)
    gidx = calloc("gidx", [128, NT, 2], I32)
    ltri = calloc("ltri", [128, 128], BF16)
    onesb = calloc("onesb", [128, 128], BF16)
    assert co[0] <= 207 * KB, co[0]
    MEMSET("pool", onesb[:, :], 1.0, ["onesb"])
    S.pool(lambda e: e.affine_select(out=ltri[:, :], in_=onesb[:, :], pattern=[[1, 128]],
                                     compare_op=ALU.is_gt, fill=0.0, base=0, channel_multiplier=-1),
           ["onesb"], ["ltri"])

    EW = 24 * KB
    ewg = [sbt(f"ewg{k}", [128, 8, 512], BF16, R2 + k * EW) for k in range(2)]
    ewu = [sbt(f"ewu{k}", [128, 8, 512], BF16, R2 + k * EW + 8 * KB) for k in range(2)]
    ewd = [sbt(f"ewd{k}", [128, 4, D], BF16, R2 + k * EW + 16 * KB) for k in range(2)]
    assert R2 + 2 * EW <= R5
    ewg.append(sbt("ewg2", [128, 8, 512], BF16, R1 + 16 * KB))
    ewu.append(sbt("ewu2", [128, 8, 512], BF16, R1 + 24 * KB))
    ewd.append(sbt("ewd2", [128, 4, D], BF16, R5 + 24 * KB))
    NWB = 3

    def load_expert(e_):
        k = e_ % NWB
        g_v = wgb_d[e_].rearrange("(c p) n -> p c n", p=128)
        u_v = wub_d[e_].rearrange("(c p) n -> p c n", p=128)
        d_v = wdb_d[e_].rearrange("(c p) n -> p c n", p=128)
        for c2 in range(2):
            DMA("sp", ewg[k][:, c2 * 4:(c2 + 1) * 4, :], g_v[:, c2 * 4:(c2 + 1) * 4, :], [("BGg", e_, c2)],
                [("ewg", k, c2)])
        for c2 in range(2):
            DMA("sp", ewu[k][:, c2 * 4:(c2 + 1) * 4, :], u_v[:, c2 * 4:(c2 + 1) * 4, :], [("BGu", e_, c2)],
                [("ewu", k, c2)])
        for c2 in range(2):
            DMA("sp", ewd[k][:, c2 * 2:(c2 + 1) * 2, :], d_v[:, c2 * 2:(c2 + 1) * 2, :], [("BGd", e_, c2)],
                [("ewd", k, c2)])

    load_expert(0)
    if n_exp > 1:
        load_expert(1)

    xn = [sbt(f"g_xn{k}", [128, D], F32, R6 + k * 4 * KB) for k in range(2)]
    h32 = [sbt(f"g_h32{k}", [128, 8, 128], F32, R6 + 8 * KB + k * 4 * KB) for k in range(2)]
    g_bc = gcols[:, 8:16].unsqueeze(2).to_broadcast([128, 8, 128])
    LB = [4, 5]
    for i in range(NT):
        ACT(hb[:, i, :], XRES[:, i, :], AF.Square, [("xres", i)], [("ss", i), ("hb", i)], accum_out=ss[:, i:i + 1])
    TS("dve", rs[:, :], ss[:, :], 1.0 / D, EPS, ALU.mult, ALU.add, [("ss", i) for i in range(NT)], ["rs"])
    ACT(rs[:, :], rs[:, :], AF.Sqrt, ["rs"], ["rs"])
    S.dve(lambda e: e.reciprocal(out=rs[:, :], in_=rs[:, :]), ["rs"], ["rs"])
    for i in range(NT):
        k = i % 2
        ACT(xn[k][:, :], XRES[:, i, :], AF.Copy, [("xres", i), "rs"], [("xn", k)], scale=rs[:, i:i + 1])
        TT("pool", hb[:, i, :], xn[k][:, :], gmb[:, :], ALU.mult, [("xn", k), "gmb"], [("hb", i)])
        pk = [("ps", 2 * k), ("ps", 2 * k + 1)]
        for c in range(8):
            TR(pst[k][:, c * 128:(c + 1) * 128], xn[k][:, c * 128:(c + 1) * 128], ident[:, :],
               [("xn", k), "ident"], pk)
        TT("dve", h32[k][:, :, :], pst[k][:, :].rearrange("p (c t) -> p c t", c=8), g_bc,
           ALU.mult, pk + ["gcols"], [("h32", k)])
        lb = LB[i // 8]
        col = (i % 8) * 36
        for c in range(8):
            MM(bank(lb)[:, col:col + 36], h32[k][:, c, :], wr[:, c, :], c == 0, c == 7, [("h32", k), "wr"],
               [("ps", lb)])

    ro = [R5]

    def ralloc(name, shape, dt=F32):
        nbytes = int(np.prod(shape[1:])) * (4 if dt in (F32, I32) else 2)
        nbytes = (nbytes + 31) // 32 * 32
        t = sbt(name, shape, dt, ro[0])
        ro[0] += nbytes
        return t

    lg = ralloc("r_lg", [128, NT, 36])
    gm = ralloc("r_gm", [128, NT])
    ohg = ralloc("r_ohg", [128, NT, 4])
    exg = ralloc("r_exg", [128, NT, 4])
    se = ralloc("r_se", [128, NT])
    pg = ralloc("r_pg", [128, NT])
    pen = ralloc("r_pen", [128, NT, 4])
    me = ralloc("r_me", [128, NT, 32])
    me2 = ralloc("r_me2", [128, NT, 32])
    oh1 = ralloc("r_oh1", [128, NT, 32])
    oh2 = ralloc("r_oh2", [128, NT, 32])
    v1 = ralloc("r_v1", [128, NT])
    v2 = ralloc("r_v2", [128, NT])
    dv = ralloc("r_dv", [128, NT])
    maskb = ralloc("r_mask", [128, NT, 32], BF16)
    posf = ralloc("r_pos", [128, NT, 32])
    posc = ralloc("r_posc", [128, NT, 32])
    ecap = ralloc("r_ecap", [128, NT, 32])
    tmpr = ralloc("r_tmp", [128, NT, 32])
    sj = ralloc("r_sj", [128, 2, NT])
    pj = ralloc("r_pj", [128, 2, NT])
    sg_ = ralloc("r_sg", [128, 2, NT])
    zrow = ralloc("zrow", [1, D])
    assert ro[0] <= R5 + 26 * KB, ro[0]
    MEMSET("dve", zrow[:, :], 0.0, ["zrow"])
    DMA("sp", Ys[NSLOT:NSLOT + 1, :], zrow[:, :], ["zrow"], ["Ys_zero"])
    RK = "route"
    brb_b = brb[:, :].unsqueeze(1).to_broadcast([128, 8, 36])
    for hlf in range(2):
        TT("dve", lg[:, hlf * 8:(hlf + 1) * 8, :], bank(LB[hlf])[:, 0:288].rearrange("p (t c) -> p t c", t=8),
           brb_b, ALU.add, [("ps", LB[hlf]), "brb"], [RK])

    def rd(fn):
        S.dve(fn, [RK], [RK])

    def bc3(ap2, n_):
        return ap2.unsqueeze(2).to_broadcast([128, NT, n_])

    rd(lambda e: e.tensor_reduce(out=gm[:, :], in_=lg[:, :, 0:4], axis=AX.X, op=ALU.max))
    rd(lambda e: e.tensor_tensor(out=ohg[:, :, :], in0=lg[:, :, 0:4], in1=bc3(gm[:, :], 4), op=ALU.is_equal))
    rd(lambda e: e.tensor_tensor(out=exg[:, :, :], in0=lg[:, :, 0:4], in1=bc3(gm[:, :], 4), op=ALU.subtract))
    ACT(exg[:, :, :], exg[:, :, :], AF.Exp, [RK], [RK])
    rd(lambda e: e.tensor_reduce(out=se[:, :], in_=exg[:, :, :], axis=AX.X, op=ALU.add))
    rd(lambda e: e.reciprocal(out=pg[:, :], in_=se[:, :]))
    rd(lambda e: e.tensor_scalar(out=pen[:, :, :], in0=ohg[:, :, :], scalar1=-1.0, scalar2=1e30,
                                 op0=ALU.add, op1=ALU.mult))
    rd(lambda e: e.tensor_tensor(out=me[:, :, :].rearrange("p t (g j) -> p t g j", g=4),
                                 in0=lg[:, :, 4:36].rearrange("p t (g j) -> p t g j", g=4),
                                 in1=pen[:, :, :].unsqueeze(3).to_broadcast([128, NT, 4, 8]), op=ALU.add))
    rd(lambda e: e.tensor_reduce(out=v1[:, :], in_=me[:, :, :], axis=AX.X, op=ALU.max))
    rd(lambda e: e.tensor_tensor(out=oh1[:, :, :], in0=me[:, :, :], in1=bc3(v1[:, :], 32), op=ALU.is_equal))
    rd(lambda e: e.scalar_tensor_tensor(out=me2[:, :, :], in0=oh1[:, :, :], scalar=-1e30, in1=me[:, :, :],
                                        op0=ALU.mult, op1=ALU.add))
    rd(lambda e: e.tensor_reduce(out=v2[:, :], in_=me2[:, :, :], axis=AX.X, op=ALU.max))
    rd(lambda e: e.tensor_tensor(out=oh2[:, :, :], in0=me2[:, :, :], in1=bc3(v2[:, :], 32), op=ALU.is_equal))
    rd(lambda e: e.tensor_tensor(out=dv[:, :], in0=v1[:, :], in1=v2[:, :], op=ALU.subtract))
    ACT(dv[:, :], dv[:, :], AF.Sigmoid, [RK], [RK])
    rd(lambda e: e.tensor_tensor(out=w12[:, 0, :], in0=dv[:, :], in1=pg[:, :], op=ALU.mult))
    rd(lambda e: e.tensor_tensor(out=w12[:, 1, :], in0=pg[:, :], in1=w12[:, 0, :], op=ALU.subtract))
    rd(lambda e: e.tensor_tensor(out=maskb[:, :, :], in0=oh1[:, :, :], in1=oh2[:, :, :], op=ALU.add))
    PB = 6
    for i in range(NT):
        MM(bank(PB)[:, i * 32:(i + 1) * 32], ltri[:, :], maskb[:, i, :], True, i == 0, [RK, "ltri"], [("ps", PB)])
        for i2 in range(i):
            MM(bank(PB)[:, i * 32:(i + 1) * 32], onesb[:, :], maskb[:, i2, :], False, i2 == i - 1,
               [RK, "onesb"], [("ps", PB)])
    S.act(lambda e: e.activation(out=posf[:, :, :], in_=bank(PB)[:, :].rearrange("p (t c) -> p t c", t=NT),
                                 func=AF.Copy), [("ps", PB)], [RK])
    S.pool(lambda e: e.iota(ecap[:, :, :], pattern=[[0, NT], [CAP, 32]], base=0, channel_multiplier=0,
                            allow_small_or_imprecise_dtypes=True), [RK], [RK])
    rd(lambda e: e.tensor_tensor(out=posc[:, :, :], in0=posf[:, :, :], in1=ecap[:, :, :], op=ALU.add))
    for j, oh in enumerate((oh1, oh2)):
        rd(lambda e, oh=oh: e.tensor_tensor(out=tmpr[:, :, :], in0=posc[:, :, :], in1=oh[:, :, :], op=ALU.mult))
        rd(lambda e, j=j: e.tensor_reduce(out=sj[:, j, :], in_=tmpr[:, :, :], axis=AX.X, op=ALU.add))
        rd(lambda e, oh=oh: e.tensor_tensor(out=tmpr[:, :, :], in0=posf[:, :, :], in1=oh[:, :, :], op=ALU.mult))
        rd(lambda e, j=j: e.tensor_reduce(out=pj[:, j, :], in_=tmpr[:, :, :], axis=AX.X, op=ALU.add))
    rd(lambda e: e.tensor_scalar(out=pj[:, :, :], in0=pj[:, :, :], scalar1=float(CAP) - 0.5, scalar2=1.0e6,
                                 op0=ALU.is_gt, op1=ALU.mult))
    rd(lambda e: e.tensor_tensor(out=sj[:, :, :], in0=sj[:, :, :], in1=pj[:, :, :], op=ALU.add))
    rd(lambda e: e.tensor_scalar(out=sg_[:, :, :], in0=sj[:, :, :], scalar1=float(NSLOT), scalar2=None,
                                 op0=ALU.min))
    S.dve(lambda e: e.tensor_copy(out=sidx[:, :, :].rearrange("p t j -> p j t"), in_=sj[:, :, :]), [RK], ["sidx"])
    S.dve(lambda e: e.tensor_copy(out=gidx[:, :, :].rearrange("p t j -> p j t"), in_=sg_[:, :, :]), [RK], ["gidx"])
    if debug:
        d_w12 = dbg_out("d_w12", [128, 2, NT], F32)
        DMA("sp", d_w12[:, :, :], w12[:, :, :], [RK], ())
        d_sj = dbg_out("d_sj", [128, 2, NT], F32)
        DMA("sp", d_sj[:, :, :], sj[:, :, :], [RK], ())
        d_sidx = dbg_out("d_sidx", [128, NT, 2], I32)
        DMA("sp", d_sidx[:, :, :], sidx[:, :, :], ["sidx"], ())
        d_gidx = dbg_out("d_gidx", [128, NT, 2], I32)
        DMA("sp", d_gidx[:, :, :], gidx[:, :, :], ["gidx"], ())
        d_oh = dbg_out("d_oh", [128, 2, NT, 32], F32)
        DMA("sp", d_oh[:, 0, :, :], oh1[:, :, :], [RK], ())
        DMA("sp", d_oh[:, 1, :, :], oh2[:, :, :], [RK], ())

    XS_KEYS = []
    for i in range(NT):
        for j in range(2):
            ky = ("Xs", i, j)
            XS_KEYS.append(ky)
            S.dma("pool", lambda e, i=i, j=j: e.indirect_dma_start(
                out=Xs[:, :], out_offset=bass.IndirectOffsetOnAxis(ap=sidx[:, i, j:j + 1], axis=0),
                in_=hb[:, i, :], in_offset=None, bounds_check=NSLOT - 1, oob_is_err=False),
                [("hb", i), "sidx"] + (XS0_KEYS if (i == 0 and j == 0) else []), [ky])
    S.barrier()
    if stop_after == "G1":
        S.run()
        return nc, dbg

    SB_ = CAP // 128
    Xb = [sbt(f"Xb{k}", [128, SB_, D], BF16, R5 + k * 4 * KB) for k in range(2)]
    XT = [sbt(f"XT{k}", [128, 8, CAP], BF16, R5 + 8 * KB + k * 4 * KB) for k in range(2)]
    ATs = [sbt(f"ATs{k}", [128, 4, CAP], BF16, R5 + 16 * KB + k * 2 * KB) for k in range(2)]
    Yb = [sbt(f"Yb{k}", [128, SB_, D], F32, R1 + k * 8 * KB) for k in range(2)]
    sgm = [sbt(f"sgm{k}", [128, CAP], F32, R5 + 20 * KB + k * KB) for k in range(4)]
    Yg = [sbt(f"Yg{k}", [128, D], F32, R6 + k * 4 * KB) for k in range(4)]
    sgi = 0
    YS_KEYS = []
    tb = pst[0][:, :].bitcast(BF16)

    def emit_T(e_):
        k = e_ % 2
        for s_ in range(SB_):
            for c in range(8):
                o0 = c * CAP + s_ * 128
                TR(tb[:, o0:o0 + 128], Xb[k][:, s_, c * 128:(c + 1) * 128], identb[:, :],
                   [("Xb", k), "identb"], [("ps", 0), ("ps", 1)])
        S.act(lambda e, k=k: e.activation(out=XT[k][:, :, :].rearrange("p c s -> p (c s)"), in_=tb[:, 0:8 * CAP],
                                          func=AF.Copy), [("ps", 0), ("ps", 1)], [("XT", k)])

    DMA("sp", Xb[0][:, :, :], Xs[0:CAP, :].rearrange("(s p) d -> p s d", p=128), [], [("Xb", 0)])
    if n_exp > 1:
        DMA("sp", Xb[1][:, :, :], Xs[CAP:2 * CAP, :].rearrange("(s p) d -> p s d", p=128), [], [("Xb", 1)])
    if n_exp > 2:
        load_expert(2)
    emit_T(0)
    for e_ in range(n_exp):
        k = e_ % 2
        kw = e_ % NWB
        for m in range(4):
            bgu = 2 + (m % 2)
            for c in range(8):
                MM(bank(bgu)[:, 0:CAP], ewg[kw][:, c, m * 128:(m + 1) * 128], XT[k][:, c, :], c == 0, c == 7,
                   [("ewg", kw, c // 4), ("XT", k)], [("ps", bgu)])
            for c in range(8):
                MM(bank(bgu)[:, CAP:2 * CAP], ewu[kw][:, c, m * 128:(m + 1) * 128], XT[k][:, c, :], c == 0, c == 7,
                   [("ewu", kw, c // 4), ("XT", k)], [("ps", bgu)])
            sx = sgi % 4
            sgi += 1
            ACT(sgm[sx][:, :], bank(bgu)[:, 0:CAP], AF.Silu, [("ps", bgu)], [("sgm", sx)])
            TT("dve", ATs[k][:, m, :], bank(bgu)[:, CAP:2 * CAP], sgm[sx][:, :], ALU.mult,
               [("ps", bgu), ("sgm", sx)], [("ATs", k, m)])
        if e_ + 1 < n_exp:
            emit_T(e_ + 1)
        if e_ + 2 < n_exp:
            DMA("sp", Xb[k][:, :, :], Xs[(e_ + 2) * CAP:(e_ + 3) * CAP, :].rearrange("(s p) d -> p s d", p=128),
                [], [("Xb", k)])
        for s_ in range(SB_):
            for hf in range(2):
                b = 4 + (s_ * 2 + hf) % 4
                for m in range(4):
                    MM(bank(b), ATs[k][:, m, s_ * 128:(s_ + 1) * 128], ewd[kw][:, m, hf * 512:(hf + 1) * 512],
                       m == 0, m == 3, [("ewd", kw, m // 2), ("ATs", k, m)], [("ps", b)])
                if hf == 0:
                    ACT(Yb[k][:, s_, hf * 512:(hf + 1) * 512], bank(b), AF.Copy, [("ps", b)], [("Yb", k, s_, hf)])
                else:
                    S.dve(lambda e, s_=s_, hf=hf, b=b, k=k: e.tensor_copy(out=Yb[k][:, s_, hf * 512:(hf + 1) * 512],
                                                                         in_=bank(b)), [("ps", b)], [("Yb", k, s_, hf)])
        yk = ("Ys", e_)
        YS_KEYS.append(yk)
        DMA("act", Ys[e_ * CAP:(e_ + 1) * CAP, :].rearrange("(s p) d -> p s d", p=128), Yb[k][:, :, :],
            [("Yb", k, s_, hf) for s_ in range(SB_) for hf in range(2)], [yk])
        if e_ + 3 < n_exp:
            load_expert(e_ + 3)

    gi_ = 0
    for i in range(NT):
        for j in range(2):
            kk = gi_ % 4
            gi_ += 1
            S.dma("pool", lambda e, i=i, j=j, kk=kk: e.indirect_dma_start(
                out=Yg[kk][:, :], out_offset=None, in_=Ys[:, :],
                in_offset=bass.IndirectOffsetOnAxis(ap=gidx[:, i, j:j + 1], axis=0)),
                (YS_KEYS + ["Ys_zero", "gidx"]) if gi_ <= 4 else ["gidx"], [("Yg", kk)])
            STT(XRES[:, i, :], Yg[kk][:, :], w12[:, j, i:i + 1], XRES[:, i, :], ALU.mult, ALU.add,
                [("Yg", kk), ("xres", i), RK], [("xres", i)])
    if debug:
        d_x2 = dbg_out("d_x2", [128, NT, D], F32)
        for i in range(NT):
            DMA("sp", d_x2[:, i, :], XRES[:, i, :], [("xres", i)], ())
    S.barrier()
    if stop_after == "G":
        S.run()
        return nc, dbg


    wpg = sbt("wpg", [128, 8, D], BF16, R2)
    wpl = sbt("wpl", [128, 2, D], BF16, R2 + 16 * KB)
    pT = sbt("pT", [128, 2, T], BF16, R2 + 20 * KB)
    pin = [sbt(f"pin{k}", [128, 256], F32, R2 + 28 * KB + k * KB) for k in range(2)]
    ht_ = [sbt(f"ht{k}", [128, 512], F32, R2 + 30 * KB + k * 2 * KB) for k in range(4)]
    wpg_v = w_pg_d.rearrange("(c p) n -> p c n", p=128)
    wpl_v = w_ple_d.rearrange("(c p) n -> p c n", p=128)
    for c2 in range(4):
        DMA("pool", wpg[:, c2 * 2:(c2 + 1) * 2, :], wpg_v[:, c2 * 2:(c2 + 1) * 2, :], (), [("wpg", c2)])
    DMA("pool", wpl[:, :, :], wpl_v[:, :, :], (), ["wpl"])
    norm_T(2, R5)
    for i in range(NT):
        k = i % 2
        DMA("sp", pin[k][:, :], p_d[i * 128:(i + 1) * 128, :], (), [("pin", k)])
        b = nbank()
        for c in range(2):
            TR(bank(b)[:, c * 128:(c + 1) * 128], pin[k][:, c * 128:(c + 1) * 128], ident[:, :],
               [("pin", k), "ident"], [("ps", b)])
        S.act(lambda e, b=b, i=i: e.activation(out=pT[:, :, i * 128:(i + 1) * 128],
                                               in_=bank(b)[:, 0:256].rearrange("p (c t) -> p c t", c=2),
                                               func=AF.Copy), [("ps", b)], [("pT", i)])
    hi = 0
    for i in range(NT):
        for hf in range(2):
            b1, b2 = nbank(), nbank()
            hs = slice(hf * 512, (hf + 1) * 512)
            for c in range(8):
                MM(bank(b1), hT[:, c, i * 128:(i + 1) * 128], wpg[:, c, hs], c == 0, c == 7,
                   [("wpg", c // 2), ("hT", i)], [("ps", b1)])
            for c in range(2):
                MM(bank(b2), pT[:, c, i * 128:(i + 1) * 128], wpl[:, c, hs], c == 0, c == 1,
                   ["wpl", ("pT", i)], [("ps", b2)])
            a_, b_ = hi % 4, (hi + 1) % 4
            hi += 2
            ACT(ht_[a_][:, :], bank(b1), AF.Sigmoid, [("ps", b1)], [("ht", a_)])
            TT("dve", ht_[b_][:, :], bank(b2), ht_[a_][:, :], ALU.mult, [("ps", b2), ("ht", a_)], [("ht", b_)])
            TT("pool", XRES[:, i, hs], XRES[:, i, hs], ht_[b_][:, :], ALU.add, [("ht", b_), ("xres", i)],
               [("xres", i)])

    gfb = sbt("gfb", [128, D], F32, R6)
    ob = [sbt(f"ob{k}", [128, D], F32, R6 + 4 * KB + k * 4 * KB) for k in range(2)]
    junk2 = sbt("junk2", [128, D], BF16, R6 + 12 * KB)
    ss2 = calloc("ss2", [128, 16], F32)
    rs2 = calloc("rs2", [128, 16], F32)
    assert co[0] <= 212736, co[0]
    DMA("sp", gfb[:, :], gfin_d.partition_broadcast(128), (), ["gfb"])
    for i in range(NT):
        k = i % 2
        ACT(junk2[:, :], XRES[:, i, :], AF.Square, [("xres", i)], ["junk2", ("ss2", i)], accum_out=ss2[:, i:i + 1])
        TS("dve", rs2[:, i:i + 1], ss2[:, i:i + 1], 1.0 / D, EPS, ALU.mult, ALU.add, [("ss2", i)], [("rs2", i)])
        ACT(rs2[:, i:i + 1], rs2[:, i:i + 1], AF.Sqrt, [("rs2", i)], [("rs2", i)])
        S.dve(lambda e, i=i: e.reciprocal(out=rs2[:, i:i + 1], in_=rs2[:, i:i + 1]), [("rs2", i)], [("rs2", i)])
        STT(ob[k][:, :], XRES[:, i, :], rs2[:, i:i + 1], gfb[:, :], ALU.mult, ALU.mult,
            [("xres", i), ("rs2", i), "gfb"], [("ob", k)])
        DMA("sp", out_d[i * 128:(i + 1) * 128, :], ob[k][:, :], [("ob", k)], ())
    S.run()
    return nc, dbg


def prep_shared(inp):
    f = np.float32
    sh = {}
    sh["w_in"] = np.ascontiguousarray(inp["w_in"][0], f)
    sh["w_glu"] = np.ascontiguousarray(inp["w_glu"][0], f)
    sh["w_conv_out"] = np.ascontiguousarray(inp["w_conv_out"][0], f)
    sh["w_out"] = np.ascontiguousarray(inp["w_out"][0], f)
    sh["w_ple_gate"] = np.ascontiguousarray(inp["w_ple_gate"][0], f)
    sh["w_ple"] = np.ascontiguousarray(inp["w_ple"][0], f)
    sh["w_exp_gate"] = np.ascontiguousarray(inp["w_exp_gate"][0], f)
    sh["w_exp_up"] = np.ascontiguousarray(inp["w_exp_up"][0], f)
    sh["w_exp_down"] = np.ascontiguousarray(inp["w_exp_down"][0], f)

    def col8(v):
        return np.asarray(v, f).reshape(-1, 128).T

    sh["gcols"] = np.ascontiguousarray(np.concatenate(
        [col8(inp["g_mix"][0]), col8(inp["g_moe"][0]), col8(inp["g_ple"][0])], axis=1))
    sh["bgate"] = np.ascontiguousarray(col8(inp["b_gate"][0]))
    sh["convw"] = np.ascontiguousarray(np.asarray(inp["conv_dw"][0], f).T.reshape(4, 128, 31).transpose(1, 0, 2))
    sh["convp"] = np.ascontiguousarray(np.concatenate(
        [col8(inp["conv_dw_b"][0]), col8(inp["conv_ln_g"][0]), col8(inp["conv_ln_b"][0])], axis=1))
    sh["ssmd"] = np.ascontiguousarray(col8(inp["ssm_d"][0]))

    def colpair(a):
        return np.asarray(a, f).reshape(16, 128).T

    ldt = np.repeat(np.asarray(inp["ssm_log_dt"][0], f)[:, None], 64, axis=1)
    sh["ssmcol"] = np.ascontiguousarray(np.concatenate(
        [colpair(inp["ssm_a_re"][0]), colpair(inp["ssm_a_im"][0]), colpair(ldt)], axis=1))

    def col_masked(A, transpose):
        A = np.asarray(A, f)
        o = np.zeros((2, 64, 16, 2, 16), f)
        for p in range(16):
            for g2 in range(2):
                g = 2 * p + g2
                o[g2, :, p, g2, :] = A[g].T if transpose else A[g]
        return np.ascontiguousarray(o.reshape(128, 16, 32))

    sh["bcol_re"] = col_masked(inp["ssm_b_re"][0], False)
    sh["bcol_im"] = col_masked(inp["ssm_b_im"][0], False)
    sh["ccol_re"] = col_masked(inp["ssm_c_re"][0], True)
    sh["ccol_im"] = col_masked(inp["ssm_c_im"][0], True)
    wrc = np.concatenate([np.asarray(inp["w_router_group"][0], f), np.asarray(inp["w_router_expert"][0], f)], axis=1)
    sh["wr"] = np.ascontiguousarray(wrc.reshape(8, 128, 36).transpose(1, 0, 2))
    sh["br"] = np.ascontiguousarray(np.concatenate(
        [np.asarray(inp["b_router_group"][0], f), np.asarray(inp["b_router_expert"][0], f)]))
    sh["gfin"] = np.ascontiguousarray(inp["g_final"], f)
    sh["gmoe"] = np.ascontiguousarray(inp["g_moe"][0], f)
    return sh


def kernel(**inputs):
    inp = {k: np.asarray(v) for k, v in inputs.items()}
    sh = prep_shared(inp)
    nc, _ = build_nc()
    x = np.asarray(inp["x"], np.float32)
    p = np.asarray(inp["p"][0], np.float32)
    in_maps = []
    for b in range(8):
        m = dict(sh)
        m["x"] = np.ascontiguousarray(x[b])
        m["p"] = np.ascontiguousarray(p[b])
        in_maps.append(m)
    res = run_bass_kernel_spmd(nc, in_maps, core_ids=list(range(8)))
    return np.stack([np.asarray(r["out"], np.float32) for r in res.results], axis=0)
```

```python
import math
from contextlib import ExitStack

import numpy as np
import concourse.bass as bass
import concourse.mybir as mybir
from concourse.bass_utils import run_bass_kernel_spmd

F32 = mybir.dt.float32
BF16 = mybir.dt.bfloat16
AF = mybir.ActivationFunctionType
ALU = mybir.AluOpType
AX = mybir.AxisListType

COMPUTE = ("pe", "act", "dve", "pool")
ALLENG = ("pe", "act", "dve", "pool", "sp")
PI = math.pi


class Op:
    __slots__ = ("eng", "fn", "reads", "writes", "dma", "waits", "signal", "sigval", "deps",
                 "needed", "idx", "barrier", "bg")

    def __init__(self, eng, fn, reads, writes, dma):
        self.eng = eng
        self.fn = fn
        self.reads = tuple(reads)
        self.writes = tuple(writes)
        self.dma = dma
        self.waits = []
        self.signal = None
        self.sigval = None
        self.deps = ()
        self.needed = False
        self.barrier = False
        self.bg = False


class Sched:
    def __init__(self, nc, ring=8):
        self.nc = nc
        self.ops = []
        self.ring = ring

    def add(self, eng, fn, reads=(), writes=(), dma=False):
        op = Op(eng, fn, reads, writes, dma)
        op.idx = len(self.ops)
        self.ops.append(op)
        return op

    def pe(self, fn, reads=(), writes=()):
        return self.add("pe", fn, reads, writes)

    def act(self, fn, reads=(), writes=()):
        return self.add("act", fn, reads, writes)

    def dve(self, fn, reads=(), writes=()):
        return self.add("dve", fn, reads, writes)

    def pool(self, fn, reads=(), writes=()):
        return self.add("pool", fn, reads, writes)

    def dma(self, eng, fn, reads=(), writes=()):
        return self.add(eng, fn, reads, writes, dma=True)

    def dma_bg(self, eng, fn, writes=(), reads=()):
        op = self.add(eng, fn, reads, writes, dma=True)
        op.bg = True
        return op

    def barrier(self):
        for e in ALLENG:
            op = self.add(e, None)
            op.barrier = True

    def schedule(self, sems_compute, sems_ring):
        ops = self.ops
        last_w = {}
        last_w_bg = {}
        readers = {}
        last_on = {}
        pending_dma = []
        i = 0
        n = len(ops)
        while i < n:
            op = ops[i]
            if op.barrier:
                grp = []
                while i < n and ops[i].barrier:
                    grp.append(ops[i])
                    i += 1
                deps = list(last_on.values()) + list(pending_dma)
                for b in grp:
                    b.deps = tuple(sorted(set(deps)))
                for d in deps:
                    ops[d].needed = True
                pending_dma = []
                last_w = {}
                readers = {}
                continue
            deps = set()
            if op.bg:
                bdeps = set()
                for k in op.reads:
                    w = last_w.get(k)
                    if w is None:
                        w = last_w_bg.get(k)
                    if w is not None:
                        bdeps.add(w)
                for k in op.writes:
                    last_w_bg[k] = op.idx
                op.deps = tuple(sorted(bdeps))
                for d_ in op.deps:
                    ops[d_].needed = True
                i += 1
                continue
            for k in op.reads:
                w = last_w.get(k)
                if w is None:
                    w = last_w_bg.get(k)
                if w is not None:
                    deps.add(w)
            for k in op.writes:
                w = last_w.get(k)
                if w is not None:
                    deps.add(w)
                for r in readers.get(k, ()):
                    deps.add(r)
            deps.discard(op.idx)
            fdeps = []
            for d in deps:
                p = ops[d]
                if (not p.dma) and p.eng == op.eng and p.eng == "pe" and not op.dma:
                    continue
                fdeps.append(d)
            op.deps = tuple(sorted(fdeps))
            for d in op.deps:
                ops[d].needed = True
            for k in op.reads:
                readers.setdefault(k, []).append(op.idx)
            for k in op.writes:
                last_w[k] = op.idx
                readers[k] = []
            if op.dma:
                pending_dma.append(op.idx)
            else:
                last_on[op.eng] = op.idx
            i += 1
        cnt = {e: 0 for e in COMPUTE}
        ring_i = {e: 0 for e in ALLENG}
        ring_cnt = {}
        waited = {e: {} for e in ALLENG}
        for op in ops:
            waits = {}
            if op.dma:
                rn = op.eng + ("_bg" if op.bg else "")
                k = ring_i.get(rn, 0)
                ring_i[rn] = k + 1
                sem = sems_ring[rn][k % self.ring]
                prev = ring_cnt.get(sem, 0)
                if prev > 0:
                    waits[sem] = prev
                ring_cnt[sem] = prev + 16
                op.signal = (sem, 16)
                op.sigval = prev + 16
            for d in op.deps:
                p = ops[d]
                sem = p.signal[0]
                v = p.sigval
                if waits.get(sem, 0) < v:
                    waits[sem] = v
            wl = []
            for sem, v in waits.items():
                if waited[op.eng].get(sem, 0) >= v:
                    continue
                waited[op.eng][sem] = v
                wl.append((sem, v))
            op.waits = wl
            if (not op.dma) and op.needed and not op.barrier:
                cnt[op.eng] += 1
                op.signal = (sems_compute[op.eng], 1)
                op.sigval = cnt[op.eng]
        self.ring_cnt = ring_cnt

    def emit_engine(self, eng_name, eng):
        for op in self.ops:
            if op.eng != eng_name:
                continue
            for sem, v in op.waits:
                eng.wait_ge(sem, v)
            if op.fn is None:
                continue
            inst = op.fn(eng)
            if op.signal is not None:
                inst.then_inc(op.signal[0], op.signal[1])

    def final_waits(self, eng_name, eng):
        for sem, v in self.ring_cnt.items():
            if sem in self._ring_of[eng_name]:
                eng.wait_ge(sem, v)

    def run(self):
        nc = self.nc
        with ExitStack() as st:
            sems_compute = {e: st.enter_context(nc.semaphore(f"c_{e}")) for e in COMPUTE}
            dma_engs = sorted({op.eng + ("_bg" if op.bg else "") for op in self.ops if op.dma})
            sems_ring = {e: [st.enter_context(nc.semaphore(f"r_{e}_{i}")) for i in range(self.ring)]
                         for e in dma_engs}
            self._ring_of = {e: set(sems_ring.get(e, ())) | set(sems_ring.get(e + "_bg", ())) for e in ALLENG}
            self.schedule(sems_compute, sems_ring)
            block = st.enter_context(nc.Block())
            sched = self

            @block.sync
            def _(e):
                sched.emit_engine("sp", e)
                sched.final_waits("sp", e)

            @block.tensor
            def _(e):
                sched.emit_engine("pe", e)

            @block.scalar
            def _(e):
                sched.emit_engine("act", e)
                sched.final_waits("act", e)

            @block.vector
            def _(e):
                sched.emit_engine("dve", e)

            @block.gpsimd
            def _(e):
                sched.emit_engine("pool", e)
                sched.final_waits("pool", e)


T = 2048
D = 1024
NT = T // 128
NP_ = T // 512
EPS = 1e-6
SB_BASE = 16640
KB = 1024
NEXP = 32


def build_nc(stop_after=None, debug=False, n_exp=NEXP):
    nc = bass.Bass("TRN2", target_bir_lowering=False)
    S = Sched(nc, ring=16)
    dbg = {}

    def din(name, shape, dt=F32):
        return nc.dram_tensor(name, list(shape), dt, kind="ExternalInput").ap()

    x_d = din("x", [T, D])
    p_d = din("p", [T, 256])
    w_in_d = din("w_in", [D, 3584])
    w_glu_d = din("w_glu", [512, 2048])
    w_co_d = din("w_conv_out", [512, D])
    w_out_d = din("w_out", [D, D])
    w_pg_d = din("w_ple_gate", [D, D])
    w_ple_d = din("w_ple", [256, D])
    wg_d = din("w_exp_gate", [n_exp, D, 512])
    wu_d = din("w_exp_up", [n_exp, D, 512])
    wd_d = din("w_exp_down", [n_exp, 512, D])
    gcols_d = din("gcols", [128, 24])
    bgate_d = din("bgate", [128, 16])
    convw_d = din("convw", [128, 4, 31])
    convp_d = din("convp", [128, 12])
    ssmd_d = din("ssmd", [128, 4])
    ssmcol_d = din("ssmcol", [128, 48])
    bcol_re_d = din("bcol_re", [128, 16, 32])
    bcol_im_d = din("bcol_im", [128, 16, 32])
    ccol_re_d = din("ccol_re", [128, 16, 32])
    ccol_im_d = din("ccol_im", [128, 16, 32])
    wr_d = din("wr", [128, 8, 36])
    br_d = din("br", [36])
    gfin_d = din("gfin", [D])
    gmoe_d = din("gmoe", [D])
    out_d = nc.dram_tensor("out", [T, D], F32, kind="ExternalOutput").ap()

    def dbg_out(name, shape, dt=F32):
        t = nc.dram_tensor(name, list(shape), dt, kind="ExternalOutput").ap()
        dbg[name] = t
        return t

    def sbt(name, shape, dt, off):
        return nc.alloc_sbuf_tensor_at(name, list(shape), dt, offset=SB_BASE + off)

    R0 = 0
    R1 = 64 * KB
    R2 = 96 * KB
    R3 = 113 * KB
    R4 = 129 * KB
    R5 = 145 * KB
    R6 = 177 * KB
    CST = 193 * KB

    XRES = sbt("xres", [128, NT, D], F32, R0)
    hT = sbt("hT", [128, 8, T], BF16, R1)

    co = [CST]

    def calloc(name, shape, dt):
        nbytes = int(np.prod(shape[1:])) * (2 if dt == BF16 else 4)
        nbytes = (nbytes + 31) // 32 * 32
        t = sbt(name, shape, dt, co[0])
        co[0] += nbytes
        return t

    ident = calloc("ident", [128, 128], F32)
    identb = calloc("identb", [128, 128], BF16)
    onesf = calloc("onesf", [128, 128], F32)
    gcols = calloc("gcols_s", [128, 24], F32)
    bgate = calloc("bgate_s", [128, 16], F32)
    convw = calloc("convw_s", [128, 4, 31], F32)
    convp = calloc("convp_s", [128, 12], F32)
    ssmd = calloc("ssmd_s", [128, 4], F32)
    ss = calloc("ss", [128, 16], F32)
    rs = calloc("rs", [128, 16], F32)
    brb = calloc("brb", [128, 36], F32)
    assert co[0] <= 206 * KB, co[0]

    pst = [nc.alloc_psum_tensor(f"ps{i}", [128, 1024], F32) for i in range(4)]

    def bank(k):
        return pst[k // 2][:, (k % 2) * 512:(k % 2) * 512 + 512]

    bank_ctr = [0]

    def nbank():
        k = bank_ctr[0] % 8
        bank_ctr[0] += 1
        return k

    def DMA(q, out, in_, reads=(), writes=()):
        S.dma(q, lambda e: e.dma_start(out=out, in_=in_), reads, writes)

    def MM(out, lhsT, rhs, start, stop, reads, writes, tp=None):
        if tp is None:
            S.pe(lambda e: e.matmul(out, lhsT=lhsT, rhs=rhs, start=start, stop=stop), reads, writes)
        else:
            S.pe(lambda e: e.matmul(out, lhsT=lhsT, rhs=rhs, start=start, stop=stop, tile_position=tp),
                 reads, writes)

    def TR(out, in_, idn, reads, writes):
        S.pe(lambda e: e.transpose(out, in_, idn), reads, writes)

    def ACT(out, in_, func, reads, writes, bias=None, scale=None, accum_out=None):
        kw = {}
        if bias is not None:
            kw["bias"] = bias
        if scale is not None:
            kw["scale"] = scale
        if accum_out is not None:
            kw["accum_out"] = accum_out
        S.act(lambda e: e.activation(out=out, in_=in_, func=func, **kw), reads, writes)

    def TT(eng, out, in0, in1, op, reads, writes):
        S.add(eng, lambda e: e.tensor_tensor(out=out, in0=in0, in1=in1, op=op), reads, writes)

    def TS(eng, out, in0, s1, s2, op0, op1, reads, writes):
        if op1 is None:
            S.add(eng, lambda e: e.tensor_scalar(out=out, in0=in0, scalar1=s1, scalar2=None, op0=op0),
                  reads, writes)
        else:
            S.add(eng, lambda e: e.tensor_scalar(out=out, in0=in0, scalar1=s1, scalar2=s2, op0=op0, op1=op1),
                  reads, writes)

    def STT(out, in0, scalar, in1, op0, op1, reads, writes):
        S.dve(lambda e: e.scalar_tensor_tensor(out=out, in0=in0, scalar=scalar, in1=in1, op0=op0, op1=op1),
              reads, writes)

    def MEMSET(eng, ap, val, writes):
        S.add(eng, lambda e: e.memset(ap, val), (), writes)

    wgb_d = nc.dram_tensor("wgb_scr", [n_exp, D, 512], BF16).ap()
    wub_d = nc.dram_tensor("wub_scr", [n_exp, D, 512], BF16).ap()
    wdb_d = nc.dram_tensor("wdb_scr", [n_exp, 512, D], BF16).ap()
    bg_list = []
    for e_ in range(n_exp):
        for c2 in range(2):
            bg_list.append((wgb_d[e_, c2 * 512:(c2 + 1) * 512, :], wg_d[e_, c2 * 512:(c2 + 1) * 512, :], ("BGg", e_, c2)))
        for c2 in range(2):
            bg_list.append((wub_d[e_, c2 * 512:(c2 + 1) * 512, :], wu_d[e_, c2 * 512:(c2 + 1) * 512, :], ("BGu", e_, c2)))
        for c2 in range(2):
            bg_list.append((wdb_d[e_, c2 * 256:(c2 + 1) * 256, :], wd_d[e_, c2 * 256:(c2 + 1) * 256, :], ("BGd", e_, c2)))
    bg_pos = [0]
    globals_ = {}

    def emit_bg(n):
        if "emit_zero_fill" in globals_:
            globals_["emit_zero_fill"](1)
        for _ in range(n):
            if bg_pos[0] >= len(bg_list):
                return
            o_, i_, ky = bg_list[bg_pos[0]]
            bg_pos[0] += 1
            S.dma_bg("pool", lambda e, o_=o_, i_=i_: e.dma_start(out=o_, in_=i_), [ky])

    MEMSET("dve", onesf[:, :], 1.0, ["onesf0"])
    MEMSET("pool", ident[:, :], 0.0, ["ident0"])
    S.pool(lambda e: e.affine_select(out=ident[:, :], in_=onesf[:, :], pattern=[[-1, 128]],
                                     compare_op=ALU.is_equal, fill=0.0, base=0, channel_multiplier=1),
           ["onesf0", "ident0"], ["ident"])
    S.dve(lambda e: e.tensor_copy(out=identb[:, :], in_=ident[:, :]), ["ident"], ["identb"])
    TS("dve", onesf[:, :], onesf[:, :], 1.0 / 512.0, None, ALU.mult, None, ["onesf0", "ident"], ["onesf"])
    DMA("sp", gcols[:, :], gcols_d[:, :], (), ["gcols"])
    DMA("sp", bgate[:, :], bgate_d[:, :], (), ["bgate"])
    DMA("sp", convw[:, :, :], convw_d[:, :, :], (), ["convw"])
    DMA("sp", convp[:, :], convp_d[:, :], (), ["convp"])
    DMA("sp", ssmd[:, :], ssmd_d[:, :], (), ["ssmd"])
    DMA("sp", brb[:, :], br_d.partition_broadcast(128), (), ["brb"])
    emit_bg(8)

    def norm_T(gi, tmp_off):
        xn = [sbt(f"nt_xn{gi}_{k}", [128, D], F32, tmp_off + k * 4 * KB) for k in range(3)]
        junk = sbt(f"nt_junk{gi}", [128, D], BF16, tmp_off + 12 * KB)
        g_bc = gcols[:, gi * 8:gi * 8 + 8].unsqueeze(2).to_broadcast([128, 8, 128])
        for i in range(NT):
            ACT(junk[:, :], XRES[:, i, :], AF.Square, [("xres", i)], [("ss", i), "junk"], accum_out=ss[:, i:i + 1])
        SS_ALL = [("ss", i) for i in range(NT)]
        TS("dve", rs[:, :], ss[:, :], 1.0 / D, EPS, ALU.mult, ALU.add, SS_ALL, ["rs"])
        ACT(rs[:, :], rs[:, :], AF.Sqrt, ["rs"], ["rs"])
        S.dve(lambda e: e.reciprocal(out=rs[:, :], in_=rs[:, :]), ["rs"], ["rs"])
        for i in range(NT):
            k = i % 3
            kp = i % 2
            ACT(xn[k][:, :], XRES[:, i, :], AF.Copy, [("xres", i), "rs"], [("xn", k)], scale=rs[:, i:i + 1])
            pk = [("ps", 2 * kp), ("ps", 2 * kp + 1)]
            for c in range(8):
                TR(pst[kp][:, c * 128:(c + 1) * 128], xn[k][:, c * 128:(c + 1) * 128], ident[:, :],
                   [("xn", k), "ident"], pk)
            TT("dve", hT[:, :, i * 128:(i + 1) * 128], pst[kp][:, :].rearrange("p (c t) -> p c t", c=8), g_bc,
               ALU.mult, pk + ["gcols"], [("hT", i)])

    CAP = 256
    NSLOT = NEXP * CAP
    if debug:
        Xs = nc.dram_tensor("Xs_scr", [NSLOT + 2, D], BF16, kind="ExternalOutput").ap()
        Ys = nc.dram_tensor("Ys_scr", [NSLOT + 1, D], F32, kind="ExternalOutput").ap()
    else:
        Xs = nc.dram_tensor("Xs_scr", [NSLOT + 2, D], BF16).ap()
        Ys = nc.dram_tensor("Ys_scr", [NSLOT + 1, D], F32).ap()
    zsrc = nc.dram_tensor("zsrc_scr", [256, D], BF16).ap()
    zt = sbt("zt", [128, 2, D], BF16, R3)
    MEMSET("pool", zt[:, :, :], 0.0, ["zt"])
    DMA("pool", zsrc[:, :].rearrange("(p a) d -> p a d", p=128), zt[:, :, :], ["zt"], ["zsrc"])
    S.barrier()
    XS0_KEYS = [("BGx0", n_) for n_ in range(NSLOT // 256)]
    zf_pos = [0]

    def emit_zero_fill(n):
        for _ in range(n):
            n_ = zf_pos[0]
            if n_ >= NSLOT // 256:
                return
            zf_pos[0] += 1
            S.dma_bg("pool", lambda e, n_=n_: e.dma_start(out=Xs[n_ * 256:(n_ + 1) * 256, :], in_=zsrc[:, :]),
                     [("BGx0", n_)], ["BGz"])

    globals_["emit_zero_fill"] = emit_zero_fill
    so = [R4]

    def salloc(name, shape, dt=F32):
        nbytes = int(np.prod(shape[1:])) * (4 if dt == F32 else 2)
        nbytes = (nbytes + 31) // 32 * 32
        t = sbt(name, shape, dt, so[0])
        so[0] += nbytes
        return t

    scol = salloc("scol", [128, 48])
    DMA("sp", scol[:, :], ssmcol_d[:, :], (), ["scol"])
    are = scol[:, 0:16]
    aim = scol[:, 16:32]
    ldt = scol[:, 32:48]
    sv = {}
    for nm in ["dt", "mag", "th", "t0", "t1", "t2", "acc", "cs", "sn", "lr", "li", "den", "nr", "zr", "zi"]:
        sv[nm] = salloc("sv_" + nm, [128, 16])
    zlr = salloc("zlr", [128, 16, 11])
    zli = salloc("zli", [128, 16, 11])
    nzli = salloc("nzli", [128, 16, 11])
    K_ = "ssmp"

    def sACT(out, in_, func, **kw):
        ACT(out, in_, func, [K_, "scol"], [K_], **kw)

    def sTT(out, a, b, op):
        TT("dve", out, a, b, op, [K_, "scol"], [K_])

    def sTS(out, a, s1, s2, op0, op1):
        TS("dve", out, a, s1, s2, op0, op1, [K_, "scol"], [K_])

    sACT(sv["dt"][:, :], ldt, AF.Exp)
    sTT(sv["t0"][:, :], are, sv["dt"][:, :], ALU.mult)
    sACT(sv["mag"][:, :], sv["t0"][:, :], AF.Exp)
    sTT(sv["th"][:, :], aim, sv["dt"][:, :], ALU.mult)

    def range_reduce(out, shift):
        sTS(sv["t1"][:, :], sv["th"][:, :], float(shift), None, ALU.add, None)
        sTS(out, sv["t1"][:, :], 1.0, None, ALU.mult, None)
        for kk in range(1, 8):
            sTS(sv["t2"][:, :], sv["t1"][:, :], (2 * kk - 1) * PI, -2 * PI, ALU.is_gt, ALU.mult)
            sTT(out, out, sv["t2"][:, :], ALU.add)
        sTS(sv["t2"][:, :], sv["t1"][:, :], -PI, 2 * PI, ALU.is_lt, ALU.mult)
        sTT(out, out, sv["t2"][:, :], ALU.add)
        sTS(out, out, 3.1415925, -3.1415925, ALU.min, ALU.max)

    range_reduce(sv["acc"][:, :], 0.0)
    sACT(sv["sn"][:, :], sv["acc"][:, :], AF.Sin)
    range_reduce(sv["acc"][:, :], PI / 2)
    sACT(sv["cs"][:, :], sv["acc"][:, :], AF.Sin)
    sTT(sv["lr"][:, :], sv["mag"][:, :], sv["cs"][:, :], ALU.mult)
    sTT(sv["li"][:, :], sv["mag"][:, :], sv["sn"][:, :], ALU.mult)
    sTT(sv["t0"][:, :], are, are, ALU.mult)
    sTT(sv["t1"][:, :], aim, aim, ALU.mult)
    sTT(sv["den"][:, :], sv["t0"][:, :], sv["t1"][:, :], ALU.add)
    S.dve(lambda e: e.reciprocal(out=sv["den"][:, :], in_=sv["den"][:, :]), [K_], [K_])
    sTS(sv["nr"][:, :], sv["lr"][:, :], -1.0, None, ALU.add, None)
    sTT(sv["t0"][:, :], sv["nr"][:, :], are, ALU.mult)
    sTT(sv["t1"][:, :], sv["li"][:, :], aim, ALU.mult)
    sTT(sv["t0"][:, :], sv["t0"][:, :], sv["t1"][:, :], ALU.add)
    sTT(sv["zr"][:, :], sv["t0"][:, :], sv["den"][:, :], ALU.mult)
    sTT(sv["t0"][:, :], sv["li"][:, :], are, ALU.mult)
    sTT(sv["t1"][:, :], sv["nr"][:, :], aim, ALU.mult)
    sTT(sv["t0"][:, :], sv["t0"][:, :], sv["t1"][:, :], ALU.subtract)
    sTT(sv["zi"][:, :], sv["t0"][:, :], sv["den"][:, :], ALU.mult)
    sTS(zlr[:, :, 0], sv["cs"][:, :], 1.0, None, ALU.mult, None)
    sTS(zli[:, :, 0], sv["sn"][:, :], 1.0, None, ALU.mult, None)
    for l in range(10):
        sTT(sv["t0"][:, :], zlr[:, :, l], zlr[:, :, l], ALU.mult)
        sTT(sv["t1"][:, :], zli[:, :, l], zli[:, :, l], ALU.mult)
        sTT(zlr[:, :, l + 1], sv["t0"][:, :], sv["t1"][:, :], ALU.subtract)
        sTT(sv["t0"][:, :], zlr[:, :, l], zli[:, :, l], ALU.mult)
        sTS(zli[:, :, l + 1], sv["t0"][:, :], 2.0, None, ALU.mult, None)

    sTS(nzli[:, :, :], zli[:, :, :], -1.0, None, ALU.mult, None)
    LPr = salloc("LPr", [128, 16, 9])
    LPi = salloc("LPi", [128, 16, 9])
    sTS(LPr[:, :, 0], sv["lr"][:, :], 0.0, 1.0, ALU.mult, ALU.add)
    sTS(LPi[:, :, 0], sv["lr"][:, :], 0.0, None, ALU.mult, None)
    for m_ in range(8):
        sTT(sv["t0"][:, :], LPr[:, :, m_], sv["lr"][:, :], ALU.mult)
        sTT(sv["t1"][:, :], LPi[:, :, m_], sv["li"][:, :], ALU.mult)
        sTT(LPr[:, :, m_ + 1], sv["t0"][:, :], sv["t1"][:, :], ALU.subtract)
        sTT(sv["t0"][:, :], LPr[:, :, m_], sv["li"][:, :], ALU.mult)
        sTT(sv["t1"][:, :], LPi[:, :, m_], sv["lr"][:, :], ALU.mult)
        sTT(LPi[:, :, m_ + 1], sv["t0"][:, :], sv["t1"][:, :], ALU.add)
    R8 = salloc("R8", [128, 16])
    sTT(sv["t0"][:, :], sv["mag"][:, :], sv["mag"][:, :], ALU.mult)
    sTT(sv["t1"][:, :], sv["t0"][:, :], sv["t0"][:, :], ALU.mult)
    sTT(R8[:, :], sv["t1"][:, :], sv["t1"][:, :], ALU.mult)
    bcr = salloc("bcr", [128, 16, 32])
    bci = salloc("bci", [128, 16, 32])
    ccr = salloc("ccr", [128, 16, 32])
    cci = salloc("cci", [128, 16, 32])
    diagd = salloc("diagd", [128, 4, 128], BF16)
    assert so[0] <= R5, so[0]
    DMA("sp", bcr[:, :, :], bcol_re_d[:, :, :], (), ["bcr"])
    DMA("sp", bci[:, :, :], bcol_im_d[:, :, :], (), ["bci"])
    DMA("sp", ccr[:, :, :], ccol_re_d[:, :, :], (), ["ccr"])
    DMA("sp", cci[:, :, :], ccol_im_d[:, :, :], (), ["cci"])
    for i in range(NT):
        DMA("sp", XRES[:, i, :], x_d[i * 128:(i + 1) * 128, :], (), [("xres", i)])
    norm_T(0, R5)
    if debug:
        d_hT = dbg_out("d_hT", [128, 8, T], BF16)
        DMA("sp", d_hT[:, :, :], hT[:, :, :], [("hT", i) for i in range(NT)], ())
    S.barrier()
    HT_ALL = ["hT_all"]

    uT = sbt("uT", [128, 4, T], BF16, R2)
    wbuf = [sbt(f"wbuf{k}", [128, 8, 512], BF16, R6 + k * 8 * KB) for k in range(2)]
    win_v = w_in_d.rearrange("(c p) n -> p c n", p=128)

    def load_win_block(blk, k):
        for c2 in range(2):
            DMA("pool", wbuf[k][:, c2 * 4:(c2 + 1) * 4, :], win_v[:, c2 * 4:(c2 + 1) * 4, blk * 512:(blk + 1) * 512],
                (), [("wbuf", k, c2)])

    load_win_block(0, 0)
    for m in range(4):
        emit_bg(4)
        for n in range(NP_):
            b = nbank()
            for c in range(8):
                MM(bank(b), wbuf[0][:, c, m * 128:(m + 1) * 128], hT[:, c, n * 512:(n + 1) * 512], c == 0, c == 7,
                   [("wbuf", 0, c // 4)], [("ps", b)])
            ACT(uT[:, m, n * 512:(n + 1) * 512], bank(b), AF.Copy, [("ps", b)], [("uT", m)])
    if debug:
        d_uT = dbg_out("d_uT", [128, 4, T], BF16)
        DMA("sp", d_uT[:, :, :], uT[:, :, :], [("uT", m) for m in range(4)], ())
    if stop_after == "B":
        S.run()
        return nc, dbg

    gT = sbt("gT", [128, 4, T], BF16, R3)
    czr = sbt("czr", [128, 16, 32], F32, R0 + 56 * KB)
    czi = sbt("czi", [128, 16, 32], F32, R0 + 58 * KB)
    czrb = sbt("czrb", [128, 16, 32], BF16, R0 + 60 * KB)
    nczib = sbt("nczib", [128, 16, 32], BF16, R0 + 61 * KB)
    xt_ = [sbt(f"xtmp{k}", [128, 16, 32], F32, R0 + k * 2 * KB) for k in range(4)]

    def b32(ap2):
        return ap2.unsqueeze(2).to_broadcast([128, 16, 32])

    CK = "cprep"
    TT("dve", xt_[0][:, :, :], ccr[:, :, :], b32(sv["zr"][:, :]), ALU.mult, ["ccr", K_], [CK])
    TT("dve", xt_[1][:, :, :], cci[:, :, :], b32(sv["zi"][:, :]), ALU.mult, ["cci", K_, CK], [CK])
    TT("dve", czr[:, :, :], xt_[0][:, :, :], xt_[1][:, :, :], ALU.subtract, [CK], [CK])
    TT("dve", xt_[0][:, :, :], ccr[:, :, :], b32(sv["zi"][:, :]), ALU.mult, [CK], [CK])
    TT("dve", xt_[1][:, :, :], cci[:, :, :], b32(sv["zr"][:, :]), ALU.mult, [CK], [CK])
    TT("dve", czi[:, :, :], xt_[0][:, :, :], xt_[1][:, :, :], ALU.add, [CK], [CK])
    S.dve(lambda e: e.tensor_copy(out=czrb[:, :, :], in_=czr[:, :, :]), [CK], [CK])
    TS("dve", nczib[:, :, :], czi[:, :, :], -1.0, None, ALU.mult, None, [CK], [CK])
    for ch in range(4):
        TS("dve", diagd[:, ch, :], identb[:, :], ssmd[:, ch:ch + 1], None, ALU.mult, None,
           ["identb", "ssmd"], ["diagd"])
    S.barrier()
    WW = [[sbt(f"ww{k}_{ri}", [128, 16, 128], BF16, R6 + k * 8 * KB + ri * 4 * KB) for ri in range(2)]
          for k in range(2)]
    for k in range(2):
        for ri in range(2):
            MEMSET("pool", WW[k][ri][:, :, :], 0.0, [("ww", k, ri)])
    WE = sbt("WE", [128, 4, 8, 2, 128], BF16, R5)
    KT = sbt("KT", [128, 4, 8, 128], BF16, R5 + 16 * KB)
    Spr = calloc("Spr", [128, 16, 256], BF16)
    Spi = sbt("Spi", [128, 16, 256], BF16, R5 + 24 * KB)
    assert co[0] <= 212736, co[0]

    def wide_build(k, src_r, src_i, lr_b, li_b, neg_im, rkeys):
        wk = [("ww", k, 0), ("ww", k, 1)]
        TT("dve", xt_[0][:, :, :], src_r, lr_b, ALU.mult, rkeys + [("xt", 0)], [("xt", 0)])
        TT("pool", xt_[1][:, :, :], src_i, li_b, ALU.mult, rkeys + [("xt", 1)], [("xt", 1)])
        TT("dve", xt_[2][:, :, :], src_r, li_b, ALU.mult, rkeys + [("xt", 2)], [("xt", 2)])
        TT("pool", xt_[3][:, :, :], src_i, lr_b, ALU.mult, rkeys + [("xt", 3)], [("xt", 3)])
        for j in range(4):
            TT("dve", WW[k][0][:, j::4, 32 * j:32 * j + 32], xt_[0][:, j::4, :], xt_[1][:, j::4, :], ALU.subtract,
               [("xt", 0), ("xt", 1)], [wk[0]])
            if neg_im:
                STT(WW[k][1][:, j::4, 32 * j:32 * j + 32], xt_[2][:, j::4, :], -1.0, xt_[3][:, j::4, :],
                    ALU.mult, ALU.subtract, [("xt", 2), ("xt", 3)], [wk[1]])
            else:
                TT("dve", WW[k][1][:, j::4, 32 * j:32 * j + 32], xt_[2][:, j::4, :], xt_[3][:, j::4, :], ALU.add,
                   [("xt", 2), ("xt", 3)], [wk[1]])

    for m_ in range(8):
        k = m_ % 2
        emit_bg(4)
        wide_build(k, bcr[:, :, :], bci[:, :, :], b32(LPr[:, :, m_]), b32(LPi[:, :, m_]), False,
                   ["bcr", "bci", K_])
        wk = [("ww", k, 0), ("ww", k, 1)]
        bK, bWr, bWi = nbank(), nbank(), nbank()
        for ch in range(4):
            for j in range(4):
                p = 4 * ch + j
                o = ch * 128 + 32 * j
                MM(bank(bK)[:, o:o + 32], WW[k][0][:, p, :], czrb[:, p, :], True, False, [wk[0], CK], [("ps", bK)])
                MM(bank(bK)[:, o:o + 32], WW[k][1][:, p, :], nczib[:, p, :], False, True, [wk[1], CK], [("ps", bK)])
        for ri, bW in ((0, bWr), (1, bWi)):
            for ch in range(4):
                for j in range(4):
                    p = 4 * ch + j
                    MM(bank(bW)[:, ch * 128:(ch + 1) * 128], WW[k][ri][:, p, :], identb[:, :], j == 0, j == 3,
                       [wk[ri], "identb"], [("ps", bW)])
        S.act(lambda e, m_=m_, bK=bK: e.activation(out=KT[:, :, m_, :],
                                                   in_=bank(bK).rearrange("p (c n) -> p c n", c=4), func=AF.Copy),
              [("ps", bK)], [("KT", m_)])
        for ri, bW in ((0, bWr), (1, bWi)):
            S.dve(lambda e, m_=m_, ri=ri, bW=bW: e.tensor_copy(out=WE[:, :, 7 - m_, ri, :],
                                                              in_=bank(bW).rearrange("p (c n) -> p c n", c=4)),
                  [("ps", bW)], [("WE", 7 - m_, ri)])
    TT("dve", KT[:, :, 0, :], KT[:, :, 0, :], diagd[:, :, :], ALU.add, [("KT", 0), "diagd"], [("KT", 0)])
    S.barrier()

    tc_ = sbt("l2c", [128, 8, 256], F32, R0)
    td_ = sbt("l2d", [128, 8, 256], F32, R0 + 8 * KB)
    Eb = sbt("l2E", [128, 8, 512], F32, R0 + 16 * KB)
    q1 = sbt("l2q1", [128, 8, 256], F32, R0 + 32 * KB)
    q2 = sbt("l2q2", [128, 8, 256], F32, R0 + 40 * KB)
    Rt = sbt("l2R", [128, 8, 256], F32, R0 + 48 * KB)
    MEMSET("pool", Spr[:, :, 0:1], 0.0, ["Spr0"])
    MEMSET("pool", Spi[:, :, 0:1], 0.0, ["Spi0"])
    for hb_ in range(2):
        ps_ = slice(hb_ * 8, hb_ * 8 + 8)
        LK = ("l2", hb_)
        emit_bg(12)
        for pp in range(8):
            p = hb_ * 8 + pp
            ch, j = p // 4, p % 4
            bE = 4 + (p % 4)
            for ri in range(2):
                for kk in range(8):
                    MM(bank(bE)[:, ri * 256:(ri + 1) * 256], WE[32 * j:32 * j + 32, ch, kk, ri, :],
                       uT[32 * j:32 * j + 32, ch, kk:T:8], kk == 0, kk == 7, [("WE", kk, ri)], [("ps", bE)],
                       tp=(32 * j, 0))
            ACT(Eb[:, pp, :], bank(bE), AF.Copy, [("ps", bE)], [("E", pp), "Er", "Ei"])
        EK = [("E", pp) for pp in range(8)]
        TK = "l2tab"
        MEMSET("dve", tc_[:, :, 0:1], 1.0, [TK])
        MEMSET("dve", td_[:, :, 0:1], 0.0, [TK])
        for l in range(8):
            n_ = 1 << l
            zr_b = zlr[:, ps_, 3 + l].unsqueeze(2).to_broadcast([128, 8, n_])
            zi_b = zli[:, ps_, 3 + l].unsqueeze(2).to_broadcast([128, 8, n_])
            TT("dve", q1[:, :, 0:n_], td_[:, :, 0:n_], zi_b, ALU.mult, [TK, "q1"], ["q1"])
            TT("dve", tc_[:, :, n_:2 * n_], tc_[:, :, 0:n_], zr_b, ALU.mult, [TK], [TK])
            TT("dve", tc_[:, :, n_:2 * n_], tc_[:, :, n_:2 * n_], q1[:, :, 0:n_], ALU.subtract, [TK, "q1"], [TK])
            TT("dve", q1[:, :, 0:n_], td_[:, :, 0:n_], zr_b, ALU.mult, [TK, "q1"], ["q1"])
            TT("dve", td_[:, :, n_:2 * n_], tc_[:, :, 0:n_], zi_b, ALU.mult, [TK], [TK])
            TT("dve", td_[:, :, n_:2 * n_], td_[:, :, n_:2 * n_], q1[:, :, 0:n_], ALU.add, [TK, "q1"], [TK])
        S.pool(lambda e, ps_=ps_: e.tensor_copy(out=Rt[:, :, :], in_=R8[:, ps_].unsqueeze(2).to_broadcast([128, 8, 256])),
               [K_, "Rt"], ["Rt"])
        MEMSET("pool", Rt[:, :, 0:1], 0.0, ["Rt"])
        Er = Eb[:, :, 0:256]
        Ei = Eb[:, :, 256:512]
        TT("dve", q1[:, :, :], tc_[:, :, :], Er, ALU.mult, [TK, "q1"] + EK, ["q1"])
        TT("pool", q2[:, :, :], td_[:, :, :], Ei, ALU.mult, [TK, "q2"] + EK, ["q2"])
        TT("dve", q1[:, :, :], q1[:, :, :], q2[:, :, :], ALU.add, ["q1", "q2"], ["q1"])
        TT("pool", q2[:, :, :], tc_[:, :, :], Ei, ALU.mult, [TK, "q2"] + EK, ["q2"])
        TT("dve", Er, td_[:, :, :], Er, ALU.mult, [TK] + EK, ["Er"])
        TT("dve", q2[:, :, :], q2[:, :, :], Er, ALU.subtract, ["q2", "Er"], ["q2"])
        q1f = q1[:, :, :].rearrange("p a s -> p (a s)")
        q2f = q2[:, :, :].rearrange("p a s -> p (a s)")
        Rtf = Rt[:, :, :].rearrange("p a s -> p (a s)")
        S.dve(lambda e, q1f=q1f, Rtf=Rtf: e.tensor_tensor_scan(out=q1f, data0=Rtf, data1=q1f, initial=0.0,
                                                               op0=ALU.mult, op1=ALU.add), ["q1", "Rt"], ["q1"])
        S.dve(lambda e, q2f=q2f, Rtf=Rtf: e.tensor_tensor_scan(out=q2f, data0=Rtf, data1=q2f, initial=0.0,
                                                               op0=ALU.mult, op1=ALU.add), ["q2", "Rt"], ["q2"])
        TT("pool", Er, tc_[:, :, :], q1[:, :, :], ALU.mult, [TK, "q1", "Er"], ["Er"])
        TT("dve", Ei, td_[:, :, :], q2[:, :, :], ALU.mult, [TK, "q2"] + EK, ["Ei"])
        TT("dve", Spr[:, ps_, 1:256], Eb[:, :, 0:255], Eb[:, :, 256:511], ALU.subtract, ["Er", "Ei"], [("Spr", hb_)])
        TT("pool", Er, tc_[:, :, :], q2[:, :, :], ALU.mult, [TK, "q2", "Er", ("Spr", hb_)], ["Er"])
        TT("dve", Ei, td_[:, :, :], q1[:, :, :], ALU.mult, [TK, "q1", "Ei", ("Spr", hb_)], ["Ei"])
        TT("dve", Spi[:, ps_, 1:256], Eb[:, :, 0:255], Eb[:, :, 256:511], ALU.add, ["Er", "Ei"], [("Spi", hb_)])
    S.barrier()

    for i_ in range(8):
        k = i_ % 2
        emit_bg(3)
        wide_build(k, czr[:, :, :], czi[:, :, :], b32(LPr[:, :, i_ + 1]), b32(LPi[:, :, i_ + 1]), True, [])
        wk = [("ww", k, 0), ("ww", k, 1)]
        for ch in range(4):
            b = nbank()
            for kk in range(i_ + 1):
                MM(bank(b)[:, 0:256], KT[:, ch, i_ - kk, :], uT[:, ch, kk:T:8], kk == 0, False, [], [("ps", b)])
            for j in range(4):
                p = 4 * ch + j
                MM(bank(b)[:, 0:256], WW[k][0][:, p, :], Spr[:, p, :], False, False, [wk[0]], [("ps", b)])
                MM(bank(b)[:, 0:256], WW[k][1][:, p, :], Spi[:, p, :], False, j == 3, [wk[1]], [("ps", b)])
            ACT(gT[:, ch, i_:T:8], bank(b)[:, 0:256], AF.Gelu, [("ps", b)], [("gT", ch)])
    if debug:
        d_gT = dbg_out("d_gT", [128, 4, T], BF16)
        DMA("sp", d_gT[:, :, :], gT[:, :, :], [("gT", m) for m in range(4)], ())
    S.barrier()
    if stop_after == "C":
        S.run()
        return nc, dbg

    zT = sbt("zT", [128, 4, T + 32], BF16, R2)
    HO = 32
    cT = sbt("cT", [128, 4, T], BF16, R4)
    zc = sbt("zc", [128, 4, T], F32, R5)
    dgm = [sbt(f"dgm{k}", [128, 31, 128], BF16, R0 + k * 8 * KB) for k in range(2)]
    sg_t = [sbt(f"sgt{k}", [128, 512], F32, R0 + 16 * KB + k * 2 * KB) for k in range(2)]
    lnm = sbt("lnm", [128, 512], F32, R0 + 20 * KB)
    lnr = sbt("lnr", [128, 512], F32, R0 + 22 * KB)
    lnt = [sbt(f"lnt{k}", [128, 512], F32, R0 + 24 * KB + k * 2 * KB) for k in range(2)]
    sqt = [sbt(f"sqt{k}", [128, 512], F32, R0 + 28 * KB + k * 2 * KB) for k in range(2)]
    load_win_block(1, 1)
    load_win_block(2, 0)
    MEMSET("pool", zT[:, :, 0:HO], 0.0, [("zT", m) for m in range(4)])
    for m in range(4):
        emit_bg(4)
        for n in range(NP_):
            bv, bg = nbank(), nbank()
            for c in range(8):
                MM(bank(bv), wbuf[1][:, c, m * 128:(m + 1) * 128], hT[:, c, n * 512:(n + 1) * 512], c == 0, c == 7,
                   [("wbuf", 1, c // 4)], [("ps", bv)])
            for c in range(8):
                MM(bank(bg), wbuf[0][:, c, m * 128:(m + 1) * 128], hT[:, c, n * 512:(n + 1) * 512], c == 0, c == 7,
                   [("wbuf", 0, c // 4)], [("ps", bg)])
            kk = (m * NP_ + n) % 2
            ACT(sg_t[kk][:, :], bank(bg), AF.Sigmoid, [("ps", bg)], [("sgt", kk)])
            TT("dve", zT[:, m, HO + n * 512:HO + (n + 1) * 512], bank(bv), sg_t[kk][:, :], ALU.mult,
               [("ps", bv), ("sgt", kk)], [("zT", m)])
    if debug:
        d_zT = dbg_out("d_zT", [128, 4, T + 32], BF16)
        DMA("sp", d_zT[:, :, :], zT[:, :, :], [("zT", m) for m in range(4)], ())
    for m in range(4):
        k = m % 2
        emit_bg(6)
        for tp_ in range(31):
            TS("dve", dgm[k][:, tp_, :], identb[:, :], convw[:, m, tp_:tp_ + 1], None, ALU.mult, None,
               ["identb", "convw"], [("dgm", k)])
        for n in range(NP_):
            b = nbank()
            for tp_ in range(31):
                s0 = HO + n * 512 + tp_ - 30
                MM(bank(b), dgm[k][:, tp_, :], zT[:, m, s0:s0 + 512], tp_ == 0, tp_ == 30,
                   [("dgm", k), ("zT", m)], [("ps", b)])
            ACT(zc[:, m, n * 512:(n + 1) * 512], bank(b), AF.Identity, [("ps", b), "convp"], [("zc", m, n)],
                bias=convp[:, m:m + 1])
    if debug:
        d_zc = dbg_out("d_zc", [128, 4, T], F32)
        DMA("sp", d_zc[:, :, :], zc[:, :, :], [("zc", m, n) for m in range(4) for n in range(NP_)], ())
    for n in range(NP_):
        sl = slice(n * 512, (n + 1) * 512)
        emit_bg(2)
        bm, bq = nbank(), nbank()
        for m in range(4):
            MM(bank(bm), onesf[:, :], zc[:, m, sl], m == 0, m == 3, ["onesf", ("zc", m, n)], [("ps", bm)])
        for m in range(4):
            kk = m % 2
            ACT(sqt[kk][:, :], zc[:, m, sl], AF.Square, [("zc", m, n)], [("sqt", kk)])
            MM(bank(bq), onesf[:, :], sqt[kk][:, :], m == 0, m == 3, ["onesf", ("sqt", kk)], [("ps", bq)])
        ACT(lnm[:, :], bank(bm), AF.Copy, [("ps", bm)], ["lnm"])
        TT("dve", lnr[:, :], lnm[:, :], lnm[:, :], ALU.mult, ["lnm"], ["lnr"])
        STT(lnr[:, :], bank(bq), EPS, lnr[:, :], ALU.add, ALU.subtract, [("ps", bq), "lnr"], ["lnr"])
        ACT(lnr[:, :], lnr[:, :], AF.Sqrt, ["lnr"], ["lnr"])
        S.dve(lambda e: e.reciprocal(out=lnr[:, :], in_=lnr[:, :]), ["lnr"], ["lnr"])
        for m in range(4):
            kk = m % 2
            TT("dve", lnt[kk][:, :], zc[:, m, sl], lnm[:, :], ALU.subtract, [("zc", m, n), "lnm"], [("lnt", kk)])
            TT("dve", lnt[kk][:, :], lnt[kk][:, :], lnr[:, :], ALU.mult, [("lnt", kk), "lnr"], [("lnt", kk)])
            ACT(cT[:, m, sl], lnt[kk][:, :], AF.Silu, [("lnt", kk), "convp"], [("cT", m)],
                scale=convp[:, 4 + m:5 + m], bias=convp[:, 8 + m:9 + m])
    if debug:
        d_cT = dbg_out("d_cT", [128, 4, T], BF16)
        DMA("sp", d_cT[:, :, :], cT[:, :, :], [("cT", m) for m in range(4)], ())
    S.barrier()
    if stop_after == "D":
        S.run()
        return nc, dbg

    mT = sbt("mT", [128, 8, T], BF16, R5)
    wE = []
    for k in range(2):
        o = R6 + k * 8 * KB
        wE.append(dict(
            gv=sbt(f"wE_gv{k}", [128, 4, 128], BF16, o),
            gg=sbt(f"wE_gg{k}", [128, 4, 128], BF16, o + 1 * KB),
            co=sbt(f"wE_co{k}", [128, 4, 128], BF16, o + 2 * KB),
            gs=sbt(f"wE_gs{k}", [128, 8, 128], BF16, o + 3 * KB),
            gc=sbt(f"wE_gc{k}", [128, 8, 128], BF16, o + 5 * KB)))
    wglu_v = w_glu_d.rearrange("(c p) n -> p c n", p=128)
    wco_v = w_co_d.rearrange("(c p) n -> p c n", p=128)
    et = [sbt(f"et{k}", [128, 512], F32, R0 + k * 2 * KB) for k in range(6)]

    def load_wE(fc):
        k = fc % 2
        w = wE[k]
        ky = ("wE", k)
        DMA("pool", w["gv"][:, :, :], wglu_v[:, :, fc * 128:(fc + 1) * 128], (), [(ky, "gv")])
        DMA("pool", w["gg"][:, :, :], wglu_v[:, :, 1024 + fc * 128:1024 + (fc + 1) * 128], (), [(ky, "gg")])
        DMA("pool", w["co"][:, :, :], wco_v[:, :, fc * 128:(fc + 1) * 128], (), [(ky, "co")])
        DMA("pool", w["gs"][:, :, :], win_v[:, :, 1536 + fc * 128:1536 + (fc + 1) * 128], (), [(ky, "gs")])
        DMA("pool", w["gc"][:, :, :], win_v[:, :, 2560 + fc * 128:2560 + (fc + 1) * 128], (), [(ky, "gc")])

    load_wE(0)
    wout = sbt("wout", [128, 8, D], BF16, R2)
    wout_v = w_out_d.rearrange("(c p) n -> p c n", p=128)
    for c2 in range(4):
        DMA("pool", wout[:, c2 * 2:(c2 + 1) * 2, :], wout_v[:, c2 * 2:(c2 + 1) * 2, :], (), [("wout", c2)])
    for i in range(3, NT):
        DMA("sp", XRES[:, i, :], x_d[i * 128:(i + 1) * 128, :], (), [("xres", i)])
    for fc in range(8):
        emit_bg(4)
        if fc + 1 < 8:
            load_wE(fc + 1)
        k = fc % 2
        w = wE[k]
        ky = ("wE", k)
        for n in range(NP_):
            sl = slice(n * 512, (n + 1) * 512)
            bzv, bzg, byc, bgs, bgc = nbank(), nbank(), nbank(), nbank(), nbank()
            for c in range(4):
                MM(bank(bzv), w["gv"][:, c, :], gT[:, c, sl], c == 0, c == 3, [(ky, "gv")], [("ps", bzv)])
            for c in range(4):
                MM(bank(bzg), w["gg"][:, c, :], gT[:, c, sl], c == 0, c == 3, [(ky, "gg")], [("ps", bzg)])
            for c in range(4):
                MM(bank(byc), w["co"][:, c, :], cT[:, c, sl], c == 0, c == 3, [(ky, "co")], [("ps", byc)])
            for c in range(8):
                MM(bank(bgs), w["gs"][:, c, :], hT[:, c, sl], c == 0, c == 7, [(ky, "gs")], [("ps", bgs)])
            for c in range(8):
                MM(bank(bgc), w["gc"][:, c, :], hT[:, c, sl], c == 0, c == 7, [(ky, "gc")], [("ps", bgc)])
            ACT(et[0][:, :], bank(bzg), AF.Sigmoid, [("ps", bzg)], [("et", 0)])
            ACT(et[1][:, :], bank(bgs), AF.Sigmoid, [("ps", bgs), "bgate"], [("et", 1)], bias=bgate[:, fc:fc + 1])
            ACT(et[2][:, :], bank(bgc), AF.Sigmoid, [("ps", bgc), "bgate"], [("et", 2)],
                bias=bgate[:, 8 + fc:9 + fc])
            TT("dve", et[3][:, :], bank(bzv), et[0][:, :], ALU.mult, [("ps", bzv), ("et", 0)], [("et", 3)])
            TT("pool", et[3][:, :], et[3][:, :], et[1][:, :], ALU.mult, [("et", 3), ("et", 1)], [("et", 3)])
            TT("dve", et[4][:, :], bank(byc), et[2][:, :], ALU.mult, [("ps", byc), ("et", 2)], [("et", 4)])
            TT("pool", mT[:, fc, sl], et[3][:, :], et[4][:, :], ALU.add, [("et", 3), ("et", 4)], [("mT", fc)])
    if debug:
        d_mT = dbg_out("d_mT", [128, 8, T], BF16)
        DMA("sp", d_mT[:, :, :], mT[:, :, :], [("mT", m) for m in range(8)], ())
    S.barrier()
    if stop_after == "E":
        S.run()
        return nc, dbg

    for i in range(3):
        DMA("sp", XRES[:, i, :], x_d[i * 128:(i + 1) * 128, :], (), [("xres", i)])
    emit_bg(1000)
    for i in range(NT):
        for hf in range(2):
            b = nbank()
            for c in range(8):
                MM(bank(b), mT[:, c, i * 128:(i + 1) * 128], wout[:, c, hf * 512:(hf + 1) * 512], c == 0, c == 7,
                   [("wout", c // 2)], [("ps", b)])
            TT("dve", XRES[:, i, hf * 512:(hf + 1) * 512], bank(b), XRES[:, i, hf * 512:(hf + 1) * 512], ALU.add,
               [("ps", b), ("xres", i)], [("xres", i)])
    if debug:
        d_x1 = dbg_out("d_x1", [128, NT, D], F32)
        for i in range(NT):
            DMA("sp", d_x1[:, i, :], XRES[:, i, :], [("xres", i)], ())
    S.barrier()
    if stop_after == "F":
        S.run()
        return nc, dbg

    I32 = mybir.dt.int32
    hb = sbt("hb", [128, NT, D], BF16, R1)
    wr = sbt("wr_s", [128, 8, 36], F32, R5 + 26 * KB)
    DMA("sp", wr[:, :, :], wr_d[:, :, :], (), ["wr"])
    gmb = sbt("gmb", [128, D], F32, R5 + 28 * KB)
    DMA("sp", gmb[:, :], gmoe_d.partition_broadcast(128), (), ["gmb"])
    w12 = calloc("w12", [128, 2, NT], F32)
    sidx = calloc("sidx", [128, NT, 2], I32)
    gidx = calloc("gidx", [128, NT, 2], I32)
    ltri = calloc("ltri", [128, 128], BF16)
    onesb = calloc("onesb", [128, 128], BF16)
    assert co[0] <= 207 * KB, co[0]
    MEMSET("pool", onesb[:, :], 1.0, ["onesb"])
    S.pool(lambda e: e.affine_select(out=ltri[:, :], in_=onesb[:, :], pattern=[[1, 128]],
                                     compare_op=ALU.is_gt, fill=0.0, base=0, channel_multiplier=-1),
           ["onesb"], ["ltri"])

    EW = 24 * KB
    ewg = [sbt(f"ewg{k}", [128, 8, 512], BF16, R2 + k * EW) for k in range(2)]
    ewu = [sbt(f"ewu{k}", [128, 8, 512], BF16, R2 + k * EW + 8 * KB) for k in range(2)]
    ewd = [sbt(f"ewd{k}", [128, 4, D], BF16, R2 + k * EW + 16 * KB) for k in range(2)]
    assert R2 + 2 * EW <= R5
    ewg.append(sbt("ewg2", [128, 8, 512], BF16, R1 + 16 * KB))
    ewu.append(sbt("ewu2", [128, 8, 512], BF16, R1 + 24 * KB))
    ewd.append(sbt("ewd2", [128, 4, D], BF16, R5 + 24 * KB))
    NWB = 3

    def load_expert(e_):
        k = e_ % NWB
        g_v = wgb_d[e_].rearrange("(c p) n -> p c n", p=128)
        u_v = wub_d[e_].rearrange("(c p) n -> p c n", p=128)
        d_v = wdb_d[e_].rearrange("(c p) n -> p c n", p=128)
        for c2 in range(2):
            DMA("sp", ewg[k][:, c2 * 4:(c2 + 1) * 4, :], g_v[:, c2 * 4:(c2 + 1) * 4, :], [("BGg", e_, c2)],
                [("ewg", k, c2)])
        for c2 in range(2):
            DMA("sp", ewu[k][:, c2 * 4:(c2 + 1) * 4, :], u_v[:, c2 * 4:(c2 + 1) * 4, :], [("BGu", e_, c2)],
                [("ewu", k, c2)])
        for c2 in range(2):
            DMA("sp", ewd[k][:, c2 * 2:(c2 + 1) * 2, :], d_v[:, c2 * 2:(c2 + 1) * 2, :], [("BGd", e_, c2)],
                [("ewd", k, c2)])

    load_expert(0)
    if n_exp > 1:
        load_expert(1)

    xn = [sbt(f"g_xn{k}", [128, D], F32, R6 + k * 4 * KB) for k in range(2)]
    h32 = [sbt(f"g_h32{k}", [128, 8, 128], F32, R6 + 8 * KB + k * 4 * KB) for k in range(2)]
    g_bc = gcols[:, 8:16].unsqueeze(2).to_broadcast([128, 8, 128])
    LB = [4, 5]
    for i in range(NT):
        ACT(hb[:, i, :], XRES[:, i, :], AF.Square, [("xres", i)], [("ss", i), ("hb", i)], accum_out=ss[:, i:i + 1])
    TS("dve", rs[:, :], ss[:, :], 1.0 / D, EPS, ALU.mult, ALU.add, [("ss", i) for i in range(NT)], ["rs"])
    ACT(rs[:, :], rs[:, :], AF.Sqrt, ["rs"], ["rs"])
    S.dve(lambda e: e.reciprocal(out=rs[:, :], in_=rs[:, :]), ["rs"], ["rs"])
    for i in range(NT):
        k = i % 2
        ACT(xn[k][:, :], XRES[:, i, :], AF.Copy, [("xres", i), "rs"], [("xn", k)], scale=rs[:, i:i + 1])
        TT("pool", hb[:, i, :], xn[k][:, :], gmb[:, :], ALU.mult, [("xn", k), "gmb"], [("hb", i)])
        pk = [("ps", 2 * k), ("ps", 2 * k + 1)]
        for c in range(8):
            TR(pst[k][:, c * 128:(c + 1) * 128], xn[k][:, c * 128:(c + 1) * 128], ident[:, :],
               [("xn", k), "ident"], pk)
        TT("dve", h32[k][:, :, :], pst[k][:, :].rearrange("p (c t) -> p c t", c=8), g_bc,
           ALU.mult, pk + ["gcols"], [("h32", k)])
        lb = LB[i // 8]
        col = (i % 8) * 36
        for c in range(8):
            MM(bank(lb)[:, col:col + 36], h32[k][:, c, :], wr[:, c, :], c == 0, c == 7, [("h32", k), "wr"],
               [("ps", lb)])

    ro = [R5]

    def ralloc(name, shape, dt=F32):
        nbytes = int(np.prod(shape[1:])) * (4 if dt in (F32, I32) else 2)
        nbytes = (nbytes + 31) // 32 * 32
        t = sbt(name, shape, dt, ro[0])
        ro[0] += nbytes
        return t

    lg = ralloc("r_lg", [128, NT, 36])
    gm = ralloc("r_gm", [128, NT])
    ohg = ralloc("r_ohg", [128, NT, 4])
    exg = ralloc("r_exg", [128, NT, 4])
    se = ralloc("r_se", [128, NT])
    pg = ralloc("r_pg", [128, NT])
    pen = ralloc("r_pen", [128, NT, 4])
    me = ralloc("r_me", [128, NT, 32])
    me2 = ralloc("r_me2", [128, NT, 32])
    oh1 = ralloc("r_oh1", [128, NT, 32])
    oh2 = ralloc("r_oh2", [128, NT, 32])
    v1 = ralloc("r_v1", [128, NT])
    v2 = ralloc("r_v2", [128, NT])
    dv = ralloc("r_dv", [128, NT])
    maskb = ralloc("r_mask", [128, NT, 32], BF16)
    posf = ralloc("r_pos", [128, NT, 32])
    posc = ralloc("r_posc", [128, NT, 32])
    ecap = ralloc("r_ecap", [128, NT, 32])
    tmpr = ralloc("r_tmp", [128, NT, 32])
    sj = ralloc("r_sj", [128, 2, NT])
    pj = ralloc("r_pj", [128, 2, NT])
    sg_ = ralloc("r_sg", [128, 2, NT])
    zrow = ralloc("zrow", [1, D])
    assert ro[0] <= R5 + 26 * KB, ro[0]
    MEMSET("dve", zrow[:, :], 0.0, ["zrow"])
    DMA("sp", Ys[NSLOT:NSLOT + 1, :], zrow[:, :], ["zrow"], ["Ys_zero"])
    RK = "route"
    brb_b = brb[:, :].unsqueeze(1).to_broadcast([128, 8, 36])
    for hlf in range(2):
        TT("dve", lg[:, hlf * 8:(hlf + 1) * 8, :], bank(LB[hlf])[:, 0:288].rearrange("p (t c) -> p t c", t=8),
           brb_b, ALU.add, [("ps", LB[hlf]), "brb"], [RK])

    def rd(fn):
        S.dve(fn, [RK], [RK])

    def bc3(ap2, n_):
        return ap2.unsqueeze(2).to_broadcast([128, NT, n_])

    rd(lambda e: e.tensor_reduce(out=gm[:, :], in_=lg[:, :, 0:4], axis=AX.X, op=ALU.max))
    rd(lambda e: e.tensor_tensor(out=ohg[:, :, :], in0=lg[:, :, 0:4], in1=bc3(gm[:, :], 4), op=ALU.is_equal))
    rd(lambda e: e.tensor_tensor(out=exg[:, :, :], in0=lg[:, :, 0:4], in1=bc3(gm[:, :], 4), op=ALU.subtract))
    ACT(exg[:, :, :], exg[:, :, :], AF.Exp, [RK], [RK])
    rd(lambda e: e.tensor_reduce(out=se[:, :], in_=exg[:, :, :], axis=AX.X, op=ALU.add))
    rd(lambda e: e.reciprocal(out=pg[:, :], in_=se[:, :]))
    rd(lambda e: e.tensor_scalar(out=pen[:, :, :], in0=ohg[:, :, :], scalar1=-1.0, scalar2=1e30,
                                 op0=ALU.add, op1=ALU.mult))
    rd(lambda e: e.tensor_tensor(out=me[:, :, :].rearrange("p t (g j) -> p t g j", g=4),
                                 in0=lg[:, :, 4:36].rearrange("p t (g j) -> p t g j", g=4),
                                 in1=pen[:, :, :].unsqueeze(3).to_broadcast([128, NT, 4, 8]), op=ALU.add))
    rd(lambda e: e.tensor_reduce(out=v1[:, :], in_=me[:, :, :], axis=AX.X, op=ALU.max))
    rd(lambda e: e.tensor_tensor(out=oh1[:, :, :], in0=me[:, :, :], in1=bc3(v1[:, :], 32), op=ALU.is_equal))
    rd(lambda e: e.scalar_tensor_tensor(out=me2[:, :, :], in0=oh1[:, :, :], scalar=-1e30, in1=me[:, :, :],
                                        op0=ALU.mult, op1=ALU.add))
    rd(lambda e: e.tensor_reduce(out=v2[:, :], in_=me2[:, :, :], axis=AX.X, op=ALU.max))
    rd(lambda e: e.tensor_tensor(out=oh2[:, :, :], in0=me2[:, :, :], in1=bc3(v2[:, :], 32), op=ALU.is_equal))
    rd(lambda e: e.tensor_tensor(out=dv[:, :], in0=v1[:, :], in1=v2[:, :], op=ALU.subtract))
    ACT(dv[:, :], dv[:, :], AF.Sigmoid, [RK], [RK])
    rd(lambda e: e.tensor_tensor(out=w12[:, 0, :], in0=dv[:, :], in1=pg[:, :], op=ALU.mult))
    rd(lambda e: e.tensor_tensor(out=w12[:, 1, :], in0=pg[:, :], in1=w12[:, 0, :], op=ALU.subtract))
    rd(lambda e: e.tensor_tensor(out=maskb[:, :, :], in0=oh1[:, :, :], in1=oh2[:, :, :], op=ALU.add))
    PB = 6
    for i in range(NT):
        MM(bank(PB)[:, i * 32:(i + 1) * 32], ltri[:, :], maskb[:, i, :], True, i == 0, [RK, "ltri"], [("ps", PB)])
        for i2 in range(i):
            MM(bank(PB)[:, i * 32:(i + 1) * 32], onesb[:, :], maskb[:, i2, :], False, i2 == i - 1,
               [RK, "onesb"], [("ps", PB)])
    S.act(lambda e: e.activation(out=posf[:, :, :], in_=bank(PB)[:, :].rearrange("p (t c) -> p t c", t=NT),
                                 func=AF.Copy), [("ps", PB)], [RK])
    S.pool(lambda e: e.iota(ecap[:, :, :], pattern=[[0, NT], [CAP, 32]], base=0, channel_multiplier=0,
                            allow_small_or_imprecise_dtypes=True), [RK], [RK])
    rd(lambda e: e.tensor_tensor(out=posc[:, :, :], in0=posf[:, :, :], in1=ecap[:, :, :], op=ALU.add))
    for j, oh in enumerate((oh1, oh2)):
        rd(lambda e, oh=oh: e.tensor_tensor(out=tmpr[:, :, :], in0=posc[:, :, :], in1=oh[:, :, :], op=ALU.mult))
        rd(lambda e, j=j: e.tensor_reduce(out=sj[:, j, :], in_=tmpr[:, :, :], axis=AX.X, op=ALU.add))
        rd(lambda e, oh=oh: e.tensor_tensor(out=tmpr[:, :, :], in0=posf[:, :, :], in1=oh[:, :, :], op=ALU.mult))
        rd(lambda e, j=j: e.tensor_reduce(out=pj[:, j, :], in_=tmpr[:, :, :], axis=AX.X, op=ALU.add))
    rd(lambda e: e.tensor_scalar(out=pj[:, :, :], in0=pj[:, :, :], scalar1=float(CAP) - 0.5, scalar2=1.0e6,
                                 op0=ALU.is_gt, op1=ALU.mult))
    rd(lambda e: e.tensor_tensor(out=sj[:, :, :], in0=sj[:, :, :], in1=pj[:, :, :], op=ALU.add))
    rd(lambda e: e.tensor_scalar(out=sg_[:, :, :], in0=sj[:, :, :], scalar1=float(NSLOT), scalar2=None,
                                 op0=ALU.min))
    S.dve(lambda e: e.tensor_copy(out=sidx[:, :, :].rearrange("p t j -> p j t"), in_=sj[:, :, :]), [RK], ["sidx"])
    S.dve(lambda e: e.tensor_copy(out=gidx[:, :, :].rearrange("p t j -> p j t"), in_=sg_[:, :, :]), [RK], ["gidx"])
    if debug:
        d_w12 = dbg_out("d_w12", [128, 2, NT], F32)
        DMA("sp", d_w12[:, :, :], w12[:, :, :], [RK], ())
        d_sj = dbg_out("d_sj", [128, 2, NT], F32)
        DMA("sp", d_sj[:, :, :], sj[:, :, :], [RK], ())
        d_sidx = dbg_out("d_sidx", [128, NT, 2], I32)
        DMA("sp", d_sidx[:, :, :], sidx[:, :, :], ["sidx"], ())
        d_gidx = dbg_out("d_gidx", [128, NT, 2], I32)
        DMA("sp", d_gidx[:, :, :], gidx[:, :, :], ["gidx"], ())
        d_oh = dbg_out("d_oh", [128, 2, NT, 32], F32)
        DMA("sp", d_oh[:, 0, :, :], oh1[:, :, :], [RK], ())
        DMA("sp", d_oh[:, 1, :, :], oh2[:, :, :], [RK], ())

    XS_KEYS = []
    for i in range(NT):
        for j in range(2):
            ky = ("Xs", i, j)
            XS_KEYS.append(ky)
            S.dma("pool", lambda e, i=i, j=j: e.indirect_dma_start(
                out=Xs[:, :], out_offset=bass.IndirectOffsetOnAxis(ap=sidx[:, i, j:j + 1], axis=0),
                in_=hb[:, i, :], in_offset=None, bounds_check=NSLOT - 1, oob_is_err=False),
                [("hb", i), "sidx"] + (XS0_KEYS if (i == 0 and j == 0) else []), [ky])
    S.barrier()
    if stop_after == "G1":
        S.run()
        return nc, dbg

    SB_ = CAP // 128
    Xb = [sbt(f"Xb{k}", [128, SB_, D], BF16, R5 + k * 4 * KB) for k in range(2)]
    XT = [sbt(f"XT{k}", [128, 8, CAP], BF16, R5 + 8 * KB + k * 4 * KB) for k in range(2)]
    ATs = [sbt(f"ATs{k}", [128, 4, CAP], BF16, R5 + 16 * KB + k * 2 * KB) for k in range(2)]
    Yb = [sbt(f"Yb{k}", [128, SB_, D], F32, R1 + k * 8 * KB) for k in range(2)]
    sgm = [sbt(f"sgm{k}", [128, CAP], F32, R5 + 20 * KB + k * KB) for k in range(4)]
    Yg = [sbt(f"Yg{k}", [128, D], F32, R6 + k * 4 * KB) for k in range(4)]
    sgi = 0
    YS_KEYS = []
    tb = pst[0][:, :].bitcast(BF16)

    def emit_T(e_):
        k = e_ % 2
        for s_ in range(SB_):
            for c in range(8):
                o0 = c * CAP + s_ * 128
                TR(tb[:, o0:o0 + 128], Xb[k][:, s_, c * 128:(c + 1) * 128], identb[:, :],
                   [("Xb", k), "identb"], [("ps", 0), ("ps", 1)])
        S.act(lambda e, k=k: e.activation(out=XT[k][:, :, :].rearrange("p c s -> p (c s)"), in_=tb[:, 0:8 * CAP],
                                          func=AF.Copy), [("ps", 0), ("ps", 1)], [("XT", k)])

    DMA("sp", Xb[0][:, :, :], Xs[0:CAP, :].rearrange("(s p) d -> p s d", p=128), [], [("Xb", 0)])
    if n_exp > 1:
        DMA("sp", Xb[1][:, :, :], Xs[CAP:2 * CAP, :].rearrange("(s p) d -> p s d", p=128), [], [("Xb", 1)])
    if n_exp > 2:
        load_expert(2)
    emit_T(0)
    for e_ in range(n_exp):
        k = e_ % 2
        kw = e_ % NWB
        for m in range(4):
            bgu = 2 + (m % 2)
            for c in range(8):
                MM(bank(bgu)[:, 0:CAP], ewg[kw][:, c, m * 128:(m + 1) * 128], XT[k][:, c, :], c == 0, c == 7,
                   [("ewg", kw, c // 4), ("XT", k)], [("ps", bgu)])
            for c in range(8):
                MM(bank(bgu)[:, CAP:2 * CAP], ewu[kw][:, c, m * 128:(m + 1) * 128], XT[k][:, c, :], c == 0, c == 7,
                   [("ewu", kw, c // 4), ("XT", k)], [("ps", bgu)])
            sx = sgi % 4
            sgi += 1
            ACT(sgm[sx][:, :], bank(bgu)[:, 0:CAP], AF.Silu, [("ps", bgu)], [("sgm", sx)])
            TT("dve", ATs[k][:, m, :], bank(bgu)[:, CAP:2 * CAP], sgm[sx][:, :], ALU.mult,
               [("ps", bgu), ("sgm", sx)], [("ATs", k, m)])
        if e_ + 1 < n_exp:
            emit_T(e_ + 1)
        if e_ + 2 < n_exp:
            DMA("sp", Xb[k][:, :, :], Xs[(e_ + 2) * CAP:(e_ + 3) * CAP, :].rearrange("(s p) d -> p s d", p=128),
                [], [("Xb", k)])
        for s_ in range(SB_):
            for hf in range(2):
                b = 4 + (s_ * 2 + hf) % 4
                for m in range(4):
                    MM(bank(b), ATs[k][:, m, s_ * 128:(s_ + 1) * 128], ewd[kw][:, m, hf * 512:(hf + 1) * 512],
                       m == 0, m == 3, [("ewd", kw, m // 2), ("ATs", k, m)], [("ps", b)])
                if hf == 0:
                    ACT(Yb[k][:, s_, hf * 512:(hf + 1) * 512], bank(b), AF.Copy, [("ps", b)], [("Yb", k, s_, hf)])
                else:
                    S.dve(lambda e, s_=s_, hf=hf, b=b, k=k: e.tensor_copy(out=Yb[k][:, s_, hf * 512:(hf + 1) * 512],
                                                                         in_=bank(b)), [("ps", b)], [("Yb", k, s_, hf)])
        yk = ("Ys", e_)
        YS_KEYS.append(yk)
        DMA("act", Ys[e_ * CAP:(e_ + 1) * CAP, :].rearrange("(s p) d -> p s d", p=128), Yb[k][:, :, :],
            [("Yb", k, s_, hf) for s_ in range(SB_) for hf in range(2)], [yk])
        if e_ + 3 < n_exp:
            load_expert(e_ + 3)

    wpg = sbt("wpg", [128, 8, D], BF16, R2)
    wpl = sbt("wpl", [128, 2, D], BF16, R2 + 16 * KB)
    wpg_v = w_pg_d.rearrange("(c p) n -> p c n", p=128)
    wpl_v = w_ple_d.rearrange("(c p) n -> p c n", p=128)
    alias_ = [("ewg", 0, 0), ("ewg", 0, 1), ("ewu", 0, 0), ("ewu", 0, 1)]
    for c2 in range(4):
        DMA("pool", wpg[:, c2 * 2:(c2 + 1) * 2, :], wpg_v[:, c2 * 2:(c2 + 1) * 2, :], (), [("wpg", c2), alias_[c2]])
    DMA("pool", wpl[:, :, :], wpl_v[:, :, :], (), ["wpl", ("ewd", 0, 0)])
    gi_ = 0
    for i in range(NT):
        for j in range(2):
            kk = gi_ % 4
            gi_ += 1
            S.dma("pool", lambda e, i=i, j=j, kk=kk: e.indirect_dma_start(
                out=Yg[kk][:, :], out_offset=None, in_=Ys[:, :],
                in_offset=bass.IndirectOffsetOnAxis(ap=gidx[:, i, j:j + 1], axis=0)),
                (YS_KEYS + ["Ys_zero", "gidx"]) if gi_ <= 4 else ["gidx"], [("Yg", kk)])
            STT(XRES[:, i, :], Yg[kk][:, :], w12[:, j, i:i + 1], XRES[:, i, :], ALU.mult, ALU.add,
                [("Yg", kk), ("xres", i), RK], [("xres", i)])
    if debug:
        d_x2 = dbg_out("d_x2", [128, NT, D], F32)
        for i in range(NT):
            DMA("sp", d_x2[:, i, :], XRES[:, i, :], [("xres", i)], ())
    S.barrier()
    if stop_after == "G":
        S.run()
        return nc, dbg


    pT = sbt("pT", [128, 2, T], BF16, R2 + 20 * KB)
    pin = [sbt(f"pin{k}", [128, 256], F32, R2 + 28 * KB + k * KB) for k in range(2)]
    ht_ = [sbt(f"ht{k}", [128, 512], F32, R2 + 30 * KB + k * 2 * KB) for k in range(4)]
    norm_T(2, R5)
    for i in range(NT):
        k = i % 2
        DMA("sp", pin[k][:, :], p_d[i * 128:(i + 1) * 128, :], (), [("pin", k)])
        b = nbank()
        for c in range(2):
            TR(bank(b)[:, c * 128:(c + 1) * 128], pin[k][:, c * 128:(c + 1) * 128], ident[:, :],
               [("pin", k), "ident"], [("ps", b)])
        S.act(lambda e, b=b, i=i: e.activation(out=pT[:, :, i * 128:(i + 1) * 128],
                                               in_=bank(b)[:, 0:256].rearrange("p (c t) -> p c t", c=2),
                                               func=AF.Copy), [("ps", b)], [("pT", i)])
    hi = 0
    for i in range(NT):
        for hf in range(2):
            b1, b2 = nbank(), nbank()
            hs = slice(hf * 512, (hf + 1) * 512)
            for c in range(8):
                MM(bank(b1), hT[:, c, i * 128:(i + 1) * 128], wpg[:, c, hs], c == 0, c == 7,
                   [("wpg", c // 2), ("hT", i)], [("ps", b1)])
            for c in range(2):
                MM(bank(b2), pT[:, c, i * 128:(i + 1) * 128], wpl[:, c, hs], c == 0, c == 1,
                   ["wpl", ("pT", i)], [("ps", b2)])
            a_, b_ = hi % 4, (hi + 1) % 4
            hi += 2
            ACT(ht_[a_][:, :], bank(b1), AF.Sigmoid, [("ps", b1)], [("ht", a_)])
            TT("dve", ht_[b_][:, :], bank(b2), ht_[a_][:, :], ALU.mult, [("ps", b2), ("ht", a_)], [("ht", b_)])
            TT("pool", XRES[:, i, hs], XRES[:, i, hs], ht_[b_][:, :], ALU.add, [("ht", b_), ("xres", i)],
               [("xres", i)])

    gfb = sbt("gfb", [128, D], F32, R6)
    ob = [sbt(f"ob{k}", [128, D], F32, R6 + 4 * KB + k * 4 * KB) for k in range(2)]
    junk2 = sbt("junk2", [128, D], BF16, R6 + 12 * KB)
    ss2 = calloc("ss2", [128, 16], F32)
    rs2 = calloc("rs2", [128, 16], F32)
    assert co[0] <= 212736, co[0]
    DMA("sp", gfb[:, :], gfin_d.partition_broadcast(128), (), ["gfb"])
    for i in range(NT):
        k = i % 2
        ACT(junk2[:, :], XRES[:, i, :], AF.Square, [("xres", i)], ["junk2", ("ss2", i)], accum_out=ss2[:, i:i + 1])
        TS("dve", rs2[:, i:i + 1], ss2[:, i:i + 1], 1.0 / D, EPS, ALU.mult, ALU.add, [("ss2", i)], [("rs2", i)])
        ACT(rs2[:, i:i + 1], rs2[:, i:i + 1], AF.Sqrt, [("rs2", i)], [("rs2", i)])
        S.dve(lambda e, i=i: e.reciprocal(out=rs2[:, i:i + 1], in_=rs2[:, i:i + 1]), [("rs2", i)], [("rs2", i)])
        STT(ob[k][:, :], XRES[:, i, :], rs2[:, i:i + 1], gfb[:, :], ALU.mult, ALU.mult,
            [("xres", i), ("rs2", i), "gfb"], [("ob", k)])
        DMA("sp", out_d[i * 128:(i + 1) * 128, :], ob[k][:, :], [("ob", k)], ())
    S.run()
    return nc, dbg


def prep_shared(inp):
    f = np.float32
    sh = {}
    sh["w_in"] = np.ascontiguousarray(inp["w_in"][0], f)
    sh["w_glu"] = np.ascontiguousarray(inp["w_glu"][0], f)
    sh["w_conv_out"] = np.ascontiguousarray(inp["w_conv_out"][0], f)
    sh["w_out"] = np.ascontiguousarray(inp["w_out"][0], f)
    sh["w_ple_gate"] = np.ascontiguousarray(inp["w_ple_gate"][0], f)
    sh["w_ple"] = np.ascontiguousarray(inp["w_ple"][0], f)
    sh["w_exp_gate"] = np.ascontiguousarray(inp["w_exp_gate"][0], f)
    sh["w_exp_up"] = np.ascontiguousarray(inp["w_exp_up"][0], f)
    sh["w_exp_down"] = np.ascontiguousarray(inp["w_exp_down"][0], f)

    def col8(v):
        return np.asarray(v, f).reshape(-1, 128).T

    sh["gcols"] = np.ascontiguousarray(np.concatenate(
        [col8(inp["g_mix"][0]), col8(inp["g_moe"][0]), col8(inp["g_ple"][0])], axis=1))
    sh["bgate"] = np.ascontiguousarray(col8(inp["b_gate"][0]))
    sh["convw"] = np.ascontiguousarray(np.asarray(inp["conv_dw"][0], f).T.reshape(4, 128, 31).transpose(1, 0, 2))
    sh["convp"] = np.ascontiguousarray(np.concatenate(
        [col8(inp["conv_dw_b"][0]), col8(inp["conv_ln_g"][0]), col8(inp["conv_ln_b"][0])], axis=1))
    sh["ssmd"] = np.ascontiguousarray(col8(inp["ssm_d"][0]))

    def colpair(a):
        return np.asarray(a, f).reshape(16, 128).T

    ldt = np.repeat(np.asarray(inp["ssm_log_dt"][0], f)[:, None], 64, axis=1)
    sh["ssmcol"] = np.ascontiguousarray(np.concatenate(
        [colpair(inp["ssm_a_re"][0]), colpair(inp["ssm_a_im"][0]), colpair(ldt)], axis=1))

    def col_masked(A, transpose):
        A = np.asarray(A, f)
        o = np.zeros((2, 64, 16, 2, 16), f)
        for p in range(16):
            for g2 in range(2):
                g = 2 * p + g2
                o[g2, :, p, g2, :] = A[g].T if transpose else A[g]
        return np.ascontiguousarray(o.reshape(128, 16, 32))

    sh["bcol_re"] = col_masked(inp["ssm_b_re"][0], False)
    sh["bcol_im"] = col_masked(inp["ssm_b_im"][0], False)
    sh["ccol_re"] = col_masked(inp["ssm_c_re"][0], True)
    sh["ccol_im"] = col_masked(inp["ssm_c_im"][0], True)
    wrc = np.concatenate([np.asarray(inp["w_router_group"][0], f), np.asarray(inp["w_router_expert"][0], f)], axis=1)
    sh["wr"] = np.ascontiguousarray(wrc.reshape(8, 128, 36).transpose(1, 0, 2))
    sh["br"] = np.ascontiguousarray(np.concatenate(
        [np.asarray(inp["b_router_group"][0], f), np.asarray(inp["b_router_expert"][0], f)]))
    sh["gfin"] = np.ascontiguousarray(inp["g_final"], f)
    sh["gmoe"] = np.ascontiguousarray(inp["g_moe"][0], f)
    return sh


def kernel(**inputs):
    inp = {k: np.asarray(v) for k, v in inputs.items()}
    sh = prep_shared(inp)
    nc, _ = build_nc()
    x = np.asarray(inp["x"], np.float32)
    p = np.asarray(inp["p"][0], np.float32)
    in_maps = []
    for b in range(8):
        m = dict(sh)
        m["x"] = np.ascontiguousarray(x[b])
        m["p"] = np.ascontiguousarray(p[b])
        in_maps.append(m)
    res = run_bass_kernel_spmd(nc, in_maps, core_ids=list(range(8)))
    return np.stack([np.asarray(r["out"], np.float32) for r in res.results], axis=0)
```

```python
import math
from contextlib import ExitStack

import numpy as np
import concourse.bass as bass
import concourse.mybir as mybir
from concourse.bass_utils import run_bass_kernel_spmd

F32 = mybir.dt.float32
BF16 = mybir.dt.bfloat16
AF = mybir.ActivationFunctionType
ALU = mybir.AluOpType
AX = mybir.AxisListType

COMPUTE = ("pe", "act", "dve", "pool")
ALLENG = ("pe", "act", "dve", "pool", "sp")
PI = math.pi


class Op:
    __slots__ = ("eng", "fn", "reads", "writes", "dma", "waits", "signal", "sigval", "deps",
                 "needed", "idx", "barrier", "bg")

    def __init__(self, eng, fn, reads, writes, dma):
        self.eng = eng
        self.fn = fn
        self.reads = tuple(reads)
        self.writes = tuple(writes)
        self.dma = dma
        self.waits = []
        self.signal = None
        self.sigval = None
        self.deps = ()
        self.needed = False
        self.barrier = False
        self.bg = False


class Sched:
    def __init__(self, nc, ring=8):
        self.nc = nc
        self.ops = []
        self.ring = ring

    def add(self, eng, fn, reads=(), writes=(), dma=False):
        op = Op(eng, fn, reads, writes, dma)
        op.idx = len(self.ops)
        self.ops.append(op)
        return op

    def pe(self, fn, reads=(), writes=()):
        return self.add("pe", fn, reads, writes)

    def act(self, fn, reads=(), writes=()):
        return self.add("act", fn, reads, writes)

    def dve(self, fn, reads=(), writes=()):
        return self.add("dve", fn, reads, writes)

    def pool(self, fn, reads=(), writes=()):
        return self.add("pool", fn, reads, writes)

    def dma(self, eng, fn, reads=(), writes=()):
        return self.add(eng, fn, reads, writes, dma=True)

    def dma_bg(self, eng, fn, writes=(), reads=()):
        op = self.add(eng, fn, reads, writes, dma=True)
        op.bg = True
        return op

    def barrier(self):
        for e in ALLENG:
            op = self.add(e, None)
            op.barrier = True

    def schedule(self, sems_compute, sems_ring):
        ops = self.ops
        last_w = {}
        last_w_bg = {}
        readers = {}
        last_on = {}
        pending_dma = []
        i = 0
        n = len(ops)
        while i < n:
            op = ops[i]
            if op.barrier:
                grp = []
                while i < n and ops[i].barrier:
                    grp.append(ops[i])
                    i += 1
                deps = list(last_on.values()) + list(pending_dma)
                for b in grp:
                    b.deps = tuple(sorted(set(deps)))
                for d in deps:
                    ops[d].needed = True
                pending_dma = []
                last_w = {}
                readers = {}
                continue
            deps = set()
            if op.bg:
                bdeps = set()
                for k in op.reads:
                    w = last_w.get(k)
                    if w is None:
                        w = last_w_bg.get(k)
                    if w is not None:
                        bdeps.add(w)
                for k in op.writes:
                    last_w_bg[k] = op.idx
                op.deps = tuple(sorted(bdeps))
                for d_ in op.deps:
                    ops[d_].needed = True
                i += 1
                continue
            for k in op.reads:
                w = last_w.get(k)
                if w is None:
                    w = last_w_bg.get(k)
                if w is not None:
                    deps.add(w)
            for k in op.writes:
                w = last_w.get(k)
                if w is not None:
                    deps.add(w)
                for r in readers.get(k, ()):
                    deps.add(r)
            deps.discard(op.idx)
            fdeps = []
            for d in deps:
                p = ops[d]
                if (not p.dma) and p.eng == op.eng and p.eng == "pe" and not op.dma:
                    continue
                fdeps.append(d)
            op.deps = tuple(sorted(fdeps))
            for d in op.deps:
                ops[d].needed = True
            for k in op.reads:
                readers.setdefault(k, []).append(op.idx)
            for k in op.writes:
                last_w[k] = op.idx
                readers[k] = []
            if op.dma:
                pending_dma.append(op.idx)
            else:
                last_on[op.eng] = op.idx
            i += 1
        cnt = {e: 0 for e in COMPUTE}
        ring_i = {e: 0 for e in ALLENG}
        ring_cnt = {}
        waited = {e: {} for e in ALLENG}
        for op in ops:
            waits = {}
            if op.dma:
                rn = op.eng + ("_bg" if op.bg else "")
                k = ring_i.get(rn, 0)
                ring_i[rn] = k + 1
                sem = sems_ring[rn][k % self.ring]
                prev = ring_cnt.get(sem, 0)
                if prev > 0:
                    waits[sem] = prev
                ring_cnt[sem] = prev + 16
                op.signal = (sem, 16)
                op.sigval = prev + 16
            for d in op.deps:
                p = ops[d]
                sem = p.signal[0]
                v = p.sigval
                if waits.get(sem, 0) < v:
                    waits[sem] = v
            wl = []
            for sem, v in waits.items():
                if waited[op.eng].get(sem, 0) >= v:
                    continue
                waited[op.eng][sem] = v
                wl.append((sem, v))
            op.waits = wl
            if (not op.dma) and op.needed and not op.barrier:
                cnt[op.eng] += 1
                op.signal = (sems_compute[op.eng], 1)
                op.sigval = cnt[op.eng]
        self.ring_cnt = ring_cnt

    def emit_engine(self, eng_name, eng):
        for op in self.ops:
            if op.eng != eng_name:
                continue
            for sem, v in op.waits:
                eng.wait_ge(sem, v)
            if op.fn is None:
                continue
            inst = op.fn(eng)
            if op.signal is not None:
                inst.then_inc(op.signal[0], op.signal[1])

    def final_waits(self, eng_name, eng):
        for sem, v in self.ring_cnt.items():
            if sem in self._ring_of[eng_name]:
                eng.wait_ge(sem, v)

    def run(self):
        nc = self.nc
        with ExitStack() as st:
            sems_compute = {e: st.enter_context(nc.semaphore(f"c_{e}")) for e in COMPUTE}
            dma_engs = sorted({op.eng + ("_bg" if op.bg else "") for op in self.ops if op.dma})
            sems_ring = {e: [st.enter_context(nc.semaphore(f"r_{e}_{i}")) for i in range(self.ring)]
                         for e in dma_engs}
            self._ring_of = {e: set(sems_ring.get(e, ())) | set(sems_ring.get(e + "_bg", ())) for e in ALLENG}
            self.schedule(sems_compute, sems_ring)
            block = st.enter_context(nc.Block())
            sched = self

            @block.sync
            def _(e):
                sched.emit_engine("sp", e)
                sched.final_waits("sp", e)

            @block.tensor
            def _(e):
                sched.emit_engine("pe", e)

            @block.scalar
            def _(e):
                sched.emit_engine("act", e)
                sched.final_waits("act", e)

            @block.vector
            def _(e):
                sched.emit_engine("dve", e)

            @block.gpsimd
            def _(e):
                sched.emit_engine("pool", e)
                sched.final_waits("pool", e)


T = 2048
D = 1024
NT = T // 128
NP_ = T // 512
EPS = 1e-6
SB_BASE = 16640
KB = 1024
NEXP = 32


def build_nc(stop_after=None, debug=False, n_exp=NEXP):
    nc = bass.Bass("TRN2", target_bir_lowering=False)
    S = Sched(nc, ring=16)
    dbg = {}

    def din(name, shape, dt=F32):
        return nc.dram_tensor(name, list(shape), dt, kind="ExternalInput").ap()

    x_d = din("x", [T, D])
    p_d = din("p", [T, 256])
    w_in_d = din("w_in", [D, 3584])
    w_glu_d = din("w_glu", [512, 2048])
    w_co_d = din("w_conv_out", [512, D])
    w_out_d = din("w_out", [D, D])
    w_pg_d = din("w_ple_gate", [D, D])
    w_ple_d = din("w_ple", [256, D])
    wg_d = din("w_exp_gate", [n_exp, D, 512])
    wu_d = din("w_exp_up", [n_exp, D, 512])
    wd_d = din("w_exp_down", [n_exp, 512, D])
    gcols_d = din("gcols", [128, 24])
    bgate_d = din("bgate", [128, 16])
    convw_d = din("convw", [128, 4, 31])
    convp_d = din("convp", [128, 12])
    ssmd_d = din("ssmd", [128, 4])
    ssmcol_d = din("ssmcol", [128, 48])
    bcol_re_d = din("bcol_re", [128, 16, 32])
    bcol_im_d = din("bcol_im", [128, 16, 32])
    ccol_re_d = din("ccol_re", [128, 16, 32])
    ccol_im_d = din("ccol_im", [128, 16, 32])
    wr_d = din("wr", [128, 8, 36])
    br_d = din("br", [36])
    gfin_d = din("gfin", [D])
    gmoe_d = din("gmoe", [D])
    out_d = nc.dram_tensor("out", [T, D], F32, kind="ExternalOutput").ap()

    def dbg_out(name, shape, dt=F32):
        t = nc.dram_tensor(name, list(shape), dt, kind="ExternalOutput").ap()
        dbg[name] = t
        return t

    def sbt(name, shape, dt, off):
        return nc.alloc_sbuf_tensor_at(name, list(shape), dt, offset=SB_BASE + off)

    R0 = 0
    R1 = 64 * KB
    R2 = 96 * KB
    R3 = 113 * KB
    R4 = 129 * KB
    R5 = 145 * KB
    R6 = 177 * KB
    CST = 193 * KB

    XRES = sbt("xres", [128, NT, D], F32, R0)
    hT = sbt("hT", [128, 8, T], BF16, R1)

    co = [CST]

    def calloc(name, shape, dt):
        nbytes = int(np.prod(shape[1:])) * (2 if dt == BF16 else 4)
        nbytes = (nbytes + 31) // 32 * 32
        t = sbt(name, shape, dt, co[0])
        co[0] += nbytes
        return t

    ident = calloc("ident", [128, 128], F32)
    identb = calloc("identb", [128, 128], BF16)
    onesf = calloc("onesf", [128, 128], F32)
    gcols = calloc("gcols_s", [128, 24], F32)
    bgate = calloc("bgate_s", [128, 16], F32)
    convw = calloc("convw_s", [128, 4, 31], F32)
    convp = calloc("convp_s", [128, 12], F32)
    ssmd = calloc("ssmd_s", [128, 4], F32)
    ss = calloc("ss", [128, 16], F32)
    rs = calloc("rs", [128, 16], F32)
    brb = calloc("brb", [128, 36], F32)
    assert co[0] <= 206 * KB, co[0]

    pst = [nc.alloc_psum_tensor(f"ps{i}", [128, 1024], F32) for i in range(4)]

    def bank(k):
        return pst[k // 2][:, (k % 2) * 512:(k % 2) * 512 + 512]

    bank_ctr = [0]

    def nbank():
        k = bank_ctr[0] % 8
        bank_ctr[0] += 1
        return k

    def DMA(q, out, in_, reads=(), writes=()):
        S.dma(q, lambda e: e.dma_start(out=out, in_=in_), reads, writes)

    def MM(out, lhsT, rhs, start, stop, reads, writes, tp=None):
        if tp is None:
            S.pe(lambda e: e.matmul(out, lhsT=lhsT, rhs=rhs, start=start, stop=stop), reads, writes)
        else:
            S.pe(lambda e: e.matmul(out, lhsT=lhsT, rhs=rhs, start=start, stop=stop, tile_position=tp),
                 reads, writes)

    def TR(out, in_, idn, reads, writes):
        S.pe(lambda e: e.transpose(out, in_, idn), reads, writes)

    def ACT(out, in_, func, reads, writes, bias=None, scale=None, accum_out=None):
        kw = {}
        if bias is not None:
            kw["bias"] = bias
        if scale is not None:
            kw["scale"] = scale
        if accum_out is not None:
            kw["accum_out"] = accum_out
        S.act(lambda e: e.activation(out=out, in_=in_, func=func, **kw), reads, writes)

    def TT(eng, out, in0, in1, op, reads, writes):
        S.add(eng, lambda e: e.tensor_tensor(out=out, in0=in0, in1=in1, op=op), reads, writes)

    def TS(eng, out, in0, s1, s2, op0, op1, reads, writes):
        if op1 is None:
            S.add(eng, lambda e: e.tensor_scalar(out=out, in0=in0, scalar1=s1, scalar2=None, op0=op0),
                  reads, writes)
        else:
            S.add(eng, lambda e: e.tensor_scalar(out=out, in0=in0, scalar1=s1, scalar2=s2, op0=op0, op1=op1),
                  reads, writes)

    def STT(out, in0, scalar, in1, op0, op1, reads, writes):
        S.dve(lambda e: e.scalar_tensor_tensor(out=out, in0=in0, scalar=scalar, in1=in1, op0=op0, op1=op1),
              reads, writes)

    def MEMSET(eng, ap, val, writes):
        S.add(eng, lambda e: e.memset(ap, val), (), writes)

    wgb_d = nc.dram_tensor("wgb_scr", [n_exp, D, 512], BF16).ap()
    wub_d = nc.dram_tensor("wub_scr", [n_exp, D, 512], BF16).ap()
    wdb_d = nc.dram_tensor("wdb_scr", [n_exp, 512, D], BF16).ap()
    bg_list = []
    for e_ in range(n_exp):
        for c2 in range(2):
            bg_list.append((wgb_d[e_, c2 * 512:(c2 + 1) * 512, :], wg_d[e_, c2 * 512:(c2 + 1) * 512, :], ("BGg", e_, c2)))
        for c2 in range(2):
            bg_list.append((wub_d[e_, c2 * 512:(c2 + 1) * 512, :], wu_d[e_, c2 * 512:(c2 + 1) * 512, :], ("BGu", e_, c2)))
        for c2 in range(2):
            bg_list.append((wdb_d[e_, c2 * 256:(c2 + 1) * 256, :], wd_d[e_, c2 * 256:(c2 + 1) * 256, :], ("BGd", e_, c2)))
    bg_pos = [0]
    globals_ = {}

    def emit_bg(n):
        if "emit_zero_fill" in globals_:
            globals_["emit_zero_fill"](1)
        for _ in range(n):
            if bg_pos[0] >= len(bg_list):
                return
            o_, i_, ky = bg_list[bg_pos[0]]
            bg_pos[0] += 1
            S.dma_bg("pool", lambda e, o_=o_, i_=i_: e.dma_start(out=o_, in_=i_), [ky])

    MEMSET("dve", onesf[:, :], 1.0, ["onesf0"])
    MEMSET("pool", ident[:, :], 0.0, ["ident0"])
    S.pool(lambda e: e.affine_select(out=ident[:, :], in_=onesf[:, :], pattern=[[-1, 128]],
                                     compare_op=ALU.is_equal, fill=0.0, base=0, channel_multiplier=1),
           ["onesf0", "ident0"], ["ident"])
    S.dve(lambda e: e.tensor_copy(out=identb[:, :], in_=ident[:, :]), ["ident"], ["identb"])
    TS("dve", onesf[:, :], onesf[:, :], 1.0 / 512.0, None, ALU.mult, None, ["onesf0", "ident"], ["onesf"])
    DMA("sp", gcols[:, :], gcols_d[:, :], (), ["gcols"])
    DMA("sp", bgate[:, :], bgate_d[:, :], (), ["bgate"])
    DMA("sp", convw[:, :, :], convw_d[:, :, :], (), ["convw"])
    DMA("sp", convp[:, :], convp_d[:, :], (), ["convp"])
    DMA("sp", ssmd[:, :], ssmd_d[:, :], (), ["ssmd"])
    DMA("sp", brb[:, :], br_d.partition_broadcast(128), (), ["brb"])
    emit_bg(8)

    def norm_T(gi, tmp_off):
        xn = [sbt(f"nt_xn{gi}_{k}", [128, D], F32, tmp_off + k * 4 * KB) for k in range(3)]
        junk = sbt(f"nt_junk{gi}", [128, D], BF16, tmp_off + 12 * KB)
        g_bc = gcols[:, gi * 8:gi * 8 + 8].unsqueeze(2).to_broadcast([128, 8, 128])
        for i in range(NT):
            ACT(junk[:, :], XRES[:, i, :], AF.Square, [("xres", i)], [("ss", i), "junk"], accum_out=ss[:, i:i + 1])
        SS_ALL = [("ss", i) for i in range(NT)]
        TS("dve", rs[:, :], ss[:, :], 1.0 / D, EPS, ALU.mult, ALU.add, SS_ALL, ["rs"])
        ACT(rs[:, :], rs[:, :], AF.Sqrt, ["rs"], ["rs"])
        S.dve(lambda e: e.reciprocal(out=rs[:, :], in_=rs[:, :]), ["rs"], ["rs"])
        for i in range(NT):
            k = i % 3
            kp = i % 2
            ACT(xn[k][:, :], XRES[:, i, :], AF.Copy, [("xres", i), "rs"], [("xn", k)], scale=rs[:, i:i + 1])
            pk = [("ps", 2 * kp), ("ps", 2 * kp + 1)]
            for c in range(8):
                TR(pst[kp][:, c * 128:(c + 1) * 128], xn[k][:, c * 128:(c + 1) * 128], ident[:, :],
                   [("xn", k), "ident"], pk)
            TT("dve", hT[:, :, i * 128:(i + 1) * 128], pst[kp][:, :].rearrange("p (c t) -> p c t", c=8), g_bc,
               ALU.mult, pk + ["gcols"], [("hT", i)])

    CAP = 256
    NSLOT = NEXP * CAP
    if debug:
        Xs = nc.dram_tensor("Xs_scr", [NSLOT + 2, D], BF16, kind="ExternalOutput").ap()
        Ys = nc.dram_tensor("Ys_scr", [NSLOT + 1, D], F32, kind="ExternalOutput").ap()
    else:
        Xs = nc.dram_tensor("Xs_scr", [NSLOT + 2, D], BF16).ap()
        Ys = nc.dram_tensor("Ys_scr", [NSLOT + 1, D], F32).ap()
    zsrc = nc.dram_tensor("zsrc_scr", [256, D], BF16).ap()
    zt = sbt("zt", [128, 2, D], BF16, R3)
    MEMSET("pool", zt[:, :, :], 0.0, ["zt"])
    DMA("pool", zsrc[:, :].rearrange("(p a) d -> p a d", p=128), zt[:, :, :], ["zt"], ["zsrc"])
    S.barrier()
    XS0_KEYS = [("BGx0", n_) for n_ in range(NSLOT // 256)]
    zf_pos = [0]

    def emit_zero_fill(n):
        for _ in range(n):
            n_ = zf_pos[0]
            if n_ >= NSLOT // 256:
                return
            zf_pos[0] += 1
            S.dma_bg("pool", lambda e, n_=n_: e.dma_start(out=Xs[n_ * 256:(n_ + 1) * 256, :], in_=zsrc[:, :]),
                     [("BGx0", n_)], ["BGz"])

    globals_["emit_zero_fill"] = emit_zero_fill
    so = [R4]

    def salloc(name, shape, dt=F32):
        nbytes = int(np.prod(shape[1:])) * (4 if dt == F32 else 2)
        nbytes = (nbytes + 31) // 32 * 32
        t = sbt(name, shape, dt, so[0])
        so[0] += nbytes
        return t

    scol = salloc("scol", [128, 48])
    DMA("sp", scol[:, :], ssmcol_d[:, :], (), ["scol"])
    are = scol[:, 0:16]
    aim = scol[:, 16:32]
    ldt = scol[:, 32:48]
    sv = {}
    for nm in ["dt", "mag", "th", "t0", "t1", "t2", "acc", "cs", "sn", "lr", "li", "den", "nr", "zr", "zi"]:
        sv[nm] = salloc("sv_" + nm, [128, 16])
    zlr = salloc("zlr", [128, 16, 11])
    zli = salloc("zli", [128, 16, 11])
    nzli = salloc("nzli", [128, 16, 11])
    K_ = "ssmp"

    def sACT(out, in_, func, **kw):
        ACT(out, in_, func, [K_, "scol"], [K_], **kw)

    def sTT(out, a, b, op):
        TT("dve", out, a, b, op, [K_, "scol"], [K_])

    def sTS(out, a, s1, s2, op0, op1):
        TS("dve", out, a, s1, s2, op0, op1, [K_, "scol"], [K_])

    sACT(sv["dt"][:, :], ldt, AF.Exp)
    sTT(sv["t0"][:, :], are, sv["dt"][:, :], ALU.mult)
    sACT(sv["mag"][:, :], sv["t0"][:, :], AF.Exp)
    sTT(sv["th"][:, :], aim, sv["dt"][:, :], ALU.mult)

    def range_reduce(out, shift):
        sTS(sv["t1"][:, :], sv["th"][:, :], float(shift), None, ALU.add, None)
        sTS(out, sv["t1"][:, :], 1.0, None, ALU.mult, None)
        for kk in range(1, 8):
            sTS(sv["t2"][:, :], sv["t1"][:, :], (2 * kk - 1) * PI, -2 * PI, ALU.is_gt, ALU.mult)
            sTT(out, out, sv["t2"][:, :], ALU.add)
        sTS(sv["t2"][:, :], sv["t1"][:, :], -PI, 2 * PI, ALU.is_lt, ALU.mult)
        sTT(out, out, sv["t2"][:, :], ALU.add)
        sTS(out, out, 3.1415925, -3.1415925, ALU.min, ALU.max)

    range_reduce(sv["acc"][:, :], 0.0)
    sACT(sv["sn"][:, :], sv["acc"][:, :], AF.Sin)
    range_reduce(sv["acc"][:, :], PI / 2)
    sACT(sv["cs"][:, :], sv["acc"][:, :], AF.Sin)
    sTT(sv["lr"][:, :], sv["mag"][:, :], sv["cs"][:, :], ALU.mult)
    sTT(sv["li"][:, :], sv["mag"][:, :], sv["sn"][:, :], ALU.mult)
    sTT(sv["t0"][:, :], are, are, ALU.mult)
    sTT(sv["t1"][:, :], aim, aim, ALU.mult)
    sTT(sv["den"][:, :], sv["t0"][:, :], sv["t1"][:, :], ALU.add)
    S.dve(lambda e: e.reciprocal(out=sv["den"][:, :], in_=sv["den"][:, :]), [K_], [K_])
    sTS(sv["nr"][:, :], sv["lr"][:, :], -1.0, None, ALU.add, None)
    sTT(sv["t0"][:, :], sv["nr"][:, :], are, ALU.mult)
    sTT(sv["t1"][:, :], sv["li"][:, :], aim, ALU.mult)
    sTT(sv["t0"][:, :], sv["t0"][:, :], sv["t1"][:, :], ALU.add)
    sTT(sv["zr"][:, :], sv["t0"][:, :], sv["den"][:, :], ALU.mult)
    sTT(sv["t0"][:, :], sv["li"][:, :], are, ALU.mult)
    sTT(sv["t1"][:, :], sv["nr"][:, :], aim, ALU.mult)
    sTT(sv["t0"][:, :], sv["t0"][:, :], sv["t1"][:, :], ALU.subtract)
    sTT(sv["zi"][:, :], sv["t0"][:, :], sv["den"][:, :], ALU.mult)
    sTS(zlr[:, :, 0], sv["cs"][:, :], 1.0, None, ALU.mult, None)
    sTS(zli[:, :, 0], sv["sn"][:, :], 1.0, None, ALU.mult, None)
    for l in range(10):
        sTT(sv["t0"][:, :], zlr[:, :, l], zlr[:, :, l], ALU.mult)
        sTT(sv["t1"][:, :], zli[:, :, l], zli[:, :, l], ALU.mult)
        sTT(zlr[:, :, l + 1], sv["t0"][:, :], sv["t1"][:, :], ALU.subtract)
        sTT(sv["t0"][:, :], zlr[:, :, l], zli[:, :, l], ALU.mult)
        sTS(zli[:, :, l + 1], sv["t0"][:, :], 2.0, None, ALU.mult, None)

    sTS(nzli[:, :, :], zli[:, :, :], -1.0, None, ALU.mult, None)
    LPr = salloc("LPr", [128, 16, 9])
    LPi = salloc("LPi", [128, 16, 9])
    sTS(LPr[:, :, 0], sv["lr"][:, :], 0.0, 1.0, ALU.mult, ALU.add)
    sTS(LPi[:, :, 0], sv["lr"][:, :], 0.0, None, ALU.mult, None)
    for m_ in range(8):
        sTT(sv["t0"][:, :], LPr[:, :, m_], sv["lr"][:, :], ALU.mult)
        sTT(sv["t1"][:, :], LPi[:, :, m_], sv["li"][:, :], ALU.mult)
        sTT(LPr[:, :, m_ + 1], sv["t0"][:, :], sv["t1"][:, :], ALU.subtract)
        sTT(sv["t0"][:, :], LPr[:, :, m_], sv["li"][:, :], ALU.mult)
        sTT(sv["t1"][:, :], LPi[:, :, m_], sv["lr"][:, :], ALU.mult)
        sTT(LPi[:, :, m_ + 1], sv["t0"][:, :], sv["t1"][:, :], ALU.add)
    R8 = salloc("R8", [128, 16])
    sTT(sv["t0"][:, :], sv["mag"][:, :], sv["mag"][:, :], ALU.mult)
    sTT(sv["t1"][:, :], sv["t0"][:, :], sv["t0"][:, :], ALU.mult)
    sTT(R8[:, :], sv["t1"][:, :], sv["t1"][:, :], ALU.mult)
    bcr = salloc("bcr", [128, 16, 32])
    bci = salloc("bci", [128, 16, 32])
    ccr = salloc("ccr", [128, 16, 32])
    cci = salloc("cci", [128, 16, 32])
    diagd = salloc("diagd", [128, 4, 128], BF16)
    assert so[0] <= R5, so[0]
    DMA("sp", bcr[:, :, :], bcol_re_d[:, :, :], (), ["bcr"])
    DMA("sp", bci[:, :, :], bcol_im_d[:, :, :], (), ["bci"])
    DMA("sp", ccr[:, :, :], ccol_re_d[:, :, :], (), ["ccr"])
    DMA("sp", cci[:, :, :], ccol_im_d[:, :, :], (), ["cci"])
    for i in range(NT):
        DMA("sp", XRES[:, i, :], x_d[i * 128:(i + 1) * 128, :], (), [("xres", i)])
    norm_T(0, R5)
    if debug:
        d_hT = dbg_out("d_hT", [128, 8, T], BF16)
        DMA("sp", d_hT[:, :, :], hT[:, :, :], [("hT", i) for i in range(NT)], ())
    S.barrier()
    HT_ALL = ["hT_all"]

    uT = sbt("uT", [128, 4, T], BF16, R2)
    wbuf = [sbt(f"wbuf{k}", [128, 8, 512], BF16, R6 + k * 8 * KB) for k in range(2)]
    win_v = w_in_d.rearrange("(c p) n -> p c n", p=128)

    def load_win_block(blk, k):
        for c2 in range(2):
            DMA("pool", wbuf[k][:, c2 * 4:(c2 + 1) * 4, :], win_v[:, c2 * 4:(c2 + 1) * 4, blk * 512:(blk + 1) * 512],
                (), [("wbuf", k, c2)])

    load_win_block(0, 0)
    for m in range(4):
        emit_bg(4)
        for n in range(NP_):
            b = nbank()
            for c in range(8):
                MM(bank(b), wbuf[0][:, c, m * 128:(m + 1) * 128], hT[:, c, n * 512:(n + 1) * 512], c == 0, c == 7,
                   [("wbuf", 0, c // 4)], [("ps", b)])
            ACT(uT[:, m, n * 512:(n + 1) * 512], bank(b), AF.Copy, [("ps", b)], [("uT", m)])
    if debug:
        d_uT = dbg_out("d_uT", [128, 4, T], BF16)
        DMA("sp", d_uT[:, :, :], uT[:, :, :], [("uT", m) for m in range(4)], ())
    if stop_after == "B":
        S.run()
        return nc, dbg

    gT = sbt("gT", [128, 4, T], BF16, R3)
    czr = sbt("czr", [128, 16, 32], F32, R0 + 56 * KB)
    czi = sbt("czi", [128, 16, 32], F32, R0 + 58 * KB)
    czrb = sbt("czrb", [128, 16, 32], BF16, R0 + 60 * KB)
    nczib = sbt("nczib", [128, 16, 32], BF16, R0 + 61 * KB)
    xt_ = [sbt(f"xtmp{k}", [128, 16, 32], F32, R0 + k * 2 * KB) for k in range(4)]

    def b32(ap2):
        return ap2.unsqueeze(2).to_broadcast([128, 16, 32])

    CK = "cprep"
    TT("dve", xt_[0][:, :, :], ccr[:, :, :], b32(sv["zr"][:, :]), ALU.mult, ["ccr", K_], [CK])
    TT("dve", xt_[1][:, :, :], cci[:, :, :], b32(sv["zi"][:, :]), ALU.mult, ["cci", K_, CK], [CK])
    TT("dve", czr[:, :, :], xt_[0][:, :, :], xt_[1][:, :, :], ALU.subtract, [CK], [CK])
    TT("dve", xt_[0][:, :, :], ccr[:, :, :], b32(sv["zi"][:, :]), ALU.mult, [CK], [CK])
    TT("dve", xt_[1][:, :, :], cci[:, :, :], b32(sv["zr"][:, :]), ALU.mult, [CK], [CK])
    TT("dve", czi[:, :, :], xt_[0][:, :, :], xt_[1][:, :, :], ALU.add, [CK], [CK])
    S.dve(lambda e: e.tensor_copy(out=czrb[:, :, :], in_=czr[:, :, :]), [CK], [CK])
    TS("dve", nczib[:, :, :], czi[:, :, :], -1.0, None, ALU.mult, None, [CK], [CK])
    for ch in range(4):
        TS("dve", diagd[:, ch, :], identb[:, :], ssmd[:, ch:ch + 1], None, ALU.mult, None,
           ["identb", "ssmd"], ["diagd"])
    S.barrier()
    WW = [[sbt(f"ww{k}_{ri}", [128, 16, 128], BF16, R6 + k * 8 * KB + ri * 4 * KB) for ri in range(2)]
          for k in range(2)]
    for k in range(2):
        for ri in range(2):
            MEMSET("pool", WW[k][ri][:, :, :], 0.0, [("ww", k, ri)])
    WE = sbt("WE", [128, 4, 8, 2, 128], BF16, R5)
    KT = sbt("KT", [128, 4, 8, 128], BF16, R5 + 16 * KB)
    Spr = calloc("Spr", [128, 16, 256], BF16)
    Spi = sbt("Spi", [128, 16, 256], BF16, R5 + 24 * KB)
    assert co[0] <= 212736, co[0]

    def wide_build(k, src_r, src_i, lr_b, li_b, neg_im, rkeys):
        wk = [("ww", k, 0), ("ww", k, 1)]
        TT("dve", xt_[0][:, :, :], src_r, lr_b, ALU.mult, rkeys + [("xt", 0)], [("xt", 0)])
        TT("pool", xt_[1][:, :, :], src_i, li_b, ALU.mult, rkeys + [("xt", 1)], [("xt", 1)])
        TT("dve", xt_[2][:, :, :], src_r, li_b, ALU.mult, rkeys + [("xt", 2)], [("xt", 2)])
        TT("pool", xt_[3][:, :, :], src_i, lr_b, ALU.mult, rkeys + [("xt", 3)], [("xt", 3)])
        for j in range(4):
            TT("dve", WW[k][0][:, j::4, 32 * j:32 * j + 32], xt_[0][:, j::4, :], xt_[1][:, j::4, :], ALU.subtract,
               [("xt", 0), ("xt", 1)], [wk[0]])
            if neg_im:
                STT(WW[k][1][:, j::4, 32 * j:32 * j + 32], xt_[2][:, j::4, :], -1.0, xt_[3][:, j::4, :],
                    ALU.mult, ALU.subtract, [("xt", 2), ("xt", 3)], [wk[1]])
            else:
                TT("dve", WW[k][1][:, j::4, 32 * j:32 * j + 32], xt_[2][:, j::4, :], xt_[3][:, j::4, :], ALU.add,
                   [("xt", 2), ("xt", 3)], [wk[1]])

    for m_ in range(8):
        k = m_ % 2
        emit_bg(4)
        wide_build(k, bcr[:, :, :], bci[:, :, :], b32(LPr[:, :, m_]), b32(LPi[:, :, m_]), False,
                   ["bcr", "bci", K_])
        wk = [("ww", k, 0), ("ww", k, 1)]
        bK, bWr, bWi = nbank(), nbank(), nbank()
        for ch in range(4):
            for j in range(4):
                p = 4 * ch + j
                o = ch * 128 + 32 * j
                MM(bank(bK)[:, o:o + 32], WW[k][0][:, p, :], czrb[:, p, :], True, False, [wk[0], CK], [("ps", bK)])
                MM(bank(bK)[:, o:o + 32], WW[k][1][:, p, :], nczib[:, p, :], False, True, [wk[1], CK], [("ps", bK)])
        for ri, bW in ((0, bWr), (1, bWi)):
            for ch in range(4):
                for j in range(4):
                    p = 4 * ch + j
                    MM(bank(bW)[:, ch * 128:(ch + 1) * 128], WW[k][ri][:, p, :], identb[:, :], j == 0, j == 3,
                       [wk[ri], "identb"], [("ps", bW)])
        S.act(lambda e, m_=m_, bK=bK: e.activation(out=KT[:, :, m_, :],
                                                   in_=bank(bK).rearrange("p (c n) -> p c n", c=4), func=AF.Copy),
              [("ps", bK)], [("KT", m_)])
        for ri, bW in ((0, bWr), (1, bWi)):
            S.dve(lambda e, m_=m_, ri=ri, bW=bW: e.tensor_copy(out=WE[:, :, 7 - m_, ri, :],
                                                              in_=bank(bW).rearrange("p (c n) -> p c n", c=4)),
                  [("ps", bW)], [("WE", 7 - m_, ri)])
    TT("dve", KT[:, :, 0, :], KT[:, :, 0, :], diagd[:, :, :], ALU.add, [("KT", 0), "diagd"], [("KT", 0)])
    S.barrier()

    tc_ = sbt("l2c", [128, 8, 256], F32, R0)
    td_ = sbt("l2d", [128, 8, 256], F32, R0 + 8 * KB)
    Eb = sbt("l2E", [128, 8, 512], F32, R0 + 16 * KB)
    q1 = sbt("l2q1", [128, 8, 256], F32, R0 + 32 * KB)
    q2 = sbt("l2q2", [128, 8, 256], F32, R0 + 40 * KB)
    Rt = sbt("l2R", [128, 8, 256], F32, R0 + 48 * KB)
    MEMSET("pool", Spr[:, :, 0:1], 0.0, ["Spr0"])
    MEMSET("pool", Spi[:, :, 0:1], 0.0, ["Spi0"])
    for hb_ in range(2):
        ps_ = slice(hb_ * 8, hb_ * 8 + 8)
        LK = ("l2", hb_)
        emit_bg(12)
        for pp in range(8):
            p = hb_ * 8 + pp
            ch, j = p // 4, p % 4
            bE = 4 + (p % 4)
            for ri in range(2):
                for kk in range(8):
                    MM(bank(bE)[:, ri * 256:(ri + 1) * 256], WE[32 * j:32 * j + 32, ch, kk, ri, :],
                       uT[32 * j:32 * j + 32, ch, kk:T:8], kk == 0, kk == 7, [("WE", kk, ri)], [("ps", bE)],
                       tp=(32 * j, 0))
            ACT(Eb[:, pp, :], bank(bE), AF.Copy, [("ps", bE)], [("E", pp), "Er", "Ei"])
        EK = [("E", pp) for pp in range(8)]
        TK = "l2tab"
        MEMSET("dve", tc_[:, :, 0:1], 1.0, [TK])
        MEMSET("dve", td_[:, :, 0:1], 0.0, [TK])
        for l in range(8):
            n_ = 1 << l
            zr_b = zlr[:, ps_, 3 + l].unsqueeze(2).to_broadcast([128, 8, n_])
            zi_b = zli[:, ps_, 3 + l].unsqueeze(2).to_broadcast([128, 8, n_])
            TT("dve", q1[:, :, 0:n_], td_[:, :, 0:n_], zi_b, ALU.mult, [TK, "q1"], ["q1"])
            TT("dve", tc_[:, :, n_:2 * n_], tc_[:, :, 0:n_], zr_b, ALU.mult, [TK], [TK])
            TT("dve", tc_[:, :, n_:2 * n_], tc_[:, :, n_:2 * n_], q1[:, :, 0:n_], ALU.subtract, [TK, "q1"], [TK])
            TT("dve", q1[:, :, 0:n_], td_[:, :, 0:n_], zr_b, ALU.mult, [TK, "q1"], ["q1"])
            TT("dve", td_[:, :, n_:2 * n_], tc_[:, :, 0:n_], zi_b, ALU.mult, [TK], [TK])
            TT("dve", td_[:, :, n_:2 * n_], td_[:, :, n_:2 * n_], q1[:, :, 0:n_], ALU.add, [TK, "q1"], [TK])
        S.pool(lambda e, ps_=ps_: e.tensor_copy(out=Rt[:, :, :], in_=R8[:, ps_].unsqueeze(2).to_broadcast([128, 8, 256])),
               [K_, "Rt"], ["Rt"])
        MEMSET("pool", Rt[:, :, 0:1], 0.0, ["Rt"])
        Er = Eb[:, :, 0:256]
        Ei = Eb[:, :, 256:512]
        TT("dve", q1[:, :, :], tc_[:, :, :], Er, ALU.mult, [TK, "q1"] + EK, ["q1"])
        TT("pool", q2[:, :, :], td_[:, :, :], Ei, ALU.mult, [TK, "q2"] + EK, ["q2"])
        TT("dve", q1[:, :, :], q1[:, :, :], q2[:, :, :], ALU.add, ["q1", "q2"], ["q1"])
        TT("pool", q2[:, :, :], tc_[:, :, :], Ei, ALU.mult, [TK, "q2"] + EK, ["q2"])
        TT("dve", Er, td_[:, :, :], Er, ALU.mult, [TK] + EK, ["Er"])
        TT("dve", q2[:, :, :], q2[:, :, :], Er, ALU.subtract, ["q2", "Er"], ["q2"])
        q1f = q1[:, :, :].rearrange("p a s -> p (a s)")
        q2f = q2[:, :, :].rearrange("p a s -> p (a s)")
        Rtf = Rt[:, :, :].rearrange("p a s -> p (a s)")
        S.dve(lambda e, q1f=q1f, Rtf=Rtf: e.tensor_tensor_scan(out=q1f, data0=Rtf, data1=q1f, initial=0.0,
                                                               op0=ALU.mult, op1=ALU.add), ["q1", "Rt"], ["q1"])
        S.dve(lambda e, q2f=q2f, Rtf=Rtf: e.tensor_tensor_scan(out=q2f, data0=Rtf, data1=q2f, initial=0.0,
                                                               op0=ALU.mult, op1=ALU.add), ["q2", "Rt"], ["q2"])
        TT("pool", Er, tc_[:, :, :], q1[:, :, :], ALU.mult, [TK, "q1", "Er"], ["Er"])
        TT("dve", Ei, td_[:, :, :], q2[:, :, :], ALU.mult, [TK, "q2"] + EK, ["Ei"])
        TT("dve", Spr[:, ps_, 1:256], Eb[:, :, 0:255], Eb[:, :, 256:511], ALU.subtract, ["Er", "Ei"], [("Spr", hb_)])
        TT("pool", Er, tc_[:, :, :], q2[:, :, :], ALU.mult, [TK, "q2", "Er", ("Spr", hb_)], ["Er"])
        TT("dve", Ei, td_[:, :, :], q1[:, :, :], ALU.mult, [TK, "q1", "Ei", ("Spr", hb_)], ["Ei"])
        TT("dve", Spi[:, ps_, 1:256], Eb[:, :, 0:255], Eb[:, :, 256:511], ALU.add, ["Er", "Ei"], [("Spi", hb_)])
    S.barrier()

    for i_ in range(8):
        k = i_ % 2
        emit_bg(3)
        wide_build(k, czr[:, :, :], czi[:, :, :], b32(LPr[:, :, i_ + 1]), b32(LPi[:, :, i_ + 1]), True, [])
        wk = [("ww", k, 0), ("ww", k, 1)]
        for ch in range(4):
            b = nbank()
            for kk in range(i_ + 1):
                MM(bank(b)[:, 0:256], KT[:, ch, i_ - kk, :], uT[:, ch, kk:T:8], kk == 0, False, [], [("ps", b)])
            for j in range(4):
                p = 4 * ch + j
                MM(bank(b)[:, 0:256], WW[k][0][:, p, :], Spr[:, p, :], False, False, [wk[0]], [("ps", b)])
                MM(bank(b)[:, 0:256], WW[k][1][:, p, :], Spi[:, p, :], False, j == 3, [wk[1]], [("ps", b)])
            ACT(gT[:, ch, i_:T:8], bank(b)[:, 0:256], AF.Gelu, [("ps", b)], [("gT", ch)])
    if debug:
        d_gT = dbg_out("d_gT", [128, 4, T], BF16)
        DMA("sp", d_gT[:, :, :], gT[:, :, :], [("gT", m) for m in range(4)], ())
    S.barrier()
    if stop_after == "C":
        S.run()
        return nc, dbg

    zT = sbt("zT", [128, 4, T + 32], BF16, R2)
    HO = 32
    cT = sbt("cT", [128, 4, T], BF16, R4)
    zc = sbt("zc", [128, 4, T], F32, R5)
    dgm = [sbt(f"dgm{k}", [128, 31, 128], BF16, R0 + k * 8 * KB) for k in range(2)]
    sg_t = [sbt(f"sgt{k}", [128, 512], F32, R0 + 16 * KB + k * 2 * KB) for k in range(2)]
    lnm = sbt("lnm", [128, 512], F32, R0 + 20 * KB)
    lnr = sbt("lnr", [128, 512], F32, R0 + 22 * KB)
    lnt = [sbt(f"lnt{k}", [128, 512], F32, R0 + 24 * KB + k * 2 * KB) for k in range(2)]
    sqt = [sbt(f"sqt{k}", [128, 512], F32, R0 + 28 * KB + k * 2 * KB) for k in range(2)]
    load_win_block(1, 1)
    load_win_block(2, 0)
    MEMSET("pool", zT[:, :, 0:HO], 0.0, [("zT", m) for m in range(4)])
    for m in range(4):
        emit_bg(4)
        for n in range(NP_):
            bv, bg = nbank(), nbank()
            for c in range(8):
                MM(bank(bv), wbuf[1][:, c, m * 128:(m + 1) * 128], hT[:, c, n * 512:(n + 1) * 512], c == 0, c == 7,
                   [("wbuf", 1, c // 4)], [("ps", bv)])
            for c in range(8):
                MM(bank(bg), wbuf[0][:, c, m * 128:(m + 1) * 128], hT[:, c, n * 512:(n + 1) * 512], c == 0, c == 7,
                   [("wbuf", 0, c // 4)], [("ps", bg)])
            kk = (m * NP_ + n) % 2
            ACT(sg_t[kk][:, :], bank(bg), AF.Sigmoid, [("ps", bg)], [("sgt", kk)])
            TT("dve", zT[:, m, HO + n * 512:HO + (n + 1) * 512], bank(bv), sg_t[kk][:, :], ALU.mult,
               [("ps", bv), ("sgt", kk)], [("zT", m)])
    if debug:
        d_zT = dbg_out("d_zT", [128, 4, T + 32], BF16)
        DMA("sp", d_zT[:, :, :], zT[:, :, :], [("zT", m) for m in range(4)], ())
    for m in range(4):
        k = m % 2
        emit_bg(6)
        for tp_ in range(31):
            TS("dve", dgm[k][:, tp_, :], identb[:, :], convw[:, m, tp_:tp_ + 1], None, ALU.mult, None,
               ["identb", "convw"], [("dgm", k)])
        for n in range(NP_):
            b = nbank()
            for tp_ in range(31):
                s0 = HO + n * 512 + tp_ - 30
                MM(bank(b), dgm[k][:, tp_, :], zT[:, m, s0:s0 + 512], tp_ == 0, tp_ == 30,
                   [("dgm", k), ("zT", m)], [("ps", b)])
            ACT(zc[:, m, n * 512:(n + 1) * 512], bank(b), AF.Identity, [("ps", b), "convp"], [("zc", m, n)],
                bias=convp[:, m:m + 1])
    if debug:
        d_zc = dbg_out("d_zc", [128, 4, T], F32)
        DMA("sp", d_zc[:, :, :], zc[:, :, :], [("zc", m, n) for m in range(4) for n in range(NP_)], ())
    for n in range(NP_):
        sl = slice(n * 512, (n + 1) * 512)
        emit_bg(2)
        bm, bq = nbank(), nbank()
        for m in range(4):
            MM(bank(bm), onesf[:, :], zc[:, m, sl], m == 0, m == 3, ["onesf", ("zc", m, n)], [("ps", bm)])
        for m in range(4):
            kk = m % 2
            ACT(sqt[kk][:, :], zc[:, m, sl], AF.Square, [("zc", m, n)], [("sqt", kk)])
            MM(bank(bq), onesf[:, :], sqt[kk][:, :], m == 0, m == 3, ["onesf", ("sqt", kk)], [("ps", bq)])
        ACT(lnm[:, :], bank(bm), AF.Copy, [("ps", bm)], ["lnm"])
        TT("dve", lnr[:, :], lnm[:, :], lnm[:, :], ALU.mult, ["lnm"], ["lnr"])
        STT(lnr[:, :], bank(bq), EPS, lnr[:, :], ALU.add, ALU.subtract, [("ps", bq), "lnr"], ["lnr"])
        ACT(lnr[:, :], lnr[:, :], AF.Sqrt, ["lnr"], ["lnr"])
        S.dve(lambda e: e.reciprocal(out=lnr[:, :], in_=lnr[:, :]), ["lnr"], ["lnr"])
        for m in range(4):
            kk = m % 2
            TT("dve", lnt[kk][:, :], zc[:, m, sl], lnm[:, :], ALU.subtract, [("zc", m, n), "lnm"], [("lnt", kk)])
            TT("dve", lnt[kk][:, :], lnt[kk][:, :], lnr[:, :], ALU.mult, [("lnt", kk), "lnr"], [("lnt", kk)])
            ACT(cT[:, m, sl], lnt[kk][:, :], AF.Silu, [("lnt", kk), "convp"], [("cT", m)],
                scale=convp[:, 4 + m:5 + m], bias=convp[:, 8 + m:9 + m])
    if debug:
        d_cT = dbg_out("d_cT", [128, 4, T], BF16)
        DMA("sp", d_cT[:, :, :], cT[:, :, :], [("cT", m) for m in range(4)], ())
    S.barrier()
    if stop_after == "D":
        S.run()
        return nc, dbg

    mT = sbt("mT", [128, 8, T], BF16, R5)
    wE = []
    for k in range(2):
        o = R6 + k * 8 * KB
        wE.append(dict(
            gv=sbt(f"wE_gv{k}", [128, 4, 128], BF16, o),
            gg=sbt(f"wE_gg{k}", [128, 4, 128], BF16, o + 1 * KB),
            co=sbt(f"wE_co{k}", [128, 4, 128], BF16, o + 2 * KB),
            gs=sbt(f"wE_gs{k}", [128, 8, 128], BF16, o + 3 * KB),
            gc=sbt(f"wE_gc{k}", [128, 8, 128], BF16, o + 5 * KB)))
    wglu_v = w_glu_d.rearrange("(c p) n -> p c n", p=128)
    wco_v = w_co_d.rearrange("(c p) n -> p c n", p=128)
    et = [sbt(f"et{k}", [128, 512], F32, R0 + k * 2 * KB) for k in range(6)]

    def load_wE(fc):
        k = fc % 2
        w = wE[k]
        ky = ("wE", k)
        DMA("pool", w["gv"][:, :, :], wglu_v[:, :, fc * 128:(fc + 1) * 128], (), [(ky, "gv")])
        DMA("pool", w["gg"][:, :, :], wglu_v[:, :, 1024 + fc * 128:1024 + (fc + 1) * 128], (), [(ky, "gg")])
        DMA("pool", w["co"][:, :, :], wco_v[:, :, fc * 128:(fc + 1) * 128], (), [(ky, "co")])
        DMA("pool", w["gs"][:, :, :], win_v[:, :, 1536 + fc * 128:1536 + (fc + 1) * 128], (), [(ky, "gs")])
        DMA("pool", w["gc"][:, :, :], win_v[:, :, 2560 + fc * 128:2560 + (fc + 1) * 128], (), [(ky, "gc")])

    load_wE(0)
    wout = sbt("wout", [128, 8, D], BF16, R2)
    wout_v = w_out_d.rearrange("(c p) n -> p c n", p=128)
    for c2 in range(4):
        DMA("pool", wout[:, c2 * 2:(c2 + 1) * 2, :], wout_v[:, c2 * 2:(c2 + 1) * 2, :], (), [("wout", c2)])
    for i in range(3, NT):
        DMA("sp", XRES[:, i, :], x_d[i * 128:(i + 1) * 128, :], (), [("xres", i)])
    for fc in range(8):
        emit_bg(4)
        if fc + 1 < 8:
            load_wE(fc + 1)
        k = fc % 2
        w = wE[k]
        ky = ("wE", k)
        for n in range(NP_):
            sl = slice(n * 512, (n + 1) * 512)
            bzv, bzg, byc, bgs, bgc = nbank(), nbank(), nbank(), nbank(), nbank()
            for c in range(4):
                MM(bank(bzv), w["gv"][:, c, :], gT[:, c, sl], c == 0, c == 3, [(ky, "gv")], [("ps", bzv)])
            for c in range(4):
                MM(bank(bzg), w["gg"][:, c, :], gT[:, c, sl], c == 0, c == 3, [(ky, "gg")], [("ps", bzg)])
            for c in range(4):
                MM(bank(byc), w["co"][:, c, :], cT[:, c, sl], c == 0, c == 3, [(ky, "co")], [("ps", byc)])
            for c in range(8):
                MM(bank(bgs), w["gs"][:, c, :], hT[:, c, sl], c == 0, c == 7, [(ky, "gs")], [("ps", bgs)])
            for c in range(8):
                MM(bank(bgc), w["gc"][:, c, :], hT[:, c, sl], c == 0, c == 7, [(ky, "gc")], [("ps", bgc)])
            ACT(et[0][:, :], bank(bzg), AF.Sigmoid, [("ps", bzg)], [("et", 0)])
            ACT(et[1][:, :], bank(bgs), AF.Sigmoid, [("ps", bgs), "bgate"], [("et", 1)], bias=bgate[:, fc:fc + 1])
            ACT(et[2][:, :], bank(bgc), AF.Sigmoid, [("ps", bgc), "bgate"], [("et", 2)],
                bias=bgate[:, 8 + fc:9 + fc])
            TT("dve", et[3][:, :], bank(bzv), et[0][:, :], ALU.mult, [("ps", bzv), ("et", 0)], [("et", 3)])
            TT("pool", et[3][:, :], et[3][:, :], et[1][:, :], ALU.mult, [("et", 3), ("et", 1)], [("et", 3)])
            TT("dve", et[4][:, :], bank(byc), et[2][:, :], ALU.mult, [("ps", byc), ("et", 2)], [("et", 4)])
            TT("pool", mT[:, fc, sl], et[3][:, :], et[4][:, :], ALU.add, [("et", 3), ("et", 4)], [("mT", fc)])
    if debug:
        d_mT = dbg_out("d_mT", [128, 8, T], BF16)
        DMA("sp", d_mT[:, :, :], mT[:, :, :], [("mT", m) for m in range(8)], ())
    S.barrier()
    if stop_after == "E":
        S.run()
        return nc, dbg

    for i in range(3):
        DMA("sp", XRES[:, i, :], x_d[i * 128:(i + 1) * 128, :], (), [("xres", i)])
    emit_bg(1000)
    for i in range(NT):
        for hf in range(2):
            b = nbank()
            for c in range(8):
                MM(bank(b), mT[:, c, i * 128:(i + 1) * 128], wout[:, c, hf * 512:(hf + 1) * 512], c == 0, c == 7,
                   [("wout", c // 2)], [("ps", b)])
            TT("dve", XRES[:, i, hf * 512:(hf + 1) * 512], bank(b), XRES[:, i, hf * 512:(hf + 1) * 512], ALU.add,
               [("ps", b), ("xres", i)], [("xres", i)])
    if debug:
        d_x1 = dbg_out("d_x1", [128, NT, D], F32)
        for i in range(NT):
            DMA("sp", d_x1[:, i, :], XRES[:, i, :], [("xres", i)], ())
    S.barrier()
    if stop_after == "F":
        S.run()
        return nc, dbg

    I32 = mybir.dt.int32
    hb = sbt("hb", [128, NT, D], BF16, R1)
    wr = sbt("wr_s", [128, 8, 36], F32, R5 + 26 * KB)
    DMA("sp", wr[:, :, :], wr_d[:, :, :], (), ["wr"])
    gmb = sbt("gmb", [128, D], F32, R5 + 28 * KB)
    DMA("sp", gmb[:, :], gmoe_d.partition_broadcast(128), (), ["gmb"])
    w12 = calloc("w12", [128, 2, NT], F32)
    sidx = calloc("sidx", [128, NT, 2], I32)
    gidx = calloc("gidx", [128, NT, 2], I32)
    ltri = calloc("ltri", [128, 128], BF16)
    onesb = calloc("onesb", [128, 128], BF16)
    assert co[0] <= 207 * KB, co[0]
    MEMSET("pool", onesb[:, :], 1.0, ["onesb"])
    S.pool(lambda e: e.affine_select(out=ltri[:, :], in_=onesb[:, :], pattern=[[1, 128]],
                                     compare_op=ALU.is_gt, fill=0.0, base=0, channel_multiplier=-1),
           ["onesb"], ["ltri"])

    EW = 24 * KB
    ewg = [sbt(f"ewg{k}", [128, 8, 512], BF16, R2 + k * EW) for k in range(2)]
    ewu = [sbt(f"ewu{k}", [128, 8, 512], BF16, R2 + k * EW + 8 * KB) for k in range(2)]
    ewd = [sbt(f"ewd{k}", [128, 4, D], BF16, R2 + k * EW + 16 * KB) for k in range(2)]
    assert R2 + 2 * EW <= R5
    ewg.append(sbt("ewg2", [128, 8, 512], BF16, R1 + 16 * KB))
    ewu.append(sbt("ewu2", [128, 8, 512], BF16, R1 + 24 * KB))
    ewd.append(sbt("ewd2", [128, 4, D], BF16, R5 + 24 * KB))
    NWB = 3

    def load_expert(e_):
        k = e_ % NWB
        g_v = wgb_d[e_].rearrange("(c p) n -> p c n", p=128)
        u_v = wub_d[e_].rearrange("(c p) n -> p c n", p=128)
        d_v = wdb_d[e_].rearrange("(c p) n -> p c n", p=128)
        for c2 in range(2):
            DMA("sp", ewg[k][:, c2 * 4:(c2 + 1) * 4, :], g_v[:, c2 * 4:(c2 + 1) * 4, :], [("BGg", e_, c2)],
                [("ewg", k, c2)])
        for c2 in range(2):
            DMA("sp", ewu[k][:, c2 * 4:(c2 + 1) * 4, :], u_v[:, c2 * 4:(c2 + 1) * 4, :], [("BGu", e_, c2)],
                [("ewu", k, c2)])
        for c2 in range(2):
            DMA("sp", ewd[k][:, c2 * 2:(c2 + 1) * 2, :], d_v[:, c2 * 2:(c2 + 1) * 2, :], [("BGd", e_, c2)],
                [("ewd", k, c2)])

    load_expert(0)
    if n_exp > 1:
        load_expert(1)

    xn = [sbt(f"g_xn{k}", [128, D], F32, R6 + k * 4 * KB) for k in range(2)]
    h32 = [sbt(f"g_h32{k}", [128, 8, 128], F32, R6 + 8 * KB + k * 4 * KB) for k in range(2)]
    g_bc = gcols[:, 8:16].unsqueeze(2).to_broadcast([128, 8, 128])
    LB = [4, 5]
    for i in range(NT):
        ACT(hb[:, i, :], XRES[:, i, :], AF.Square, [("xres", i)], [("ss", i), ("hb", i)], accum_out=ss[:, i:i + 1])
    TS("dve", rs[:, :], ss[:, :], 1.0 / D, EPS, ALU.mult, ALU.add, [("ss", i) for i in range(NT)], ["rs"])
    ACT(rs[:, :], rs[:, :], AF.Sqrt, ["rs"], ["rs"])
    S.dve(lambda e: e.reciprocal(out=rs[:, :], in_=rs[:, :]), ["rs"], ["rs"])
    for i in range(NT):
        k = i % 2
        ACT(xn[k][:, :], XRES[:, i, :], AF.Copy, [("xres", i), "rs"], [("xn", k)], scale=rs[:, i:i + 1])
        TT("pool", hb[:, i, :], xn[k][:, :], gmb[:, :], ALU.mult, [("xn", k), "gmb"], [("hb", i)])
        pk = [("ps", 2 * k), ("ps", 2 * k + 1)]
        for c in range(8):
            TR(pst[k][:, c * 128:(c + 1) * 128], xn[k][:, c * 128:(c + 1) * 128], ident[:, :],
               [("xn", k), "ident"], pk)
        TT("dve", h32[k][:, :, :], pst[k][:, :].rearrange("p (c t) -> p c t", c=8), g_bc,
           ALU.mult, pk + ["gcols"], [("h32", k)])
        lb = LB[i // 8]
        col = (i % 8) * 36
        for c in range(8):
            MM(bank(lb)[:, col:col + 36], h32[k][:, c, :], wr[:, c, :], c == 0, c == 7, [("h32", k), "wr"],
               [("ps", lb)])

    ro = [R5]

    def ralloc(name, shape, dt=F32):
        nbytes = int(np.prod(shape[1:])) * (4 if dt in (F32, I32) else 2)
        nbytes = (nbytes + 31) // 32 * 32
        t = sbt(name, shape, dt, ro[0])
        ro[0] += nbytes
        return t

    lg = ralloc("r_lg", [128, NT, 36])
    gm = ralloc("r_gm", [128, NT])
    ohg = ralloc("r_ohg", [128, NT, 4])
    exg = ralloc("r_exg", [128, NT, 4])
    se = ralloc("r_se", [128, NT])
    pg = ralloc("r_pg", [128, NT])
    pen = ralloc("r_pen", [128, NT, 4])
    me = ralloc("r_me", [128, NT, 32])
    me2 = ralloc("r_me2", [128, NT, 32])
    oh1 = ralloc("r_oh1", [128, NT, 32])
    oh2 = ralloc("r_oh2", [128, NT, 32])
    v1 = ralloc("r_v1", [128, NT])
    v2 = ralloc("r_v2", [128, NT])
    dv = ralloc("r_dv", [128, NT])
    maskb = ralloc("r_mask", [128, NT, 32], BF16)
    posf = ralloc("r_pos", [128, NT, 32])
    posc = ralloc("r_posc", [128, NT, 32])
    ecap = ralloc("r_ecap", [128, NT, 32])
    tmpr = ralloc("r_tmp", [128, NT, 32])
    sj = ralloc("r_sj", [128, 2, NT])
    pj = ralloc("r_pj", [128, 2, NT])
    sg_ = ralloc("r_sg", [128, 2, NT])
    zrow = ralloc("zrow", [1, D])
    assert ro[0] <= R5 + 26 * KB, ro[0]
    MEMSET("dve", zrow[:, :], 0.0, ["zrow"])
    DMA("sp", Ys[NSLOT:NSLOT + 1, :], zrow[:, :], ["zrow"], ["Ys_zero"])
    RK = "route"
    brb_b = brb[:, :].unsqueeze(1).to_broadcast([128, 8, 36])
    for hlf in range(2):
        TT("dve", lg[:, hlf * 8:(hlf + 1) * 8, :], bank(LB[hlf])[:, 0:288].rearrange("p (t c) -> p t c", t=8),
           brb_b, ALU.add, [("ps", LB[hlf]), "brb"], [RK])

    def rd(fn):
        S.dve(fn, [RK], [RK])

    def bc3(ap2, n_):
        return ap2.unsqueeze(2).to_broadcast([128, NT, n_])

    rd(lambda e: e.tensor_reduce(out=gm[:, :], in_=lg[:, :, 0:4], axis=AX.X, op=ALU.max))
    rd(lambda e: e.tensor_tensor(out=ohg[:, :, :], in0=lg[:, :, 0:4], in1=bc3(gm[:, :], 4), op=ALU.is_equal))
    rd(lambda e: e.tensor_tensor(out=exg[:, :, :], in0=lg[:, :, 0:4], in1=bc3(gm[:, :], 4), op=ALU.subtract))
    ACT(exg[:, :, :], exg[:, :, :], AF.Exp, [RK], [RK])
    rd(lambda e: e.tensor_reduce(out=se[:, :], in_=exg[:, :, :], axis=AX.X, op=ALU.add))
    rd(lambda e: e.reciprocal(out=pg[:, :], in_=se[:, :]))
    rd(lambda e: e.tensor_scalar(out=pen[:, :, :], in0=ohg[:, :, :], scalar1=-1.0, scalar2=1e30,
                                 op0=ALU.add, op1=ALU.mult))
    rd(lambda e: e.tensor_tensor(out=me[:, :, :].rearrange("p t (g j) -> p t g j", g=4),
                                 in0=lg[:, :, 4:36].rearrange("p t (g j) -> p t g j", g=4),
                                 in1=pen[:, :, :].unsqueeze(3).to_broadcast([128, NT, 4, 8]), op=ALU.add))
    rd(lambda e: e.tensor_reduce(out=v1[:, :], in_=me[:, :, :], axis=AX.X, op=ALU.max))
    rd(lambda e: e.tensor_tensor(out=oh1[:, :, :], in0=me[:, :, :], in1=bc3(v1[:, :], 32), op=ALU.is_equal))
    rd(lambda e: e.scalar_tensor_tensor(out=me2[:, :, :], in0=oh1[:, :, :], scalar=-1e30, in1=me[:, :, :],
                                        op0=ALU.mult, op1=ALU.add))
    rd(lambda e: e.tensor_reduce(out=v2[:, :], in_=me2[:, :, :], axis=AX.X, op=ALU.max))
    rd(lambda e: e.tensor_tensor(out=oh2[:, :, :], in0=me2[:, :, :], in1=bc3(v2[:, :], 32), op=ALU.is_equal))
    rd(lambda e: e.tensor_tensor(out=dv[:, :], in0=v1[:, :], in1=v2[:, :], op=ALU.subtract))
    ACT(dv[:, :], dv[:, :], AF.Sigmoid, [RK], [RK])
    rd(lambda e: e.tensor_tensor(out=w12[:, 0, :], in0=dv[:, :], in1=pg[:, :], op=ALU.mult))
    rd(lambda e: e.tensor_tensor(out=w12[:, 1, :], in0=pg[:, :], in1=w12[:, 0, :], op=ALU.subtract))
    rd(lambda e: e.tensor_tensor(out=maskb[:, :, :], in0=oh1[:, :, :], in1=oh2[:, :, :], op=ALU.add))
    PB = 6
    for i in range(NT):
        MM(bank(PB)[:, i * 32:(i + 1) * 32], ltri[:, :], maskb[:, i, :], True, i == 0, [RK, "ltri"], [("ps", PB)])
        for i2 in range(i):
            MM(bank(PB)[:, i * 32:(i + 1) * 32], onesb[:, :], maskb[:, i2, :], False, i2 == i - 1,
               [RK, "onesb"], [("ps", PB)])
    S.act(lambda e: e.activation(out=posf[:, :, :], in_=bank(PB)[:, :].rearrange("p (t c) -> p t c", t=NT),
                                 func=AF.Copy), [("ps", PB)], [RK])
    S.pool(lambda e: e.iota(ecap[:, :, :], pattern=[[0, NT], [CAP, 32]], base=0, channel_multiplier=0,
                            allow_small_or_imprecise_dtypes=True), [RK], [RK])
    rd(lambda e: e.tensor_tensor(out=posc[:, :, :], in0=posf[:, :, :], in1=ecap[:, :, :], op=ALU.add))
    for j, oh in enumerate((oh1, oh2)):
        rd(lambda e, oh=oh: e.tensor_tensor(out=tmpr[:, :, :], in0=posc[:, :, :], in1=oh[:, :, :], op=ALU.mult))
        rd(lambda e, j=j: e.tensor_reduce(out=sj[:, j, :], in_=tmpr[:, :, :], axis=AX.X, op=ALU.add))
        rd(lambda e, oh=oh: e.tensor_tensor(out=tmpr[:, :, :], in0=posf[:, :, :], in1=oh[:, :, :], op=ALU.mult))
        rd(lambda e, j=j: e.tensor_reduce(out=pj[:, j, :], in_=tmpr[:, :, :], axis=AX.X, op=ALU.add))
    rd(lambda e: e.tensor_scalar(out=pj[:, :, :], in0=pj[:, :, :], scalar1=float(CAP) - 0.5, scalar2=1.0e6,
                                 op0=ALU.is_gt, op1=ALU.mult))
    rd(lambda e: e.tensor_tensor(out=sj[:, :, :], in0=sj[:, :, :], in1=pj[:, :, :], op=ALU.add))
    rd(lambda e: e.tensor_scalar(out=sg_[:, :, :], in0=sj[:, :, :], scalar1=float(NSLOT), scalar2=None,
                                 op0=ALU.min))
    S.dve(lambda e: e.tensor_copy(out=sidx[:, :, :].rearrange("p t j -> p j t"), in_=sj[:, :, :]), [RK], ["sidx"])
    S.dve(lambda e: e.tensor_copy(out=gidx[:, :, :].rearrange("p t j -> p j t"), in_=sg_[:, :, :]), [RK], ["gidx"])
    if debug:
        d_w12 = dbg_out("d_w12", [128, 2, NT], F32)
        DMA("sp", d_w12[:, :, :], w12[:, :, :], [RK], ())
        d_sj = dbg_out("d_sj", [128, 2, NT], F32)
        DMA("sp", d_sj[:, :, :], sj[:, :, :], [RK], ())
        d_sidx = dbg_out("d_sidx", [128, NT, 2], I32)
        DMA("sp", d_sidx[:, :, :], sidx[:, :, :], ["sidx"], ())
        d_gidx = dbg_out("d_gidx", [128, NT, 2], I32)
        DMA("sp", d_gidx[:, :, :], gidx[:, :, :], ["gidx"], ())
        d_oh = dbg_out("d_oh", [128, 2, NT, 32], F32)
        DMA("sp", d_oh[:, 0, :, :], oh1[:, :, :], [RK], ())
        DMA("sp", d_oh[:, 1, :, :], oh2[:, :, :], [RK], ())

    XS_KEYS = []
    for i in range(NT):
        for j in range(2):
            ky = ("Xs", i, j)
            XS_KEYS.append(ky)
            S.dma("pool", lambda e, i=i, j=j: e.indirect_dma_start(
                out=Xs[:, :], out_offset=bass.IndirectOffsetOnAxis(ap=sidx[:, i, j:j + 1], axis=0),
                in_=hb[:, i, :], in_offset=None, bounds_check=NSLOT - 1, oob_is_err=False),
                [("hb", i), "sidx"] + (XS0_KEYS if (i == 0 and j == 0) else []), [ky])
    S.barrier()
    if stop_after == "G1":
        S.run()
        return nc, dbg

    SB_ = CAP // 128
    Xb = [sbt(f"Xb{k}", [128, SB_, D], BF16, R5 + k * 4 * KB) for k in range(2)]
    XT = [sbt(f"XT{k}", [128, 8, CAP], BF16, R5 + 8 * KB + k * 4 * KB) for k in range(2)]
    ATs = [sbt(f"ATs{k}", [128, 4, CAP], BF16, R5 + 16 * KB + k * 2 * KB) for k in range(2)]
    Yb = [sbt(f"Yb{k}", [128, SB_, D], F32, R1 + k * 8 * KB) for k in range(2)]
    sgm = [sbt(f"sgm{k}", [128, CAP], F32, R5 + 20 * KB + k * KB) for k in range(4)]
    Yg = [sbt(f"Yg{k}", [128, D], F32, R6 + k * 4 * KB) for k in range(4)]
    sgi = 0
    YS_KEYS = []
    tb = pst[0][:, :].bitcast(BF16)

    def emit_T(e_):
        k = e_ % 2
        for s_ in range(SB_):
            for c in range(8):
                o0 = c * CAP + s_ * 128
                TR(tb[:, o0:o0 + 128], Xb[k][:, s_, c * 128:(c + 1) * 128], identb[:, :],
                   [("Xb", k), "identb"], [("ps", 0), ("ps", 1)])
        S.act(lambda e, k=k: e.activation(out=XT[k][:, :, :].rearrange("p c s -> p (c s)"), in_=tb[:, 0:8 * CAP],
                                          func=AF.Copy), [("ps", 0), ("ps", 1)], [("XT", k)])

    DMA("sp", Xb[0][:, :, :], Xs[0:CAP, :].rearrange("(s p) d -> p s d", p=128), [], [("Xb", 0)])
    if n_exp > 1:
        DMA("sp", Xb[1][:, :, :], Xs[CAP:2 * CAP, :].rearrange("(s p) d -> p s d", p=128), [], [("Xb", 1)])
    if n_exp > 2:
        load_expert(2)
    emit_T(0)
    for e_ in range(n_exp):
        k = e_ % 2
        kw = e_ % NWB
        for m in range(4):
            bgu = 2 + (m % 2)
            for c in range(8):
                MM(bank(bgu)[:, 0:CAP], ewg[kw][:, c, m * 128:(m + 1) * 128], XT[k][:, c, :], c == 0, c == 7,
                   [("ewg", kw, c // 4), ("XT", k)], [("ps", bgu)])
            for c in range(8):
                MM(bank(bgu)[:, CAP:2 * CAP], ewu[kw][:, c, m * 128:(m + 1) * 128], XT[k][:, c, :], c == 0, c == 7,
                   [("ewu", kw, c // 4), ("XT", k)], [("ps", bgu)])
            sx = sgi % 4
            sgi += 1
            ACT(sgm[sx][:, :], bank(bgu)[:, 0:CAP], AF.Silu, [("ps", bgu)], [("sgm", sx)])
            TT("dve", ATs[k][:, m, :], bank(bgu)[:, CAP:2 * CAP], sgm[sx][:, :], ALU.mult,
               [("ps", bgu), ("sgm", sx)], [("ATs", k, m)])
        if e_ + 1 < n_exp:
            emit_T(e_ + 1)
        if e_ + 2 < n_exp:
            DMA("sp", Xb[k][:, :, :], Xs[(e_ + 2) * CAP:(e_ + 3) * CAP, :].rearrange("(s p) d -> p s d", p=128),
                [], [("Xb", k)])
        for s_ in range(SB_):
            for hf in range(2):
                b = 4 + (s_ * 2 + hf) % 4
                for m in range(4):
                    MM(bank(b), ATs[k][:, m, s_ * 128:(s_ + 1) * 128], ewd[kw][:, m, hf * 512:(hf + 1) * 512],
                       m == 0, m == 3, [("ewd", kw, m // 2), ("ATs", k, m)], [("ps", b)])
                if hf == 0:
                    ACT(Yb[k][:, s_, hf * 512:(hf + 1) * 512], bank(b), AF.Copy, [("ps", b)], [("Yb", k, s_, hf)])
                else:
                    S.dve(lambda e, s_=s_, hf=hf, b=b, k=k: e.tensor_copy(out=Yb[k][:, s_, hf * 512:(hf + 1) * 512],
                                                                         in_=bank(b)), [("ps", b)], [("Yb", k, s_, hf)])
        yk = ("Ys", e_)
        YS_KEYS.append(yk)
        DMA("act", Ys[e_ * CAP:(e_ + 1) * CAP, :].rearrange("(s p) d -> p s d", p=128), Yb[k][:, :, :],
            [("Yb", k, s_, hf) for s_ in range(SB_) for hf in range(2)], [yk])
        if e_ + 3 < n_exp:
            load_expert(e_ + 3)

    gi_ = 0
    for i in range(NT):
        for j in range(2):
            kk = gi_ % 4
            gi_ += 1
            S.dma("pool", lambda e, i=i, j=j, kk=kk: e.indirect_dma_start(
                out=Yg[kk][:, :], out_offset=None, in_=Ys[:, :],
                in_offset=bass.IndirectOffsetOnAxis(ap=gidx[:, i, j:j + 1], axis=0)),
                (YS_KEYS + ["Ys_zero", "gidx"]) if gi_ <= 4 else ["gidx"], [("Yg", kk)])
            STT(XRES[:, i, :], Yg[kk][:, :], w12[:, j, i:i + 1], XRES[:, i, :], ALU.mult, ALU.add,
                [("Yg", kk), ("xres", i), RK], [("xres", i)])
    if debug:
        d_x2 = dbg_out("d_x2", [128, NT, D], F32)
        for i in range(NT):
            DMA("sp", d_x2[:, i, :], XRES[:, i, :], [("xres", i)], ())
    S.barrier()
    if stop_after == "G":
        S.run()
        return nc, dbg


    wpg = sbt("wpg", [128, 8, D], BF16, R2)
    wpl = sbt("wpl", [128, 2, D], BF16, R2 + 16 * KB)
    pT = sbt("pT", [128, 2, T], BF16, R2 + 20 * KB)
    pin = [sbt(f"pin{k}", [128, 256], F32, R2 + 28 * KB + k * KB) for k in range(2)]
    ht_ = [sbt(f"ht{k}", [128, 512], F32, R2 + 30 * KB + k * 2 * KB) for k in range(4)]
    wpg_v = w_pg_d.rearrange("(c p) n -> p c n", p=128)
    wpl_v = w_ple_d.rearrange("(c p) n -> p c n", p=128)
    for c2 in range(4):
        DMA("pool", wpg[:, c2 * 2:(c2 + 1) * 2, :], wpg_v[:, c2 * 2:(c2 + 1) * 2, :], (), [("wpg", c2)])
    DMA("pool", wpl[:, :, :], wpl_v[:, :, :], (), ["wpl"])
    norm_T(2, R5)
    for i in range(NT):
        k = i % 2
        DMA("sp", pin[k][:, :], p_d[i * 128:(i + 1) * 128, :], (), [("pin", k)])
        b = nbank()
        for c in range(2):
            TR(bank(b)[:, c * 128:(c + 1) * 128], pin[k][:, c * 128:(c + 1) * 128], ident[:, :],
               [("pin", k), "ident"], [("ps", b)])
        S.act(lambda e, b=b, i=i: e.activation(out=pT[:, :, i * 128:(i + 1) * 128],
                                               in_=bank(b)[:, 0:256].rearrange("p (c t) -> p c t", c=2),
                                               func=AF.Copy), [("ps", b)], [("pT", i)])
    hi = 0
    for i in range(NT):
        for hf in range(2):
            b1, b2 = nbank(), nbank()
            hs = slice(hf * 512, (hf + 1) * 512)
            for c in range(8):
                MM(bank(b1), hT[:, c, i * 128:(i + 1) * 128], wpg[:, c, hs], c == 0, c == 7,
                   [("wpg", c // 2), ("hT", i)], [("ps", b1)])
            for c in range(2):
                MM(bank(b2), pT[:, c, i * 128:(i + 1) * 128], wpl[:, c, hs], c == 0, c == 1,
                   ["wpl", ("pT", i)], [("ps", b2)])
            a_, b_ = hi % 4, (hi + 1) % 4
            hi += 2
            ACT(ht_[a_][:, :], bank(b1), AF.Sigmoid, [("ps", b1)], [("ht", a_)])
            TT("dve", ht_[b_][:, :], bank(b2), ht_[a_][:, :], ALU.mult, [("ps", b2), ("ht", a_)], [("ht", b_)])
            TT("pool", XRES[:, i, hs], XRES[:, i, hs], ht_[b_][:, :], ALU.add, [("ht", b_), ("xres", i)],
               [("xres", i)])

    gfb = sbt("gfb", [128, D], F32, R6)
    ob = [sbt(f"ob{k}", [128, D], F32, R6 + 4 * KB + k * 4 * KB) for k in range(2)]
    junk2 = sbt("junk2", [128, D], BF16, R6 + 12 * KB)
    ss2 = calloc("ss2", [128, 16], F32)
    rs2 = calloc("rs2", [128, 16], F32)
    assert co[0] <= 212736, co[0]
    DMA("sp", gfb[:, :], gfin_d.partition_broadcast(128), (), ["gfb"])
    for i in range(NT):
        k = i % 2
        ACT(junk2[:, :], XRES[:, i, :], AF.Square, [("xres", i)], ["junk2", ("ss2", i)], accum_out=ss2[:, i:i + 1])
        TS("dve", rs2[:, i:i + 1], ss2[:, i:i + 1], 1.0 / D, EPS, ALU.mult, ALU.add, [("ss2", i)], [("rs2", i)])
        ACT(rs2[:, i:i + 1], rs2[:, i:i + 1], AF.Sqrt, [("rs2", i)], [("rs2", i)])
        S.dve(lambda e, i=i: e.reciprocal(out=rs2[:, i:i + 1], in_=rs2[:, i:i + 1]), [("rs2", i)], [("rs2", i)])
        STT(ob[k][:, :], XRES[:, i, :], rs2[:, i:i + 1], gfb[:, :], ALU.mult, ALU.mult,
            [("xres", i), ("rs2", i), "gfb"], [("ob", k)])
        DMA("sp" if i % 2 == 0 else "pool", out_d[i * 128:(i + 1) * 128, :], ob[k][:, :], [("ob", k)], ())
    S.run()
    return nc, dbg


def prep_shared(inp):
    f = np.float32
    sh = {}
    sh["w_in"] = np.ascontiguousarray(inp["w_in"][0], f)
    sh["w_glu"] = np.ascontiguousarray(inp["w_glu"][0], f)
    sh["w_conv_out"] = np.ascontiguousarray(inp["w_conv_out"][0], f)
    sh["w_out"] = np.ascontiguousarray(inp["w_out"][0], f)
    sh["w_ple_gate"] = np.ascontiguousarray(inp["w_ple_gate"][0], f)
    sh["w_ple"] = np.ascontiguousarray(inp["w_ple"][0], f)
    sh["w_exp_gate"] = np.ascontiguousarray(inp["w_exp_gate"][0], f)
    sh["w_exp_up"] = np.ascontiguousarray(inp["w_exp_up"][0], f)
    sh["w_exp_down"] = np.ascontiguousarray(inp["w_exp_down"][0], f)

    def col8(v):
        return np.asarray(v, f).reshape(-1, 128).T

    sh["gcols"] = np.ascontiguousarray(np.concatenate(
        [col8(inp["g_mix"][0]), col8(inp["g_moe"][0]), col8(inp["g_ple"][0])], axis=1))
    sh["bgate"] = np.ascontiguousarray(col8(inp["b_gate"][0]))
    sh["convw"] = np.ascontiguousarray(np.asarray(inp["conv_dw"][0], f).T.reshape(4, 128, 31).transpose(1, 0, 2))
    sh["convp"] = np.ascontiguousarray(np.concatenate(
        [col8(inp["conv_dw_b"][0]), col8(inp["conv_ln_g"][0]), col8(inp["conv_ln_b"][0])], axis=1))
    sh["ssmd"] = np.ascontiguousarray(col8(inp["ssm_d"][0]))

    def colpair(a):
        return np.asarray(a, f).reshape(16, 128).T

    ldt = np.repeat(np.asarray(inp["ssm_log_dt"][0], f)[:, None], 64, axis=1)
    sh["ssmcol"] = np.ascontiguousarray(np.concatenate(
        [colpair(inp["ssm_a_re"][0]), colpair(inp["ssm_a_im"][0]), colpair(ldt)], axis=1))

    def col_masked(A, transpose):
        A = np.asarray(A, f)
        o = np.zeros((2, 64, 16, 2, 16), f)
        for p in range(16):
            for g2 in range(2):
                g = 2 * p + g2
                o[g2, :, p, g2, :] = A[g].T if transpose else A[g]
        return np.ascontiguousarray(o.reshape(128, 16, 32))

    sh["bcol_re"] = col_masked(inp["ssm_b_re"][0], False)
    sh["bcol_im"] = col_masked(inp["ssm_b_im"][0], False)
    sh["ccol_re"] = col_masked(inp["ssm_c_re"][0], True)
    sh["ccol_im"] = col_masked(inp["ssm_c_im"][0], True)
    wrc = np.concatenate([np.asarray(inp["w_router_group"][0], f), np.asarray(inp["w_router_expert"][0], f)], axis=1)
    sh["wr"] = np.ascontiguousarray(wrc.reshape(8, 128, 36).transpose(1, 0, 2))
    sh["br"] = np.ascontiguousarray(np.concatenate(
        [np.asarray(inp["b_router_group"][0], f), np.asarray(inp["b_router_expert"][0], f)]))
    sh["gfin"] = np.ascontiguousarray(inp["g_final"], f)
    sh["gmoe"] = np.ascontiguousarray(inp["g_moe"][0], f)
    return sh


def kernel(**inputs):
    inp = {k: np.asarray(v) for k, v in inputs.items()}
    sh = prep_shared(inp)
    nc, _ = build_nc()
    x = np.asarray(inp["x"], np.float32)
    p = np.asarray(inp["p"][0], np.float32)
    in_maps = []
    for b in range(8):
        m = dict(sh)
        m["x"] = np.ascontiguousarray(x[b])
        m["p"] = np.ascontiguousarray(p[b])
        in_maps.append(m)
    res = run_bass_kernel_spmd(nc, in_maps, core_ids=list(range(8)))
    return np.stack([np.asarray(r["out"], np.float32) for r in res.results], axis=0)
```
